# Optimizing a Trainium2 kernel written in Bass

```python
import math
import jax, jax.numpy as jnp
from jax import lax
import numpy as np

D_MODEL = 1024
BATCH = 8
SEQ = 4096
DEPTH = 2

CHUNK = 64
NORM_EPS = 1e-6
S5_WIDTH = D_MODEL // 2
S5_GROUP = 16
S5_GROUPS = S5_WIDTH // S5_GROUP
S5_STATE = 64
CONV_WIDTH = D_MODEL // 2
CONV_KERNEL = 31
EVEN_IN = 2 * S5_WIDTH + 3 * CONV_WIDTH
N_HEADS = 16
HEAD_DIM = D_MODEL // N_HEADS
N_KV_HEADS = 4
Q_PER_KV = N_HEADS // N_KV_HEADS
ATT_WIDTH = N_HEADS * HEAD_DIM
KV_WIDTH = N_KV_HEADS * HEAD_DIM
IDX_HEADS = 8
IDX_DIM = 64
TOPK_MAX = 256
ODD_IN = 2 * ATT_WIDTH + 2 * KV_WIDTH + IDX_HEADS * IDX_DIM + IDX_DIM + IDX_HEADS
ROPE_THETA = 500000.0
ROT_DIM = HEAD_DIM // 4
N_EVEN = (DEPTH + 1) // 2
N_ODD = DEPTH // 2

kernel_name = "chunk_causal_s5_conformer_dsa_hybrid"


def rms_norm(x, g):
    xf = x.astype(jnp.float32)
    y = xf * lax.rsqrt(jnp.mean(xf * xf, axis=-1, keepdims=True) + NORM_EPS)
    return (y * g.astype(jnp.float32)).astype(x.dtype)


def rope_tables(length):
    pos = jnp.arange(length, dtype=jnp.float32)
    inv = ROPE_THETA ** (-jnp.arange(0, ROT_DIM, 2, dtype=jnp.float32) / ROT_DIM)
    ang = pos[:, None] * inv[None, :]
    return jnp.cos(ang), jnp.sin(ang)


def partial_rope(x, cos, sin):
    half = ROT_DIM // 2
    x1, x2, rest = x[..., :half], x[..., half:ROT_DIM], x[..., ROT_DIM:]
    shape = (1, cos.shape[0]) + (1,) * (x.ndim - 3) + (half,)
    c = cos.reshape(shape).astype(x.dtype)
    s = sin.reshape(shape).astype(x.dtype)
    return jnp.concatenate([x1 * c - x2 * s, x1 * s + x2 * c, rest], axis=-1)


def s5_mixer(u, lam_re, lam_im, log_step, b_re, b_im, c_re, c_im, d_skip, w_glu, b_glu):
    f32 = jnp.float32
    bsz, length, _ = u.shape
    uf = u.astype(f32).reshape(bsz, length, S5_GROUPS, S5_GROUP)
    lr = jnp.minimum(lam_re.astype(f32), -1e-4)
    li = lam_im.astype(f32)
    dt = jnp.exp(log_step.astype(f32))[:, None]
    mag = jnp.exp(lr * dt)
    ab_re = mag * jnp.cos(li * dt)
    ab_im = mag * jnp.sin(li * dt)
    den = lr * lr + li * li
    nr = ab_re - 1.0
    ni = ab_im
    k_re = (nr * lr + ni * li) / den
    k_im = (ni * lr - nr * li) / den
    br = b_re.astype(f32)
    bi = b_im.astype(f32)
    bb_re = k_re[..., None] * br - k_im[..., None] * bi
    bb_im = k_re[..., None] * bi + k_im[..., None] * br
    x_re = jnp.einsum("blgc,gnc->blgn", uf, bb_re)
    x_im = jnp.einsum("blgc,gnc->blgn", uf, bb_im)
    a_re = jnp.broadcast_to(ab_re[None, None], (1, length, S5_GROUPS, S5_STATE))
    a_im = jnp.broadcast_to(ab_im[None, None], (1, length, S5_GROUPS, S5_STATE))

    def combine(e1, e2):
        a1r, a1i, b1r, b1i = e1
        a2r, a2i, b2r, b2i = e2
        return (a2r * a1r - a2i * a1i,
                a2r * a1i + a2i * a1r,
                a2r * b1r - a2i * b1i + b2r,
                a2r * b1i + a2i * b1r + b2i)

    _, _, s_re, s_im = lax.associative_scan(combine, (a_re, a_im, x_re, x_im), axis=1)
    y = (jnp.einsum("blgn,gcn->blgc", s_re, c_re.astype(f32))
         - jnp.einsum("blgn,gcn->blgc", s_im, c_im.astype(f32)))
    y = y + uf * d_skip.astype(f32).reshape(S5_GROUPS, S5_GROUP)
    g = jax.nn.gelu(y.reshape(bsz, length, S5_WIDTH))
    out = g * jax.nn.sigmoid(g @ w_glu.astype(f32) + b_glu.astype(f32))
    return out.astype(u.dtype)


def conformer_conv(v, g, conv_w, conv_b, ln_g, ln_b):
    h = v * jax.nn.sigmoid(g)
    h = lax.conv_general_dilated(
        h, conv_w[:, None, :], window_strides=(1,), padding=[(CONV_KERNEL - 1, 0)],
        dimension_numbers=("NWC", "WIO", "NWC"), feature_group_count=CONV_WIDTH) + conv_b
    hf = h.astype(jnp.float32)
    mu = jnp.mean(hf, axis=-1, keepdims=True)
    var = jnp.mean(jnp.square(hf - mu), axis=-1, keepdims=True)
    hf = (hf - mu) * lax.rsqrt(var + NORM_EPS) * ln_g.astype(jnp.float32) + ln_b.astype(jnp.float32)
    return jax.nn.silu(hf).astype(v.dtype)


def even_layer(h, w_in, lam_re, lam_im, log_step, b_re, b_im, c_re, c_im, d_skip, w_glu, b_glu,
               conv_w, conv_b, ln_g, ln_b, w_out):
    proj = h @ w_in
    u_a = proj[..., :S5_WIDTH]
    z_a = proj[..., S5_WIDTH:2 * S5_WIDTH]
    o = 2 * S5_WIDTH
    v_b = proj[..., o:o + CONV_WIDTH]
    g_b = proj[..., o + CONV_WIDTH:o + 2 * CONV_WIDTH]
    z_b = proj[..., o + 2 * CONV_WIDTH:]
    y_a = s5_mixer(u_a, lam_re, lam_im, log_step, b_re, b_im, c_re, c_im, d_skip, w_glu, b_glu)
    y_b = conformer_conv(v_b, g_b, conv_w, conv_b, ln_g, ln_b)
    y = jnp.concatenate([y_a * jax.nn.silu(z_a), y_b * jax.nn.silu(z_b)], axis=-1)
    return y @ w_out


def odd_layer(h, w_in, w_out):
    bsz, length, _ = h.shape
    f32 = jnp.float32
    proj = h @ w_in
    sizes = [ATT_WIDTH, ATT_WIDTH, KV_WIDTH, KV_WIDTH, IDX_HEADS * IDX_DIM, IDX_DIM]
    offs = np.cumsum([0] + sizes).tolist()
    q, z, k, v, qi, ki = [proj[..., offs[i]:offs[i + 1]] for i in range(len(sizes))]
    wi = proj[..., offs[-1]:]
    cos, sin = rope_tables(length)
    q = partial_rope(q.reshape(bsz, length, N_KV_HEADS, Q_PER_KV, HEAD_DIM), cos, sin)
    k = partial_rope(k.reshape(bsz, length, N_KV_HEADS, HEAD_DIM), cos, sin)
    v = v.reshape(bsz, length, N_KV_HEADS, HEAD_DIM)
    qi = partial_rope(qi.reshape(bsz, length, IDX_HEADS, IDX_DIM), cos, sin).astype(f32)
    ki = partial_rope(ki, cos, sin).astype(f32)
    wi = wi.astype(f32) * (IDX_HEADS ** -0.5) * (IDX_DIM ** -0.5)
    n_blk = length // CHUNK
    ksel = min(TOPK_MAX, length // 4)
    key_chunk = jnp.arange(length) // CHUNK
    scale = HEAD_DIM ** -0.5

    def block(c):
        s0 = c * CHUNK
        qb = lax.dynamic_slice_in_dim(q, s0, CHUNK, axis=1)
        qib = lax.dynamic_slice_in_dim(qi, s0, CHUNK, axis=1)
        wib = lax.dynamic_slice_in_dim(wi, s0, CHUNK, axis=1)
        isc = jax.nn.relu(jnp.einsum("bqhd,bsd->bqhs", qib, ki))
        score = jnp.einsum("bqh,bqhs->bqs", wib, isc)
        score = jnp.where((key_chunk <= c)[None, None, :], score, -jnp.inf)
        _, idx = lax.top_k(score, ksel)
        valid = (idx // CHUNK) <= c
        kg = jax.vmap(lambda kk, ii: kk[ii])(k, idx)
        vg = jax.vmap(lambda vv, ii: vv[ii])(v, idx)
        logits = jnp.einsum("bqhgd,bqkhd->bqhgk", qb.astype(f32), kg.astype(f32)) * scale
        logits = jnp.where(valid[:, :, None, None, :], logits, -jnp.inf)
        p = jax.nn.softmax(logits, axis=-1).astype(v.dtype)
        ob = jnp.einsum("bqhgk,bqkhd->bqhgd", p, vg)
        return ob.reshape(bsz, CHUNK, ATT_WIDTH)

    out = lax.map(block, jnp.arange(n_blk))
    out = out.transpose(1, 0, 2, 3).reshape(bsz, length, ATT_WIDTH)
    return (out * jax.nn.silu(z)) @ w_out


def setup_inputs(seed: int = 0) -> dict:
    key = jax.random.key(seed)
    ks = jax.random.split(key, 24)
    f32 = jnp.float32
    nrm = lambda k, shape, s: jax.random.normal(k, shape, f32) * s
    G, N, C = S5_GROUPS, S5_STATE, S5_GROUP
    n_idx = jnp.arange(N, dtype=f32)
    return {
        "x": nrm(ks[0], (BATCH, SEQ, D_MODEL), 1.0),
        "norm_g": 1.0 + nrm(ks[1], (DEPTH, D_MODEL), 0.02),
        "e_w_in": nrm(ks[2], (N_EVEN, D_MODEL, EVEN_IN), D_MODEL ** -0.5),
        "e_lam_re": -0.5 + nrm(ks[3], (N_EVEN, G, N), 0.01),
        "e_lam_im": jnp.pi * n_idx[None, None, :] + nrm(ks[4], (N_EVEN, G, N), 0.01),
        "e_log_step": jax.random.uniform(ks[5], (N_EVEN, G), f32, math.log(1e-3), math.log(1e-1)),
        "e_b_re": nrm(ks[6], (N_EVEN, G, N, C), (2 * C) ** -0.5),
        "e_b_im": nrm(ks[7], (N_EVEN, G, N, C), (2 * C) ** -0.5),
        "e_c_re": nrm(ks[8], (N_EVEN, G, C, N), N ** -0.5),
        "e_c_im": nrm(ks[9], (N_EVEN, G, C, N), N ** -0.5),
        "e_d_skip": nrm(ks[10], (N_EVEN, S5_WIDTH), 1.0),
        "e_w_glu": nrm(ks[11], (N_EVEN, S5_WIDTH, S5_WIDTH), S5_WIDTH ** -0.5),
        "e_b_glu": nrm(ks[12], (N_EVEN, S5_WIDTH), 0.01),
        "e_conv_w": nrm(ks[13], (N_EVEN, CONV_KERNEL, CONV_WIDTH), CONV_KERNEL ** -0.5),
        "e_conv_b": nrm(ks[14], (N_EVEN, CONV_WIDTH), 0.01),
        "e_ln_g": 1.0 + nrm(ks[15], (N_EVEN, CONV_WIDTH), 0.02),
        "e_ln_b": nrm(ks[16], (N_EVEN, CONV_WIDTH), 0.01),
        "e_w_out": nrm(ks[17], (N_EVEN, S5_WIDTH + CONV_WIDTH, D_MODEL), (S5_WIDTH + CONV_WIDTH) ** -0.5),
        "o_w_in": nrm(ks[18], (N_ODD, D_MODEL, ODD_IN), D_MODEL ** -0.5),
        "o_w_out": nrm(ks[19], (N_ODD, ATT_WIDTH, D_MODEL), ATT_WIDTH ** -0.5),
        "final_g": 1.0 + nrm(ks[20], (D_MODEL,), 0.02),
    }


def reference(x, norm_g, e_w_in, e_lam_re, e_lam_im, e_log_step, e_b_re, e_b_im, e_c_re, e_c_im,
              e_d_skip, e_w_glu, e_b_glu, e_conv_w, e_conv_b, e_ln_g, e_ln_b, e_w_out,
              o_w_in, o_w_out, final_g):
    h = x
    for layer in range(DEPTH):
        hn = rms_norm(h, norm_g[layer])
        if layer % 2 == 0:
            j = layer // 2
            y = even_layer(hn, e_w_in[j], e_lam_re[j], e_lam_im[j], e_log_step[j], e_b_re[j], e_b_im[j],
                           e_c_re[j], e_c_im[j], e_d_skip[j], e_w_glu[j], e_b_glu[j], e_conv_w[j],
                           e_conv_b[j], e_ln_g[j], e_ln_b[j], e_w_out[j])
        else:
            j = layer // 2
            y = odd_layer(hn, o_w_in[j], o_w_out[j])
        h = h + y.astype(h.dtype)
    return rms_norm(h, final_g)
```

```python
import math
from contextlib import ExitStack

import numpy as np
import concourse.bass as bass
import concourse.mybir as mybir
from concourse.bass_utils import run_bass_kernel_spmd

F32 = mybir.dt.float32
BF16 = mybir.dt.bfloat16
ALU = mybir.AluOpType
AF = mybir.ActivationFunctionType

L = 4096
D = 1024
NT = L // 128
NB = L // 512
EPS = 1e-6
E_IN = 2560
O_IN = 3144

ENGS = ["pe", "act", "dve", "pool", "sp"]
EPOCH_LEN = 3000


class Buf:
    __slots__ = ("lw", "rd", "name")

    def __init__(self, name=""):
        self.lw = None
        self.rd = {}
        self.name = name


class PBuf(Buf):
    __slots__ = ()


class Prog:
    def __init__(self, nc, es, n_dma_sems=24):
        self.nc = nc
        self.es = es
        self.sem = {}
        self.epoch = {e: 0 for e in ENGS}
        self.cnt = {e: 0 for e in ENGS}
        for e in ENGS:
            self.sem[e + "#0"] = es.enter_context(nc.semaphore("s_%s_0" % e))
        self.ops = {e: [] for e in ENGS}
        self.known = {e: {} for e in ENGS}
        self.pending = {e: {} for e in ENGS}
        self.dsem = [es.enter_context(nc.semaphore("d%d" % i)) for i in range(n_dma_sems)]
        self.dval = [0] * n_dma_sems
        self.dnext = 0

    def semh(self, key):
        if isinstance(key, tuple):
            return self.dsem[key[1]]
        return self.sem[key]

    def op(self, E, fn, r=(), w=(), dma=False):
        waits = {}
        known = self.known[E]

        def need(k, v):
            if E == "pe" and isinstance(k, str) and k.startswith("pe#"):
                return
            if known.get(k, 0) >= v:
                return
            if waits.get(k, 0) < v:
                waits[k] = v

        for k, v in self.pending[E].items():
            need(k, v)
        self.pending[E] = {}
        for b in r:
            if b.lw is not None:
                need(*b.lw)
            if isinstance(b, PBuf):
                for k, v in b.rd.items():
                    if not (isinstance(k, str) and k.split("#")[0] == E):
                        need(k, v)
        for b in w:
            if b.lw is not None:
                need(*b.lw)
            for k, v in b.rd.items():
                need(k, v)
        if dma:
            i = self.dnext
            self.dnext = (i + 1) % len(self.dsem)
            if self.dval[i] > 0:
                need(("d", i), self.dval[i])
            self.dval[i] += 16
            tok = (("d", i), self.dval[i])
            inc = 16
        else:
            if self.cnt[E] >= EPOCH_LEN:
                self.epoch[E] += 1
                self.cnt[E] = 0
                self.sem["%s#%d" % (E, self.epoch[E])] = self.es.enter_context(
                    self.nc.semaphore("s_%s_%d" % (E, self.epoch[E])))
            self.cnt[E] += 1
            tok = ("%s#%d" % (E, self.epoch[E]), self.cnt[E])
            inc = 1
        for k, v in waits.items():
            known[k] = v
        self.ops[E].append((list(waits.items()), fn, tok[0], inc))
        for b in r:
            if b.rd.get(tok[0], 0) < tok[1]:
                b.rd[tok[0]] = tok[1]
        for b in w:
            b.lw = tok
            b.rd = {}
        return tok

    def barrier(self):
        snap = {"%s#%d" % (e, self.epoch[e]): self.cnt[e] for e in ENGS if self.cnt[e] > 0}
        for i, v in enumerate(self.dval):
            if v > 0:
                snap[("d", i)] = v
        for e in ENGS:
            p = self.pending[e]
            for k, v in snap.items():
                if p.get(k, 0) < v:
                    p[k] = v

    def emit(self):
        self.barrier()
        final = {e: [(k, v) for k, v in self.pending[e].items()
                     if not (e == "pe" and isinstance(k, str) and k.startswith("pe#"))
                     and self.known[e].get(k, 0) < v] for e in ENGS}
        nc = self.nc
        with nc.Block() as block:
            def mk(E):
                def body(eng):
                    for waits, fn, key, inc in self.ops[E]:
                        for k, v in waits:
                            eng.wait_ge(self.semh(k), v)
                        ins = fn(eng)
                        ins.then_inc(self.semh(key), inc)
                    for k, v in final[E]:
                        eng.wait_ge(self.semh(k), v)
                return body
            block.tensor(mk("pe"))
            block.scalar(mk("act"))
            block.vector(mk("dve"))
            block.gpsimd(mk("pool"))
            block.sync(mk("sp"))

    def mm(self, out, lhsT, rhs, start, stop, r=(), w=()):
        return self.op("pe", lambda e: e.matmul(out, lhsT, rhs, start=start, stop=stop), r, w)

    def tr(self, out, in_, ident, r=(), w=()):
        return self.op("pe", lambda e: e.transpose(out, in_, ident), r, w)

    def act(self, out, in_, func, bias=None, scale=None, accum=None, r=(), w=()):
        kw = {}
        if bias is not None:
            kw["bias"] = bias
        if scale is not None:
            kw["scale"] = scale
        if accum is not None:
            kw["accum_out"] = accum
        return self.op("act", lambda e: e.activation(out, in_, func, **kw), r, w)

    def ts(self, E, out, in0, s1, s2, op0, op1=None, accum=None, r=(), w=()):
        kw = {}
        if op1 is not None:
            kw["op1"] = op1
        if accum is not None:
            kw["accum_out"] = accum
        return self.op(E, lambda e: e.tensor_scalar(out, in0, s1, s2, op0, **kw), r, w)

    def tt(self, E, out, in0, in1, op, r=(), w=()):
        return self.op(E, lambda e: e.tensor_tensor(out, in0, in1, op), r, w)

    def stt(self, out, in0, scalar, in1, op0, op1, r=(), w=()):
        return self.op("dve", lambda e: e.scalar_tensor_tensor(out, in0, scalar, in1, op0, op1), r, w)

    def cp(self, E, out, in_, r=(), w=()):
        if E == "act":
            return self.op("act", lambda e: e.copy(out, in_), r, w)
        return self.op(E, lambda e: e.tensor_copy(out, in_), r, w)

    def memset(self, E, ap, val, w=()):
        return self.op(E, lambda e: e.memset(ap, val), (), w)

    def dma(self, E, out, in_, r=(), w=(), slow=False):
        if slow:
            return self.op(E, lambda e: e.dma_start(out=out, in_=in_, allow_slow_non_contiguous=True), r, w, dma=True)
        return self.op(E, lambda e: e.dma_start(out=out, in_=in_), r, w, dma=True)


class Ctx:
    pass


def sb(nc, es, name, shape, dt):
    return es.enter_context(nc.sbuf_tensor(name, shape, dt))


def ps(nc, es, name, shape, dt):
    return es.enter_context(nc.psum_tensor(name, shape, dt))


def build_consts(C):
    nc, P, es = C.nc, C.P, C.es
    C.ident_f = sb(nc, es, "ident_f", [128, 128], F32)
    C.ident_b = sb(nc, es, "ident_b", [128, 128], BF16)
    C.b_ident = Buf("ident")
    P.memset("pool", C.ident_f[:], 0.0, w=[C.b_ident])
    P.op("pool", lambda e: e.affine_select(out=C.ident_f[:], in_=C.ident_f[:], pattern=[[-1, 128]],
                                           compare_op=ALU.not_equal, fill=1.0, base=0,
                                           channel_multiplier=1), [C.b_ident], [C.b_ident])
    P.cp("pool", C.ident_b[:], C.ident_f[:], r=[C.b_ident], w=[C.b_ident])
    C.eps_t = sb(nc, es, "eps_t", [128, 1], F32)
    C.b_eps = Buf("eps")
    P.memset("pool", C.eps_t[:], EPS, w=[C.b_eps])


def rms_tiles_to_hnT(C, src_ap_fn, hnT, hnT_bufs, blk, st):
    nc, P = C.nc, C.P
    for i in range(4):
        tt = blk * 4 + i
        s = st["xs_i"] % len(st["xs"])
        st["xs_i"] += 1
        xt, bx = st["xs"][s]
        P.dma("sp", xt[:], src_ap_fn(tt), w=[bx])
        ss, bss = st["ss"][s]
        junk, bj = st["junk"]
        P.act(junk[:], xt[:], AF.Square, accum=ss[:], r=[bx], w=[bj, bss])
        P.act(ss[:], ss[:], AF.Sqrt, bias=C.eps_t[:], scale=1.0 / D, r=[bss, C.b_eps], w=[bss])
        P.op("dve", lambda e, ss=ss: e.reciprocal(ss[:], ss[:]), [bss], [bss])
        xn, bxn = st["xn"][s % len(st["xn"])]
        P.ts("dve", xn[:], xt[:], ss[:], None, ALU.mult, r=[bx, bss], w=[bxn])
        pt, bpt = st["pst"][st["pst_i"] % len(st["pst"])]
        st["pst_i"] += 1
        for k in range(8):
            P.tr(pt[:, k, :], xn[:, k * 128:(k + 1) * 128], C.ident_b[:], r=[bxn, C.b_ident], w=[bpt])
        eng = "act" if (i % 2 == 0) else "dve"
        P.cp(eng, hnT[:, :, i * 128:(i + 1) * 128], pt[:], r=[bpt], w=[hnT_bufs])


def load_weight_bf16(C, wdst, bw, wsrc, ncols, g_t, bg, stage, col0=0, colmap=None):
    P = C.P
    for k in range(8):
        stg, bs = stage[k % len(stage)]
        P.dma("sp", stg[:, 0:ncols], wsrc[k * 128:(k + 1) * 128, :], w=[bs])
        eng = "dve" if k % 2 == 0 else "pool"
        P.ts(eng, wdst[:, k, col0:col0 + ncols], stg[:, 0:ncols], g_t[:, k:k + 1], None, ALU.mult,
             r=[bs, bg], w=[bw])


PW = [1] + [16 * (2 ** i) for i in range(8)]
PI = math.pi
CW1 = 6.28125
CW2 = 2 * math.pi - 6.28125


def s5_setup(C, ins, es):
    nc, P = C.nc, C.P
    S = Ctx()
    bS = Buf("s5setup")
    S.b = bS

    def t(name, shape, dt=F32):
        return sb(nc, es, "s5_" + name, shape, dt)

    lr, li, dt_ = t("lr", [128, 32]), t("li", [128, 32]), t("dt", [128, 32])
    for half in range(2):
        P.dma("sp", lr[half * 64:(half + 1) * 64, :], ins["e_lam_re"][0].rearrange("g n -> n g"), w=[bS], slow=True)
        P.dma("sp", li[half * 64:(half + 1) * 64, :], ins["e_lam_im"][0].rearrange("g n -> n g"), w=[bS], slow=True)
    P.dma("sp", dt_[:], ins["e_log_step"][0].partition_broadcast(128), w=[bS])
    bre, bim = t("bre", [128, 32, 16]), t("bim", [128, 32, 16])
    for half in range(2):
        P.dma("sp", bre[half * 64:(half + 1) * 64], ins["e_b_re"][0].rearrange("g n c -> n g c"), w=[bS])
        P.dma("sp", bim[half * 64:(half + 1) * 64], ins["e_b_im"][0].rearrange("g n c -> n g c"), w=[bS])
    cnat = t("cnat", [128, 4, 128])
    P.dma("sp", cnat[:, :, 0:64], ins["e_c_re"][0].rearrange("(t q) c n -> (q c) t n", q=8), w=[bS])
    P.dma("sp", cnat[:, :, 64:128], ins["e_c_im"][0].rearrange("(t q) c n -> (q c) t n", q=8), w=[bS])
    S.dskip = t("dskip", [128, 4])
    P.dma("sp", S.dskip[:], ins["e_d_skip"][0].rearrange("(t p) -> p t", p=128), w=[bS], slow=True)

    pidx_i = t("pidx_i", [128, 1], mybir.dt.int32)
    pidx = t("pidx", [128, 1])
    P.op("pool", lambda e: e.iota(pidx_i[:], pattern=[[0, 1]], base=0, channel_multiplier=1), (), [bS])
    P.cp("dve", pidx[:], pidx_i[:], r=[bS], w=[bS])
    mlo, mhi, sgn = t("mlo", [128, 1]), t("mhi", [128, 1]), t("sgn", [128, 1])
    P.ts("dve", mlo[:], pidx[:], 64.0, None, ALU.is_lt, r=[bS], w=[bS])
    P.ts("dve", mhi[:], mlo[:], -1.0, 1.0, ALU.mult, ALU.add, r=[bS], w=[bS])
    P.ts("dve", sgn[:], mlo[:], 2.0, -1.0, ALU.mult, ALU.add, r=[bS], w=[bS])
    mpar = t("mpar", [128, 4])
    pm64 = t("pm64", [128, 1])
    mtmp = t("mtmp", [128, 1])
    P.stt(pm64[:], mhi[:], -64.0, pidx[:], ALU.mult, ALU.add, r=[bS], w=[bS])
    for r4 in range(4):
        P.ts("dve", mtmp[:], pm64[:], 16.0 * r4 - 0.5, None, ALU.is_gt, r=[bS], w=[bS])
        P.ts("dve", mpar[:, r4:r4 + 1], pm64[:], 16.0 * r4 + 15.5, mtmp[:], ALU.is_lt, ALU.mult, r=[bS], w=[bS])
    negpi = t("negpi", [128, 1])
    P.memset("dve", negpi[:], -PI, w=[bS])
    S.sgn = sgn

    P.ts("dve", lr[:], lr[:], -1e-4, None, ALU.min, r=[bS], w=[bS])
    P.act(dt_[:], dt_[:], AF.Exp, r=[bS], w=[bS])
    lrdt, lidt = t("lrdt", [128, 32]), t("lidt", [128, 32])
    P.tt("dve", lrdt[:], lr[:], dt_[:], ALU.mult, r=[bS], w=[bS])
    P.tt("dve", lidt[:], li[:], dt_[:], ALU.mult, r=[bS], w=[bS])
    npw = len(PW)
    S.vr, S.vi = t("vr", [128, npw, 32]), t("vi", [128, npw, 32])
    mag, sn, cs, ph = t("mag", [128, 32]), t("sn", [128, 32]), t("cs", [128, 32]), t("ph", [128, 32])
    ai1 = t("ai1", [128, 32])
    ki_ = t("ki_", [128, 32], mybir.dt.int32)
    kf = t("kf", [128, 32])
    for ip, p in enumerate(PW):
        P.act(mag[:], lrdt[:], AF.Exp, scale=float(p), r=[bS], w=[bS])
        for dst, off in ((sn, 0.0), (cs, 0.5 * PI)):
            P.ts("dve", ph[:], lidt[:], float(p), off, ALU.mult, ALU.add, r=[bS], w=[bS])
            P.ts("dve", ki_[:], ph[:], 1.0 / (2 * PI), None, ALU.mult, r=[bS], w=[bS])
            P.cp("dve", kf[:], ki_[:], r=[bS], w=[bS])
            P.stt(ph[:], kf[:], -CW1, ph[:], ALU.mult, ALU.add, r=[bS], w=[bS])
            P.stt(ph[:], kf[:], -CW2, ph[:], ALU.mult, ALU.add, r=[bS], w=[bS])
            P.ts("dve", kf[:], ph[:], PI, None, ALU.is_gt, r=[bS], w=[bS])
            P.stt(ph[:], kf[:], -2 * PI, ph[:], ALU.mult, ALU.add, r=[bS], w=[bS])
            P.ts("dve", ph[:], ph[:], -PI, PI, ALU.max, ALU.min, r=[bS], w=[bS])
            P.act(dst[:], ph[:], AF.Sin, r=[bS], w=[bS])
        P.tt("dve", S.vr[:, ip, :], mag[:], cs[:], ALU.mult, r=[bS], w=[bS])
        if ip == 0:
            P.tt("dve", ai1[:], mag[:], sn[:], ALU.mult, r=[bS], w=[bS])
        P.stt(S.vi[:, ip, :], sn[:], sgn[:], mag[:], ALU.mult, ALU.mult, r=[bS], w=[bS])
    nr, den, kr, ki, tmp = t("nr", [128, 32]), t("den", [128, 32]), t("kr", [128, 32]), t("ki", [128, 32]), t("tmp", [128, 32])
    P.ts("dve", nr[:], S.vr[:, 0, :], -1.0, None, ALU.add, r=[bS], w=[bS])
    P.tt("dve", den[:], lr[:], lr[:], ALU.mult, r=[bS], w=[bS])
    P.tt("dve", tmp[:], li[:], li[:], ALU.mult, r=[bS], w=[bS])
    P.tt("dve", den[:], den[:], tmp[:], ALU.add, r=[bS], w=[bS])
    P.op("dve", lambda e: e.reciprocal(den[:], den[:]), [bS], [bS])
    P.tt("dve", kr[:], nr[:], lr[:], ALU.mult, r=[bS], w=[bS])
    P.tt("dve", tmp[:], ai1[:], li[:], ALU.mult, r=[bS], w=[bS])
    P.tt("dve", kr[:], kr[:], tmp[:], ALU.add, r=[bS], w=[bS])
    P.tt("dve", kr[:], kr[:], den[:], ALU.mult, r=[bS], w=[bS])
    P.tt("dve", ki[:], ai1[:], lr[:], ALU.mult, r=[bS], w=[bS])
    P.tt("dve", tmp[:], nr[:], li[:], ALU.mult, r=[bS], w=[bS])
    P.tt("dve", ki[:], ki[:], tmp[:], ALU.subtract, r=[bS], w=[bS])
    P.tt("dve", ki[:], ki[:], den[:], ALU.mult, r=[bS], w=[bS])
    KA, KB = t("KA", [128, 32]), t("KB", [128, 32])
    P.ts("dve", KA[:], kr[:], mlo[:], None, ALU.mult, r=[bS], w=[bS])
    P.stt(KA[:], ki[:], mhi[:], KA[:], ALU.mult, ALU.add, r=[bS], w=[bS])
    P.ts("dve", KB[:], kr[:], mhi[:], None, ALU.mult, r=[bS], w=[bS])
    P.ts("dve", tmp[:], ki[:], mlo[:], None, ALU.mult, r=[bS], w=[bS])
    P.tt("dve", KB[:], KB[:], tmp[:], ALU.subtract, r=[bS], w=[bS])
    bbar, btmp = t("bbar", [128, 32, 16]), t("btmp", [128, 32, 16])
    P.tt("dve", bbar[:], bre[:], KA[:].unsqueeze(2).broadcast_to([128, 32, 16]), ALU.mult, r=[bS], w=[bS])
    P.tt("dve", btmp[:], bim[:], KB[:].unsqueeze(2).broadcast_to([128, 32, 16]), ALU.mult, r=[bS], w=[bS])
    P.tt("dve", bbar[:], bbar[:], btmp[:], ALU.add, r=[bS], w=[bS])
    S.BT = t("BT", [128, 4, 4, 128], BF16)
    S.Cpad = t("Cpad", [128, 32, 128], BF16)
    P.memset("pool", S.Cpad[:], 0.0, w=[bS])
    pst = ps(nc, es, "s5_pst", [128, 128], F32)
    CSt = t("CSt", [128, 128])
    for T in range(4):
        P.tr(pst[:], bbar[:, 8 * T:8 * T + 8, :].rearrange("p g c -> p (g c)"), C.ident_f[:], r=[bS, C.b_ident], w=[bS])
        for par in range(4):
            P.ts("dve", S.BT[:, T, par, :], pst[:], mpar[:, par:par + 1], None, ALU.mult, r=[bS], w=[bS])
        P.tr(pst[:], cnat[:, T, :], C.ident_f[:], r=[bS, C.b_ident], w=[bS])
        P.ts("dve", CSt[:], pst[:], sgn[:], None, ALU.mult, r=[bS], w=[bS])
        for q in range(8):
            P.cp("dve", S.Cpad[:, 8 * T + q, 16 * q:16 * q + 16], CSt[:, 16 * q:16 * q + 16], r=[bS], w=[bS])
    S.J = t("J", [128, 128])
    P.cp("dve", S.J[:, 64:128], C.ident_f[:, 0:64], r=[C.b_ident], w=[bS])
    P.cp("dve", S.J[:, 0:64], C.ident_f[:, 64:128], r=[C.b_ident], w=[bS])
    return S


def s5_main(C, S, U, bU, G, bG, es, dbg):
    nc, P = C.nc, C.P
    bS = S.b
    pstep = [(ps(nc, es, "s5_pp%d" % i, [128, 256], F32), PBuf()) for i in range(5)]
    py = [(ps(nc, es, "s5_py%d" % i, [128, 256], F32), PBuf()) for i in range(2)]
    Sst = [[(sb(nc, es, "s5_S%d_%d" % (q, i), [128, 256], BF16), Buf()) for i in range(2)] for q in range(8)]
    Epp = [[(sb(nc, es, "s5_E%d_%d" % (q, i), [128, 256], F32), Buf()) for i in range(2)] for q in range(8)]
    Eprev = [(sb(nc, es, "s5_Ep%d" % q, [128, 256], BF16), Buf()) for q in range(8)]
    A1 = [(sb(nc, es, "s5_A1_%d" % q, [128, 2, 128], BF16), Buf()) for q in range(8)]
    Af = [(sb(nc, es, "s5_Af%d" % i, [128, 128], F32), Buf()) for i in range(4)]
    Atmp = (sb(nc, es, "s5_Atmp", [128, 128], F32), Buf())
    Ahi32 = (sb(nc, es, "s5_Ahi32", [128, 128], F32), Buf())
    yat = [(sb(nc, es, "s5_ya%d" % i, [128, 256], F32), Buf()) for i in range(2)]
    ppi = 0
    afi = 0
    yi = 0

    def gen_A(dst, bd, ip, g, eng):
        P.ts(eng, dst[:], C.ident_f[:], S.vr[:, ip, g:g + 1], None, ALU.mult, r=[C.b_ident, bS], w=[bd])
        P.stt(dst[:], S.J[:], S.vi[:, ip, g:g + 1], dst[:], ALU.mult, ALU.add, r=[bS, bd], w=[bd])

    for T in range(4):
        for q in range(8):
            g = 8 * T + q
            a1, ba = A1[q]
            gen_A(Atmp[0], Atmp[1], 0, g, "dve")
            P.cp("dve", a1[:, 0, :], Atmp[0][:], r=[Atmp[1]], w=[ba])
            P.cp("dve", Ahi32[0][:], a1[:, 0, :], r=[ba], w=[Ahi32[1]])
            P.tt("dve", a1[:, 1, :], Atmp[0][:], Ahi32[0][:], ALU.subtract, r=[Atmp[1], Ahi32[1]], w=[ba])
        for pas in range(2):
            for j in range(16):
                for q in range(8):
                    g = 8 * T + q
                    pr = q // 4
                    pp, bp = pstep[ppi % len(pstep)]
                    ppi += 1
                    a1, ba = A1[q]
                    sprev = None
                    if j > 0:
                        sprev = Sst[q][(j - 1) % 2]
                    elif pas == 1:
                        sprev = Eprev[q]
                    P.mm(pp[:], S.BT[64 * pr:64 * pr + 64, T, q % 4, :], U[64 * pr:64 * pr + 64, T, j, :],
                         True, sprev is None, r=[bS, bU[T]], w=[bp])
                    if sprev is not None:
                        P.mm(pp[:], a1[:, 0, :], sprev[0][:], False, False, r=[ba, sprev[1]], w=[bp])
                        P.mm(pp[:], a1[:, 1, :], sprev[0][:], False, True, r=[ba, sprev[1]], w=[bp])
                    sc, bs = Sst[q][j % 2]
                    if pas == 0 and j == 15:
                        P.cp("act", Epp[q][0][0][:], pp[:], r=[bp], w=[Epp[q][0][1]])
                    else:
                        eng = "act" if (q % 2 == 0) else "dve"
                        P.cp(eng, sc[:], pp[:], r=[bp], w=[bs])
                if pas == 1:
                    pyt, bpy = py[yi % 2]
                    ya, bya = yat[yi % 2]
                    yi += 1
                    for q in range(8):
                        sc, bs = Sst[q][j % 2]
                        P.mm(pyt[:], S.Cpad[:, 8 * T + q, :], sc[:], q == 0, q == 7, r=[bS, bs], w=[bpy])
                    P.stt(ya[:], U[:, T, j, :], S.dskip[:, T:T + 1], pyt[:], ALU.mult, ALU.add,
                          r=[bU[T], bS, bpy], w=[bya])
                    P.act(G[:, T, j, :], ya[:], AF.Gelu, r=[bya], w=[bG[T]])
                    if dbg is not None and dbg.get("stage") == "S5" and T == 0:
                        P.dma("sp", dbg["out32"][0:128, j * 256:(j + 1) * 256], ya[:], r=[bya])
            if pas == 0:
                for q in range(8):
                    g = 8 * T + q
                    cur = 0
                    for i in range(8):
                        d = 2 ** i
                        am, bam = Af[afi % len(Af)]
                        afi += 1
                        gen_A(am, bam, 1 + i, g, "pool" if False else "dve")
                        Ec, bEc = Epp[q][cur]
                        En, bEn = Epp[q][1 - cur]
                        pp, bp = pstep[ppi % len(pstep)]
                        ppi += 1
                        P.mm(pp[:, 0:256 - d], am[:], Ec[:, 0:256 - d], True, True, r=[bam, bEc], w=[bp])
                        P.tt("dve", En[:, d:256], Ec[:, d:256], pp[:, 0:256 - d], ALU.add, r=[bEc, bp], w=[bEn])
                        P.cp("act", En[:, 0:d], Ec[:, 0:d], r=[bEc], w=[bEn])
                        cur = 1 - cur
                    Ef, bEf = Epp[q][cur]
                    ep, bep = Eprev[q]
                    P.memset("pool", ep[:, 0:1], 0.0, w=[bep])
                    P.cp("act", ep[:, 1:256], Ef[:, 0:255], r=[bEf], w=[bep])


def load_cast(C, wdst, bw, wsrc_fn, nk, ncols, stage):
    P = C.P
    for k in range(nk):
        stg, bs = stage[k % len(stage)]
        P.dma("sp", stg[:, 0:ncols], wsrc_fn(k), w=[bs])
        P.cp("dve" if k % 2 == 0 else "pool", wdst[:, k, 0:ncols], stg[:, 0:ncols], r=[bs], w=[bw])


def glu_stage(C, ins, G, bG, za_d, ycat_d, es):
    nc, P = C.nc, C.P
    wg = sb(nc, es, "wglu", [128, 4, 512], BF16)
    bwg = Buf()
    stage = [(sb(nc, es, "gst%d" % i, [128, 512], F32), Buf()) for i in range(2)]
    load_cast(C, wg, bwg, lambda k: ins["e_w_glu"][0][k * 128:(k + 1) * 128, :], 4, 512, stage)
    bgl = sb(nc, es, "bglu", [128, 4], F32)
    bb = Buf()
    P.dma("sp", bgl[:], ins["e_b_glu"][0].rearrange("(t p) -> p t", p=128), w=[bb], slow=True)
    YAn = sb(nc, es, "YAn", [128, 4, L], BF16)
    bY = [Buf() for _ in range(4)]
    pg = [(ps(nc, es, "pglu%d" % i, [128, 512], F32), PBuf()) for i in range(3)]
    sg = [(sb(nc, es, "gsig%d" % i, [128, 512], F32), Buf()) for i in range(2)]
    zt = [(sb(nc, es, "gza%d" % i, [128, 512], BF16), Buf()) for i in range(3)]
    tm = [(sb(nc, es, "gtm%d" % i, [128, 512], F32), Buf()) for i in range(2)]
    n = 0
    for b in range(8):
        for T in range(4):
            p, bp = pg[n % 3]
            s, bs = sg[n % 2]
            z, bz = zt[n % 3]
            t, bt = tm[n % 2]
            n += 1
            P.dma("sp", z[:], za_d[T][:, b * 512:(b + 1) * 512], w=[bz])
            for kc in range(4):
                P.mm(p[:], wg[:, kc, T * 128:(T + 1) * 128],
                     G[:, kc, 2 * b:2 * b + 2, :].rearrange("p j m -> p (j m)"), kc == 0, kc == 3,
                     r=[bwg, bG[kc]], w=[bp])
            P.act(s[:], p[:], AF.Sigmoid, bias=bgl[:, T:T + 1], r=[bp, bb], w=[bs])
            P.tt("dve", t[:], G[:, T, 2 * b:2 * b + 2, :].rearrange("p j m -> p (j m)"), s[:], ALU.mult,
                 r=[bG[T], bs], w=[bt])
            P.tt("pool", YAn[:, T].rearrange("p (m j) -> p j m", j=16)[:, 2 * b:2 * b + 2, :],
                 t[:].rearrange("p (j m) -> p j m", j=2), z[:].rearrange("p (j m) -> p j m", j=2), ALU.mult,
                 r=[bt, bz], w=[bY[T]])
    for T in range(4):
        P.dma("sp", ycat_d[T], YAn[:, T], r=[bY[T]])


def conv_stage(C, ins, H, bH, zb_d, ycat_d, es):
    nc, P = C.nc, C.P
    bc = Buf("convsetup")
    cwn = sb(nc, es, "cwn", [31, 512], F32)
    P.dma("sp", cwn[:], ins["e_conv_w"][0], w=[bc])
    cw = sb(nc, es, "cw", [128, 4, 31], F32)
    pcw = ps(nc, es, "pcw", [128, 31], F32)
    for T in range(4):
        P.tr(pcw[:], cwn[0:31, T * 128:(T + 1) * 128], C.ident_f[0:31, 0:31], r=[bc, C.b_ident], w=[bc])
        P.cp("dve", cw[:, T, :], pcw[:], r=[bc], w=[bc])
    vecs = {}
    for nm in ("e_conv_b", "e_ln_g", "e_ln_b"):
        v = sb(nc, es, "cv_" + nm, [128, 4], F32)
        P.dma("sp", v[:], ins[nm][0].rearrange("(t p) -> p t", p=128), w=[bc], slow=True)
        vecs[nm] = v
    Dg = sb(nc, es, "Dg", [128, 4, 31, 128], BF16)
    for T in range(4):
        for k in range(31):
            P.ts("dve" if (k % 2 == 0) else "pool", Dg[:, T, k, :], C.ident_b[:], cw[:, T, k:k + 1], None, ALU.mult,
                 r=[bc, C.b_ident], w=[bc])
    ones = sb(nc, es, "ones_f", [128, 128], F32)
    P.memset("pool", ones[:], 1.0, w=[bc])
    pc = [(ps(nc, es, "pconv%d" % i, [128, 512], F32), PBuf()) for i in range(3)]
    pmu = (ps(nc, es, "pmu", [128, 512], F32), PBuf())
    psq = (ps(nc, es, "psq", [128, 512], F32), PBuf())
    HC = [[(sb(nc, es, "HC%d_%d" % (i, T), [128, 512], F32), Buf()) for T in range(4)] for i in range(2)]
    SQ = [(sb(nc, es, "SQ%d" % i, [128, 512], F32), Buf()) for i in range(2)]
    mu = (sb(nc, es, "c_mu", [128, 512], F32), Buf())
    rs = (sb(nc, es, "c_rs", [128, 512], F32), Buf())
    t1 = [(sb(nc, es, "c_t1%d" % i, [128, 512], F32), Buf()) for i in range(2)]
    zb = [(sb(nc, es, "c_zb%d" % i, [128, 512], BF16), Buf()) for i in range(3)]
    yo = [(sb(nc, es, "c_yo%d" % i, [128, 512], BF16), Buf()) for i in range(3)]
    n = 0
    nq = 0
    for b in range(8):
        hc = HC[b % 2]
        for T in range(4):
            p, bp = pc[n % 3]
            n += 1
            for k in range(31):
                P.mm(p[:], Dg[:, T, k, :], H[:, T, b * 512 + k:b * 512 + k + 512], k == 0, k == 30,
                     r=[bc, bH[T]], w=[bp])
            P.act(hc[T][0][:], p[:], AF.Identity, bias=vecs["e_conv_b"][:, T:T + 1], r=[bp, bc], w=[hc[T][1]])
            sq, bsq = SQ[nq % 2]
            nq += 1
            P.act(sq[:], p[:], AF.Square, bias=vecs["e_conv_b"][:, T:T + 1], r=[bp, bc], w=[bsq])
            P.mm(pmu[0][:], ones[:], hc[T][0][:], T == 0, T == 3, r=[bc, hc[T][1]], w=[pmu[1]])
            P.mm(psq[0][:], ones[:], sq[:], T == 0, T == 3, r=[bc, bsq], w=[psq[1]])
        P.act(mu[0][:], pmu[0][:], AF.Copy, scale=1.0 / 512, r=[pmu[1]], w=[mu[1]])
        P.act(rs[0][:], pmu[0][:], AF.Square, scale=1.0 / 512, r=[pmu[1]], w=[rs[1]])
        P.stt(rs[0][:], psq[0][:], 1.0 / 512, rs[0][:], ALU.mult, ALU.subtract, r=[psq[1], rs[1]], w=[rs[1]])
        P.act(rs[0][:], rs[0][:], AF.Sqrt, bias=C.eps_t[:], r=[rs[1], C.b_eps], w=[rs[1]])
        P.op("dve", lambda e: e.reciprocal(rs[0][:], rs[0][:]), [rs[1]], [rs[1]])
        for T in range(4):
            t, bt = t1[T % 2]
            z, bz = zb[(b * 4 + T) % 3]
            y, by = yo[(b * 4 + T) % 3]
            P.dma("sp", z[:], zb_d[T][:, b * 512:(b + 1) * 512], w=[bz])
            P.tt("dve", t[:], hc[T][0][:], mu[0][:], ALU.subtract, r=[hc[T][1], mu[1]], w=[bt])
            P.tt("pool", t[:], t[:], rs[0][:], ALU.mult, r=[bt, rs[1]], w=[bt])
            P.act(t[:], t[:], AF.Silu, bias=vecs["e_ln_b"][:, T:T + 1], scale=vecs["e_ln_g"][:, T:T + 1],
                  r=[bt, bc], w=[bt])
            P.tt("dve", y[:], t[:], z[:], ALU.mult, r=[bt, bz], w=[by])
            P.dma("sp", ycat_d[4 + T][:, b * 512:(b + 1) * 512], y[:], r=[by])


def outproj_stage(C, w_ap, ycat_d, x_ap, o_ap, es, final_g=None):
    nc, P = C.nc, C.P
    wo = sb(nc, es, "wout", [128, 8, D], BF16)
    bwo = Buf()
    stage = [(sb(nc, es, "ost%d" % i, [128, D], F32), Buf()) for i in range(2)]
    load_cast(C, wo, bwo, lambda k: w_ap[k * 128:(k + 1) * 128, :], 8, D, stage)
    yc = [(sb(nc, es, "o_yc%d" % i, [128, 8, 128], BF16), Buf()) for i in range(3)]
    xs = [(sb(nc, es, "o_xs%d" % i, [128, D], F32), Buf()) for i in range(3)]
    ho = [(sb(nc, es, "o_ho%d" % i, [128, D], F32), Buf()) for i in range(2)]
    po = [(ps(nc, es, "o_ps%d" % i, [128, 512], F32), PBuf()) for i in range(4)]
    if final_g is not None:
        fg = sb(nc, es, "o_fg", [128, D], F32)
        bfg = Buf()
        P.dma("sp", fg[:], final_g.partition_broadcast(128), w=[bfg])
        ss = [(sb(nc, es, "o_ss%d" % i, [128, 1], F32), Buf()) for i in range(2)]
        junk = (sb(nc, es, "o_junk", [128, D], BF16), Buf())
    n = 0
    for tt in range(NT):
        y, by = yc[tt % 3]
        x, bx = xs[tt % 3]
        h, bh = ho[tt % 2]
        P.dma("sp", y[:], ycat_d[:, :, tt * 128:(tt + 1) * 128].rearrange("c p t -> p c t"), w=[by])
        P.dma("sp", x[:], x_ap[tt * 128:(tt + 1) * 128, :], w=[bx])
        for half in range(2):
            p, bp = po[n % 4]
            n += 1
            for c in range(8):
                P.mm(p[:], y[:, c, :], wo[:, c, half * 512:(half + 1) * 512], c == 0, c == 7, r=[by, bwo], w=[bp])
            P.tt("dve", h[:, half * 512:(half + 1) * 512], p[:], x[:, half * 512:(half + 1) * 512], ALU.add,
                 r=[bp, bx], w=[bh])
        if final_g is not None:
            s, bs = ss[tt % 2]
            P.act(junk[0][:], h[:], AF.Square, accum=s[:], r=[bh], w=[junk[1], bs])
            P.act(s[:], s[:], AF.Sqrt, bias=C.eps_t[:], scale=1.0 / D, r=[bs, C.b_eps], w=[bs])
            P.op("dve", lambda e, s=s: e.reciprocal(s[:], s[:]), [bs], [bs])
            P.stt(h[:], h[:], s[:], fg[:], ALU.mult, ALU.mult, r=[bh, bs, bfg], w=[bh])
        P.dma("sp", o_ap[tt * 128:(tt + 1) * 128, :], h[:], r=[bh])


def layer0(C, x_ap, h1_ap, ins, dbg=None):
    nc, P = C.nc, C.P
    with ExitStack() as es:
        U = sb(nc, es, "U", [128, 4, 16, 256], BF16)
        bU = [Buf("U%d" % t) for t in range(4)]
        H = sb(nc, es, "H", [128, 4, 30 + L], BF16)
        bH = [Buf("H%d" % t) for t in range(4)]
        for t in range(4):
            P.memset("pool", H[:, t, 0:30], 0.0, w=[bH[t]])
        za_d = nc.dram_tensor("za_d", [4, 128, L], BF16, kind="Internal").ap()
        zb_d = nc.dram_tensor("zb_d", [4, 128, L], BF16, kind="Internal").ap()

        with ExitStack() as ea:
            W = sb(nc, ea, "W0", [128, 8, E_IN], BF16)
            bW = Buf("W0")
            g_t = sb(nc, ea, "g0", [128, 8], F32)
            bg = Buf("g0")
            P.dma("sp", g_t[:], ins["norm_g"][0].rearrange("(k p) -> p k", p=128), w=[bg], slow=True)
            stage = [(sb(nc, ea, "wst%d" % i, [128, E_IN], F32), Buf()) for i in range(2)]
            load_weight_bf16(C, W, bW, ins["e_w_in"][0], E_IN, g_t, bg, stage)
            st = {
                "xs": [(sb(nc, ea, "xs%d" % i, [128, D], F32), Buf()) for i in range(3)],
                "ss": [(sb(nc, ea, "ss%d" % i, [128, 1], F32), Buf()) for i in range(3)],
                "xn": [(sb(nc, ea, "xn%d" % i, [128, D], BF16), Buf()) for i in range(3)],
                "junk": (sb(nc, ea, "junk", [128, D], BF16), Buf()),
                "pst": [(ps(nc, ea, "pst%d" % i, [128, 8, 128], BF16), PBuf()) for i in range(2)],
                "xs_i": 0, "pst_i": 0,
            }
            hnTs = [(sb(nc, ea, "hnT%d" % i, [128, 8, 512], BF16), Buf()) for i in range(2)]
            pmm = [(ps(nc, ea, "pmm%d" % i, [128, 512], F32), PBuf()) for i in range(4)]
            sig = [(sb(nc, ea, "sig%d" % i, [128, 512], F32), Buf()) for i in range(2)]
            zst = [(sb(nc, ea, "zst%d" % i, [128, 512], BF16), Buf()) for i in range(4)]
            pi = 0
            zi = 0
            order = [0, 1, 2, 3, 4, 5, 6, 7, 12, 8, 13, 9, 14, 10, 15, 11, 16, 17, 18, 19]
            for blk in range(NB):
                hnT, bh = hnTs[blk % 2]
                rms_tiles_to_hnT(C, lambda tt: x_ap[tt * 128:(tt + 1) * 128, :], hnT, bh, blk, st)
                for ct in order:
                    pm, bp = pmm[pi % 4]
                    pi += 1
                    for k in range(8):
                        P.mm(pm[:], W[:, k, ct * 128:(ct + 1) * 128], hnT[:, k, :], k == 0, k == 7,
                             r=[bW, bh], w=[bp])
                    m0 = blk * 32
                    if ct < 4:
                        P.cp("dve", U[:, ct, :, m0:m0 + 32],
                             pm[:].rearrange("p (m j) -> p j m", j=16), r=[bp], w=[bU[ct]])
                    elif ct < 8:
                        z, bz = zst[zi % 4]
                        zi += 1
                        P.act(z[:].rearrange("p (j m) -> p j m", j=16),
                              pm[:].rearrange("p (m j) -> p j m", j=16), AF.Silu, r=[bp], w=[bz])
                        P.dma("sp", za_d[ct - 4].rearrange("p (j m) -> p j m", j=16)[:, :, m0:m0 + 32],
                              z[:].rearrange("p (j m) -> p j m", j=16), r=[bz])
                    elif ct < 12:
                        sg, bs = sig[(ct - 8) % 2]
                        P.tt("dve", H[:, ct - 8, 30 + blk * 512:30 + (blk + 1) * 512], pm[:], sg[:], ALU.mult,
                             r=[bp, bs], w=[bH[ct - 8]])
                    elif ct < 16:
                        sg, bs = sig[(ct - 12) % 2]
                        P.act(sg[:], pm[:], AF.Sigmoid, r=[bp], w=[bs])
                    else:
                        z, bz = zst[zi % 4]
                        zi += 1
                        P.act(z[:], pm[:], AF.Silu, r=[bp], w=[bz])
                        P.dma("sp", zb_d[ct - 16][:, blk * 512:(blk + 1) * 512], z[:], r=[bz])
            if dbg is not None and dbg.get("stage") == "A":
                P.dma("sp", dbg["out"][0:128, 0:L], U[:, 0].rearrange("p j m -> p (j m)"), r=[bU[0]])
                P.dma("sp", dbg["out"][128:256, 0:L], H[:, 0, 30:30 + L], r=[bH[0]])
            P.barrier()
        if dbg is not None and dbg.get("stage") == "A":
            return

        G = sb(nc, es, "G", [128, 4, 16, 256], BF16)
        bG = [Buf("G%d" % t) for t in range(4)]
        with ExitStack() as e5:
            S5 = s5_setup(C, ins, e5)
            s5_main(C, S5, U, bU, G, bG, e5, dbg)
            P.barrier()
        if dbg is not None and dbg.get("stage") == "S5":
            return
        ycat_d = nc.dram_tensor("ycat_d", [8, 128, L], BF16, kind="Internal").ap()
        with ExitStack() as eg:
            glu_stage(C, ins, G, bG, za_d, ycat_d, eg)
            P.barrier()
        with ExitStack() as ec:
            conv_stage(C, ins, H, bH, zb_d, ycat_d, ec)
            P.barrier()
        with ExitStack() as eo:
            outproj_stage(C, ins["e_w_out"][0], ycat_d, x_ap, h1_ap, eo)
            P.barrier()
        if dbg is not None and dbg.get("stage") == "L0":
            return


NIT = 18
cQ, cZ, cK, cQI, cKI, cV, cWI, W1C = 0, 1024, 2048, 2560, 3072, 3200, 3456, 3464
AX = mybir.AxisListType


def rope_tables(C, es, cosT, sinT, bT):
    nc, P = C.nc, C.P
    b = Buf("ropesetup")
    I32 = mybir.dt.int32

    def t(name, shape, dt=F32):
        return sb(nc, es, "rp_" + name, shape, dt)

    pidx_i, tmp_i = t("pidx_i", [128, 1], I32), t("tmp_i", [128, 1], I32)
    P.op("pool", lambda e: e.iota(pidx_i[:], pattern=[[0, 1]], base=0, channel_multiplier=1), (), [b])
    invp, m16, sg, f = t("invp", [128, 1]), t("m16", [128, 1]), t("sg", [128, 1]), t("f", [128, 1])
    P.ts("dve", tmp_i[:], pidx_i[:], 7, None, ALU.bitwise_and, r=[b], w=[b])
    P.cp("dve", f[:], tmp_i[:], r=[b], w=[b])
    P.act(invp[:], f[:], AF.Exp, scale=-math.log(500000.0) / 8.0, r=[b], w=[b])
    P.ts("dve", tmp_i[:], pidx_i[:], 48, None, ALU.bitwise_and, r=[b], w=[b])
    P.cp("dve", f[:], tmp_i[:], r=[b], w=[b])
    P.ts("dve", m16[:], f[:], 0.5, None, ALU.is_lt, r=[b], w=[b])
    P.tt("dve", invp[:], invp[:], m16[:], ALU.mult, r=[b], w=[b])
    P.ts("dve", tmp_i[:], pidx_i[:], 56, None, ALU.bitwise_and, r=[b], w=[b])
    P.cp("dve", f[:], tmp_i[:], r=[b], w=[b])
    P.ts("dve", sg[:], f[:], 0.5, None, ALU.is_lt, r=[b], w=[b])
    P.ts("dve", sg[:], sg[:], -2.0, 1.0, ALU.mult, ALU.add, r=[b], w=[b])
    CH = 1024
    pos_i, pos, ang, ph, kf = t("pos_i", [128, CH], I32), t("pos", [128, CH]), t("ang", [128, CH]), t("ph", [128, CH]), t("kf", [128, CH])
    ki_ = t("ki", [128, CH], I32)
    for c in range(L // CH):
        P.op("pool", lambda e, c=c: e.iota(pos_i[:], pattern=[[1, CH]], base=c * CH, channel_multiplier=0), (), [b])
        P.cp("dve", pos[:], pos_i[:], r=[b], w=[b])
        P.ts("dve", ang[:], pos[:], invp[:], None, ALU.mult, r=[b], w=[b])
        for dst, off in ((sinT, 0.0), (cosT, 0.5 * PI)):
            P.ts("dve", ph[:], ang[:], off, None, ALU.add, r=[b], w=[b])
            P.ts("dve", ki_[:], ph[:], 1.0 / (2 * PI), None, ALU.mult, r=[b], w=[b])
            P.cp("dve", kf[:], ki_[:], r=[b], w=[b])
            P.stt(ph[:], kf[:], -CW1, ph[:], ALU.mult, ALU.add, r=[b], w=[b])
            P.stt(ph[:], kf[:], -CW2, ph[:], ALU.mult, ALU.add, r=[b], w=[b])
            P.ts("dve", kf[:], ph[:], PI, None, ALU.is_gt, r=[b], w=[b])
            P.stt(ph[:], kf[:], -2 * PI, ph[:], ALU.mult, ALU.add, r=[b], w=[b])
            P.ts("dve", ph[:], ph[:], -PI, PI, ALU.max, ALU.min, r=[b], w=[b])
            P.act(ph[:], ph[:], AF.Sin, r=[b], w=[b])
            if dst is sinT:
                P.ts("dve", dst[:, c * CH:(c + 1) * CH], ph[:], sg[:], None, ALU.mult, r=[b], w=[b, bT])
            else:
                P.cp("dve", dst[:, c * CH:(c + 1) * CH], ph[:], r=[b], w=[b, bT])


def layer1(C, x_ap, o_ap, ins):
    nc, P = C.nc, C.P
    with ExitStack() as es:
        KT2 = sb(nc, es, "KT2", [128, 4, L], BF16)
        bKT = Buf("KT2")
        Vtok = sb(nc, es, "Vtok", [128, NT, 4, 66], BF16)
        bV = Buf("Vtok")
        KI2 = sb(nc, es, "KI2", [128, L], BF16)
        bKI = Buf("KI2")
        WI = sb(nc, es, "WI", [128, NT, 8], F32)
        bWI = Buf("WI")
        P.memset("pool", Vtok[:, :, :, 64:65], 1.0, w=[bV])
        q_d = nc.dram_tensor("q_d", [8, 128, L], BF16, kind="Internal").ap()
        z_d = nc.dram_tensor("z_d", [8, 128, L], BF16, kind="Internal").ap()
        qi_d = nc.dram_tensor("qi_d", [4, 128, L], BF16, kind="Internal").ap()
        w_in = ins["o_w_in"][0]

        with ExitStack() as ea:
            cosT = sb(nc, ea, "cosT", [128, L], BF16)
            sinT = sb(nc, ea, "sinT", [128, L], BF16)
            bT = Buf("ropeT")
            with ExitStack() as er:
                rope_tables(C, er, cosT, sinT, bT)
                P.barrier()
            if getattr(C, "stop", None) == "R":
                return
            Pm = sb(nc, ea, "Pm", [128, 128], BF16)
            bPm = Buf("Pm")
            P.memset("pool", Pm[:], 0.0, w=[bPm])
            for base in (0, 64):
                P.cp("dve", Pm[:, base:base + 8], C.ident_b[:, base + 8:base + 16], r=[C.b_ident], w=[bPm])
                P.cp("dve", Pm[:, base + 8:base + 16], C.ident_b[:, base:base + 8], r=[C.b_ident], w=[bPm])
            W = sb(nc, ea, "W1", [128, 8, W1C], BF16)
            bW = Buf("W1")
            g_t = sb(nc, ea, "g1", [128, 8], F32)
            bg = Buf("g1")
            P.dma("sp", g_t[:], ins["norm_g"][1].rearrange("(k p) -> p k", p=128), w=[bg], slow=True)
            stg = sb(nc, ea, "w1st", [128, W1C], F32)
            bst = Buf("w1st")
            for k in range(8):
                rows = w_in[k * 128:(k + 1) * 128, :]
                P.dma("sp", stg[:, 0:2048], rows[:, 0:2048], w=[bst])
                for a in range(2):
                    P.dma("sp", stg[:, cK:cK + 512].rearrange("p (g two d) -> p g two d", two=2, d=64)[:, :, a, :],
                          rows[:, 2048:2304].rearrange("p (g d) -> p g d", d=64), w=[bst])
                    P.dma("sp", stg[:, cKI + 64 * a:cKI + 64 * a + 64], rows[:, 3072:3136], w=[bst])
                P.dma("sp", stg[:, cQI:cQI + 512], rows[:, 2560:3072], w=[bst])
                P.dma("sp", stg[:, cV:cV + 256], rows[:, 2304:2560], w=[bst])
                P.dma("sp", stg[:, cWI:cWI + 8], rows[:, 3136:3144], w=[bst])
                P.ts("dve", W[:, k, 0:1792], stg[:, 0:1792], g_t[:, k:k + 1], None, ALU.mult, r=[bst, bg], w=[bW])
                P.ts("pool", W[:, k, 1792:W1C], stg[:, 1792:W1C], g_t[:, k:k + 1], None, ALU.mult, r=[bst, bg], w=[bW])
            if getattr(C, "stop", None) == "W":
                return
            st = {
                "xs": [(sb(nc, ea, "b_xs%d" % i, [128, D], F32), Buf()) for i in range(2)],
                "ss": [(sb(nc, ea, "b_ss%d" % i, [128, 1], F32), Buf()) for i in range(2)],
                "xn": [(sb(nc, ea, "b_xn%d" % i, [128, D], BF16), Buf()) for i in range(2)],
                "junk": (sb(nc, ea, "b_junk", [128, D], BF16), Buf()),
                "pst": [(ps(nc, ea, "b_pst%d" % i, [128, 8, 128], BF16), PBuf()) for i in range(2)],
                "xs_i": 0, "pst_i": 0,
            }
            hnTs = [(sb(nc, ea, "b_hnT%d" % i, [128, 8, 512], BF16), Buf()) for i in range(2)]
            pmm = [(ps(nc, ea, "b_pmm%d" % i, [128, 512], F32), PBuf()) for i in range(3)]
            prr = [(ps(nc, ea, "b_prr%d" % i, [128, 512], F32), PBuf()) for i in range(2)]
            pvv = (ps(nc, ea, "b_pvv", [128, 512], F32), PBuf())
            xb = [(sb(nc, ea, "b_xb%d" % i, [128, 512], BF16), Buf()) for i in range(2)]
            ta = [(sb(nc, ea, "b_ta%d" % i, [128, 512], F32), Buf()) for i in range(2)]
            tb = [(sb(nc, ea, "b_tb%d" % i, [128, 512], F32), Buf()) for i in range(2)]
            so = [(sb(nc, ea, "b_so%d" % i, [128, 512], BF16), Buf()) for i in range(4)]
            n_mm = n_r = n_so = 0
            tiles = [("q", cQ + 128 * i, i) for i in range(8)] + [("z", cZ + 128 * i, i) for i in range(8)] + \
                    [("k", cK + 128 * i, i) for i in range(4)] + [("qi", cQI + 128 * i, i) for i in range(4)] + \
                    [("ki", cKI, 0)]
            sub = getattr(C, "stop", None)
            nblk = NB
            do_v = True
            if sub is not None and sub.startswith("B1") and len(sub) > 2:
                nblk = 1
                kinds = {"q": ["q"], "z": ["z"], "k": ["k"], "i": ["ki"], "v": []}[sub[2]]
                tiles = [t_ for t_ in tiles if t_[0] in kinds]
                do_v = sub[2] == "v"
            for blk in range(nblk):
                hnT, bh = hnTs[blk % 2]
                rms_tiles_to_hnT(C, lambda tt: x_ap[tt * 128:(tt + 1) * 128, :], hnT, bh, blk, st)
                sl = slice(blk * 512, (blk + 1) * 512)
                for kind, c0, idx in tiles:
                    pm, bp = pmm[n_mm % 3]
                    n_mm += 1
                    for k in range(8):
                        P.mm(pm[:], W[:, k, c0:c0 + 128], hnT[:, k, :], k == 0, k == 7, r=[bW, bh], w=[bp])
                    if kind == "z":
                        s, bs = so[n_so % 4]
                        n_so += 1
                        P.act(s[:], pm[:], AF.Silu, r=[bp], w=[bs])
                        P.dma("sp", z_d[idx][:, sl], s[:], r=[bs])
                        continue
                    x2, bx2 = xb[n_r % 2]
                    pr, bpr = prr[n_r % 2]
                    a, ba = ta[n_r % 2]
                    b2, bb2 = tb[n_r % 2]
                    n_r += 1
                    P.cp("act", x2[:], pm[:], r=[bp], w=[bx2])
                    P.mm(pr[:], Pm[:], x2[:], True, True, r=[bPm, bx2], w=[bpr])
                    P.tt("dve", a[:], pm[:], cosT[:, sl], ALU.mult, r=[bp, bT], w=[ba])
                    P.tt("dve", b2[:], pr[:], sinT[:, sl], ALU.mult, r=[bpr, bT], w=[bb2])
                    if kind == "k":
                        P.tt("pool", KT2[:, idx, sl], a[:], b2[:], ALU.add, r=[ba, bb2], w=[bKT])
                    elif kind == "ki":
                        P.tt("pool", KI2[:, sl], a[:], b2[:], ALU.add, r=[ba, bb2], w=[bKI])
                    else:
                        s, bs = so[n_so % 4]
                        n_so += 1
                        P.tt("pool", s[:], a[:], b2[:], ALU.add, r=[ba, bb2], w=[bs])
                        dst = q_d if kind == "q" else qi_d
                        P.dma("sp", dst[idx][:, sl], s[:], r=[bs])
                for i in range(4 if do_v else 0):
                    tt_ = blk * 4 + i
                    pv, bpv = pvv
                    for k in range(8):
                        P.mm(pv[:, 0:264], hnT[:, k, i * 128:(i + 1) * 128], W[:, k, cV:cV + 264], k == 0, k == 7,
                             r=[bh, bW], w=[bpv])
                    import os
                    vv = os.environ.get("VVAR", "")
                    if vv != "2":
                        P.cp("act", Vtok[:, tt_, :, 0:64], pv[:, 0:256].rearrange("p (g d) -> p g d", d=64), r=[bpv], w=[bV])
                    if vv != "1":
                        P.ts("dve", WI[:, tt_, :], pv[:, 256:264], (8 ** -0.5) * (64 ** -0.5), None, ALU.mult,
                             r=[bpv], w=[bWI])
            P.barrier()

        if getattr(C, "stop", "").startswith("B1"):
            return
        with ExitStack() as eb:
            w_out = ins["o_w_out"][0]
            wo = sb(nc, eb, "wo64", [64, 16, D], BF16)
            bwo = Buf("wo64")
            wst = [(sb(nc, eb, "wo_st%d" % i, [64, 1, D], F32), Buf()) for i in range(2)]
            for hp in range(16):
                s, bs = wst[hp % 2]
                P.dma("sp", s[:], w_out[hp * 64:(hp + 1) * 64, :].rearrange("(h d) c -> d h c", d=64), w=[bs])
                P.cp("dve" if hp % 2 == 0 else "pool", wo[:, hp:hp + 1, :], s[:], r=[bs], w=[bwo])
            fg = sb(nc, eb, "fg", [128, D], F32)
            bfg = Buf("fg")
            P.dma("sp", fg[:], ins["final_g"].partition_broadcast(128), w=[bfg])
            ones = sb(nc, eb, "Esel", [128, 128], F32)
            bon = Buf("Esel")
            P.memset("pool", ones[:], 0.0, w=[bon])
            P.memset("pool", ones[64:65, :], 1.0, w=[bon])
            cpow = sb(nc, eb, "cpow", [128, NIT + 1], F32)
            bcp = Buf("cpow")
            for n in range(NIT + 1):
                P.memset("pool", cpow[:, n:n + 1], 2.0 ** -(n + 1), w=[bcp])
            SC = [(sb(nc, eb, "SC%d" % i, [128, L], F32), Buf()) for i in range(1)]
            Mq = (sb(nc, eb, "Mq", [128, L], BF16), Buf())
            junk = Mq
            MT = [(sb(nc, eb, "MT%d" % i, [128, NT, 128], BF16), Buf()) for i in range(1)]
            qq = [(sb(nc, eb, "qq%d" % i, [128, 8, 128], BF16), Buf()) for i in range(2)]
            zq = [(sb(nc, eb, "zq%d" % i, [64, 16, 128], BF16), Buf()) for i in range(2)]
            qiq = [(sb(nc, eb, "qiq%d" % i, [128, 4, 128], BF16), Buf()) for i in range(2)]
            xs = [(sb(nc, eb, "c_xs%d" % i, [128, D], F32), Buf()) for i in range(2)]
            rl = [(sb(nc, eb, "rl%d" % i, [128, 512], F32), Buf()) for i in range(2)]
            sm = {nm: (sb(nc, eb, "sm_" + nm, [128, 1], F32), Buf()) for nm in ("rmax", "rmin", "w0", "mid", "cnt", "t", "thr")}
            hs = (sb(nc, eb, "hs", [128, NIT + 1], F32), Buf())
            pe_ = [(sb(nc, eb, "pexp%d" % i, [128, 512], BF16), Buf()) for i in range(3)]
            pmk = [(sb(nc, eb, "pmk%d" % i, [128, 512], BF16), Buf()) for i in range(3)]
            osb = [(sb(nc, eb, "osb%d" % i, [65, 512], F32), Buf()) for i in range(2)]
            on_ = [(sb(nc, eb, "on%d" % i, [64, 512], F32), Buf()) for i in range(2)]
            rbc = [(sb(nc, eb, "rbc%d" % i, [64, 512], F32), Buf()) for i in range(2)]
            og = [(sb(nc, eb, "og%d" % i, [64, 16, 128], BF16), Buf()) for i in range(2)]
            ho = [(sb(nc, eb, "c_ho%d" % i, [128, D], F32), Buf()) for i in range(2)]
            ss = [(sb(nc, eb, "c_ss%d" % i, [128, 1], F32), Buf()) for i in range(2)]
            fjunk = (sb(nc, eb, "c_fjunk", [128, D], BF16), Buf())
            psc = [(ps(nc, eb, "psc%d" % i, [128, 512], F32), PBuf()) for i in range(1)]
            ptr = [(ps(nc, eb, "ptr%d" % i, [128, 4, 128], BF16), PBuf()) for i in range(1)]
            pl_ = [(ps(nc, eb, "pl%d" % i, [128, 512], F32), PBuf()) for i in range(4)]
            po_ = [(ps(nc, eb, "po%d" % i, [128, 512], F32), PBuf()) for i in range(1)]
            pop = (ps(nc, eb, "pop", [128, 512], F32), PBuf())
            pbc = (pop[0][0:64, :], pop[1])
            n_sc = n_rl = n_l = n_e = n_m = n_g = 0
            import os
            nqt = int(os.environ.get("NQT", NT))
            b2stop = os.environ.get("B2STOP", "")
            for qt in range(nqt):
                nk = (qt + 1) * 128
                nkb = (nk + 511) // 512
                tsl = slice(qt * 128, (qt + 1) * 128)
                q_t, bq = qq[qt % 2]
                z_t, bz = zq[qt % 2]
                qi_t, bqi = qiq[qt % 2]
                x_t, bx = xs[qt % 2]
                P.dma("sp", q_t[:], q_d[:, :, tsl].rearrange("t p l -> p t l"), w=[bq])
                P.dma("sp", qi_t[:], qi_d[:, :, tsl].rearrange("t p l -> p t l"), w=[bqi])
                P.dma("sp", z_t[:], z_d.rearrange("t (a d) l -> d (t a) l", a=2)[:, :, tsl], w=[bz])
                P.dma("sp", x_t[:], x_ap[tsl, :], w=[bx])
                sc, bsc = SC[0]
                for hi in range(8):
                    til, half = hi // 2, hi % 2
                    for kb in range(nkb):
                        w_ = min(512, nk - 512 * kb)
                        ksl = slice(512 * kb, 512 * kb + w_)
                        p, bp = psc[0]
                        n_sc += 1
                        P.mm(p[:, 0:w_], qi_t[64 * half:64 * half + 64, til, :], KI2[64 * half:64 * half + 64, ksl],
                             True, True, r=[bqi, bKI], w=[bp])
                        if hi == 0:
                            P.ts("dve", sc[:, ksl], p[:, 0:w_], 0.0, WI[:, qt, 0:1], ALU.max, ALU.mult,
                                 r=[bp, bWI], w=[bsc])
                        else:
                            r_, br = rl[n_rl % 2]
                            n_rl += 1
                            P.act(r_[:, 0:w_], p[:, 0:w_], AF.Relu, r=[bp], w=[br])
                            P.stt(sc[:, ksl], r_[:, 0:w_], WI[:, qt, hi:hi + 1], sc[:, ksl], ALU.mult, ALU.add,
                                  r=[br, bWI, bsc], w=[bsc])
                if b2stop == "s":
                    continue
                rmax, rmin, w0, mid, cnt, t_, thr = (sm[k_] for k_ in ("rmax", "rmin", "w0", "mid", "cnt", "t", "thr"))
                P.op("dve", lambda e, sc=sc, nk=nk: e.tensor_reduce(rmax[0][:], sc[:, 0:nk], AX.X, ALU.max), [bsc], [rmax[1]])
                P.op("dve", lambda e, sc=sc, nk=nk: e.tensor_reduce(rmin[0][:], sc[:, 0:nk], AX.X, ALU.min), [bsc], [rmin[1]])
                P.memset("pool", sc[0:64, nk - 64:nk], -1e30, w=[bsc])
                P.tt("dve", w0[0][:], rmax[0][:], rmin[0][:], ALU.subtract, r=[rmax[1], rmin[1]], w=[w0[1]])
                P.tt("dve", hs[0][:], cpow[:], w0[0][:].broadcast_to([128, NIT + 1]), ALU.mult, r=[bcp, w0[1]], w=[hs[1]])
                P.tt("dve", mid[0][:], rmin[0][:], hs[0][:, 0:1], ALU.add, r=[rmin[1], hs[1]], w=[mid[1]])
                for n in range(NIT):
                    P.ts("dve", junk[0][:, 0:nk], sc[:, 0:nk], mid[0][:], 0.0, ALU.is_ge, ALU.add, accum=cnt[0][:],
                         r=[bsc, mid[1]], w=[junk[1], cnt[1]])
                    P.ts("dve", t_[0][:], cnt[0][:], 255.5, hs[0][:, n:n + 1], ALU.is_gt, ALU.mult,
                         r=[cnt[1], hs[1]], w=[t_[1]])
                    P.stt(mid[0][:], t_[0][:], hs[0][:, n + 1:n + 2], mid[0][:], ALU.subtract, ALU.add,
                          r=[t_[1], hs[1], mid[1]], w=[mid[1]])
                P.tt("dve", thr[0][:], mid[0][:], hs[0][:, NIT:NIT + 1], ALU.subtract, r=[mid[1], hs[1]], w=[thr[1]])
                P.ts("dve", Mq[0][:, 0:nk], sc[:, 0:nk], thr[0][:], None, ALU.is_ge, r=[bsc, thr[1]], w=[Mq[1]])
                mt, bmt = MT[0]
                for k0 in range(0, qt + 1, 4):
                    kn = min(4, qt + 1 - k0)
                    pt, bpt = ptr[0]
                    for kk in range(kn):
                        kt = k0 + kk
                        P.tr(pt[:, kk, :], Mq[0][:, kt * 128:(kt + 1) * 128], C.ident_b[:], r=[Mq[1], C.b_ident], w=[bpt])
                    P.cp("act", mt[:, k0:k0 + kn, :], pt[:, 0:kn, :], r=[bpt], w=[bmt])
                if b2stop == "t":
                    continue
                og_t, bog = og[qt % 2]
                for g in range(4):
                    po, bpo = po_[0]
                    for kt in range(qt + 1):
                        pl, bpl = pl_[(2 * n_l) % 4]
                        pl2, bpl2 = pl_[(2 * n_l + 1) % 4]
                        n_l += 1
                        ks = slice(kt * 128, (kt + 1) * 128)
                        P.mm(pl[:, 0:256], KT2[0:64, g, ks], q_t[0:64, 2 * g:2 * g + 2, :], True, True, r=[bKT, bq], w=[bpl])
                        P.mm(pl2[:, 0:256], KT2[64:128, g, ks], q_t[64:128, 2 * g:2 * g + 2, :], True, True, r=[bKT, bq], w=[bpl2])
                        pe, bpe = pe_[n_e % 3]
                        pk, bpk = pmk[n_e % 3]
                        n_e += 1
                        P.act(pe[:, 0:256], pl[:, 0:256], AF.Exp, scale=0.125, r=[bpl], w=[bpe])
                        P.act(pe[:, 256:512], pl2[:, 0:256], AF.Exp, scale=0.125, r=[bpl2], w=[bpe])
                        P.tt("dve" if n_e % 2 == 0 else "pool", pk[:].rearrange("p (s t) -> p s t", s=4),
                             pe[:].rearrange("p (s t) -> p s t", s=4),
                             mt[:, kt, :].unsqueeze(1).broadcast_to([128, 4, 128]), ALU.mult, r=[bpe, bmt], w=[bpk])
                        P.mm(po[0:65, :], Vtok[:, kt, g, 0:65], pk[:], kt == 0, kt == qt, r=[bV, bpk], w=[bpo])
                    o_s, bos = osb[n_g % 2]
                    o_n, bonn = on_[n_g % 2]
                    n_g += 1
                    P.cp("act", o_s[:], po[0:65, :], r=[bpo], w=[bos])
                    P.mm(pop[0][:], ones[0:65, :], o_s[0:65, :], True, True, r=[bon, bos], w=[pop[1]])
                    r_b, brb = rbc[n_g % 2]
                    P.op("dve", lambda e, r_b=r_b: e.reciprocal(r_b[:], pop[0][0:64, :]), [pop[1]], [brb])
                    P.tt("dve", o_n[:], o_s[0:64, :], r_b[:], ALU.mult, r=[bos, brb], w=[bonn])
                    P.tt("pool", og_t[:, 4 * g:4 * g + 4, :].rearrange("p (i1 i0) t -> p i0 i1 t", i0=2),
                         o_n[:].rearrange("p (s1 s0 t) -> p s1 s0 t", s1=2, s0=2),
                         z_t[:, 4 * g:4 * g + 4, :].rearrange("p (i1 i0) t -> p i0 i1 t", i0=2), ALU.mult,
                         r=[bonn, bz], w=[bog])
                if b2stop == "a":
                    continue
                h, bh = ho[qt % 2]
                for half in range(2):
                    for hh in range(16):
                        P.mm(pop[0][:], og_t[:, hh, :], wo[:, hh, half * 512:(half + 1) * 512], hh == 0, hh == 15,
                             r=[bog, bwo], w=[pop[1]])
                    P.tt("dve", h[:, half * 512:(half + 1) * 512], pop[0][:], x_t[:, half * 512:(half + 1) * 512], ALU.add,
                         r=[pop[1], bx], w=[bh])
                s_, bs_ = ss[qt % 2]
                P.act(fjunk[0][:], h[:], AF.Square, accum=s_[:], r=[bh], w=[fjunk[1], bs_])
                P.act(s_[:], s_[:], AF.Sqrt, bias=C.eps_t[:], scale=1.0 / D, r=[bs_, C.b_eps], w=[bs_])
                P.op("dve", lambda e, s_=s_: e.reciprocal(s_[:], s_[:]), [bs_], [bs_])
                P.stt(h[:], h[:], s_[:], fg[:], ALU.mult, ALU.mult, r=[bh, bs_, bfg], w=[bh])
                P.dma("sp", o_ap[tsl, :], h[:], r=[bh])
            P.barrier()


def build(mode="full"):
    nc = bass.Bass("TRN2", target_bir_lowering=False)
    ins = {}

    def din(name, shape):
        ins[name] = nc.dram_tensor(name, list(shape), F32, kind="ExternalInput").ap()

    need0 = mode in ("full", "L0", "A", "S5")
    need1 = mode in ("full",) + ("L1", "B1", "R", "W", "B1q", "B1z", "B1k", "B1i", "B1v")
    din("x", (L, D))
    din("norm_g", (2, D))
    if need0:
        for nm in L0_NAMES:
            din(nm, SHAPES[nm])
    if need1:
        for nm in L1_NAMES:
            din(nm, SHAPES[nm])
    out = nc.dram_tensor("out", [L, D], F32, kind="ExternalOutput").ap()
    dbg = None
    if mode in ("A", "S5"):
        dbg = {"stage": mode,
               "out": nc.dram_tensor("dbg", [512, L], BF16, kind="ExternalOutput").ap(),
               "out32": nc.dram_tensor("dbg32", [512, L], F32, kind="ExternalOutput").ap()}
    with ExitStack() as es:
        C = Ctx()
        C.nc = nc
        C.es = es
        C.P = Prog(nc, es)
        build_consts(C)
        if mode == "full":
            h1 = nc.dram_tensor("h1_d", [L, D], F32, kind="Internal").ap()
            layer0(C, ins["x"], h1, ins, None)
            layer1(C, h1, out, ins)
        elif mode in ("L1", "B1", "R", "W", "B1q", "B1z", "B1k", "B1i", "B1v"):
            C.stop = mode if mode != "L1" else ""
            layer1(C, ins["x"], out, ins)
        else:
            layer0(C, ins["x"], out, ins, dbg)
        C.P.emit()
    return nc


SHAPES = {"e_w_in": (1, D, E_IN), "e_lam_re": (1, 32, 64), "e_lam_im": (1, 32, 64), "e_log_step": (1, 32),
          "e_b_re": (1, 32, 64, 16), "e_b_im": (1, 32, 64, 16), "e_c_re": (1, 32, 16, 64), "e_c_im": (1, 32, 16, 64),
          "e_d_skip": (1, 512), "e_w_glu": (1, 512, 512), "e_b_glu": (1, 512), "e_conv_w": (1, 31, 512),
          "e_conv_b": (1, 512), "e_ln_g": (1, 512), "e_ln_b": (1, 512), "e_w_out": (1, D, D),
          "o_w_in": (1, D, O_IN), "o_w_out": (1, D, D), "final_g": (D,)}
L0_NAMES = ["e_w_in", "e_lam_re", "e_lam_im", "e_log_step", "e_b_re", "e_b_im", "e_c_re", "e_c_im", "e_d_skip",
            "e_w_glu", "e_b_glu", "e_conv_w", "e_conv_b", "e_ln_g", "e_ln_b", "e_w_out"]
L1_NAMES = ["o_w_in", "o_w_out", "final_g"]


def run(inputs, mode="full", x_override=None, trace=False):
    nc = build(mode)
    names = ["norm_g"]
    if mode in ("full", "L0", "A", "S5"):
        names += L0_NAMES
    if mode in ("full",) + ("L1", "B1", "R", "W", "B1q", "B1z", "B1k", "B1i", "B1v"):
        names += L1_NAMES
    shared = {k: np.ascontiguousarray(np.asarray(inputs[k], dtype=np.float32)) for k in names}
    x = x_override if x_override is not None else inputs["x"]
    in_maps = []
    for c in range(8):
        m = dict(shared)
        m["x"] = np.ascontiguousarray(np.asarray(x[c], dtype=np.float32))
        in_maps.append(m)
    return run_bass_kernel_spmd(nc, in_maps, core_ids=list(range(8)), trace=trace)


MODE = "full"


def kernel(**inputs):
    if MODE == "full":
        res = run(inputs, "full")
        return np.stack([np.asarray(r["out"]) for r in res.results], axis=0).astype(np.float32)
    r0 = run(inputs, "L0")
    h1 = [np.asarray(r["out"]) for r in r0.results]
    r1 = run(inputs, "L1", x_override=h1)
    return np.stack([np.asarray(r["out"]) for r in r1.results], axis=0).astype(np.float32)
```

```python
import math
from contextlib import ExitStack

import numpy as np
import concourse.bass as bass
import concourse.mybir as mybir
from concourse.bass_utils import run_bass_kernel_spmd

F32 = mybir.dt.float32
BF16 = mybir.dt.bfloat16
ALU = mybir.AluOpType
AF = mybir.ActivationFunctionType

L = 4096
D = 1024
NT = L // 128
NB = L // 512
EPS = 1e-6
E_IN = 2560
O_IN = 3144

ENGS = ["pe", "act", "dve", "pool", "sp"]
EPOCH_LEN = 3000


class Buf:
    __slots__ = ("lw", "rd", "name")

    def __init__(self, name=""):
        self.lw = None
        self.rd = {}
        self.name = name


class PBuf(Buf):
    __slots__ = ()


class Prog:
    def __init__(self, nc, es, n_dma_sems=24):
        self.nc = nc
        self.es = es
        self.sem = {}
        self.epoch = {e: 0 for e in ENGS}
        self.cnt = {e: 0 for e in ENGS}
        for e in ENGS:
            self.sem[e + "#0"] = es.enter_context(nc.semaphore("s_%s_0" % e))
        self.ops = {e: [] for e in ENGS}
        self.known = {e: {} for e in ENGS}
        self.pending = {e: {} for e in ENGS}
        self.dsem = [es.enter_context(nc.semaphore("d%d" % i)) for i in range(n_dma_sems)]
        self.dval = [0] * n_dma_sems
        self.dnext = 0

    def semh(self, key):
        if isinstance(key, tuple):
            return self.dsem[key[1]]
        return self.sem[key]

    def op(self, E, fn, r=(), w=(), dma=False):
        waits = {}
        known = self.known[E]

        def need(k, v):
            if E == "pe" and isinstance(k, str) and k.startswith("pe#"):
                return
            if known.get(k, 0) >= v:
                return
            if waits.get(k, 0) < v:
                waits[k] = v

        for k, v in self.pending[E].items():
            need(k, v)
        self.pending[E] = {}
        for b in r:
            if b.lw is not None:
                need(*b.lw)
            if isinstance(b, PBuf):
                for k, v in b.rd.items():
                    if not (isinstance(k, str) and k.split("#")[0] == E):
                        need(k, v)
        for b in w:
            if b.lw is not None:
                need(*b.lw)
            for k, v in b.rd.items():
                need(k, v)
        if dma:
            i = self.dnext
            self.dnext = (i + 1) % len(self.dsem)
            if self.dval[i] > 0:
                need(("d", i), self.dval[i])
            self.dval[i] += 16
            tok = (("d", i), self.dval[i])
            inc = 16
        else:
            if self.cnt[E] >= EPOCH_LEN:
                self.epoch[E] += 1
                self.cnt[E] = 0
                self.sem["%s#%d" % (E, self.epoch[E])] = self.es.enter_context(
                    self.nc.semaphore("s_%s_%d" % (E, self.epoch[E])))
            self.cnt[E] += 1
            tok = ("%s#%d" % (E, self.epoch[E]), self.cnt[E])
            inc = 1
        for k, v in waits.items():
            known[k] = v
        self.ops[E].append((list(waits.items()), fn, tok[0], inc))
        for b in r:
            if b.rd.get(tok[0], 0) < tok[1]:
                b.rd[tok[0]] = tok[1]
        for b in w:
            b.lw = tok
            b.rd = {}
        return tok

    def barrier(self):
        snap = {"%s#%d" % (e, self.epoch[e]): self.cnt[e] for e in ENGS if self.cnt[e] > 0}
        for i, v in enumerate(self.dval):
            if v > 0:
                snap[("d", i)] = v
        for e in ENGS:
            p = self.pending[e]
            for k, v in snap.items():
                if p.get(k, 0) < v:
                    p[k] = v

    def emit(self):
        self.barrier()
        final = {e: [(k, v) for k, v in self.pending[e].items()
                     if not (e == "pe" and isinstance(k, str) and k.startswith("pe#"))
                     and self.known[e].get(k, 0) < v] for e in ENGS}
        nc = self.nc
        with nc.Block() as block:
            def mk(E):
                def body(eng):
                    for waits, fn, key, inc in self.ops[E]:
                        for k, v in waits:
                            eng.wait_ge(self.semh(k), v)
                        ins = fn(eng)
                        ins.then_inc(self.semh(key), inc)
                    for k, v in final[E]:
                        eng.wait_ge(self.semh(k), v)
                return body
            block.tensor(mk("pe"))
            block.scalar(mk("act"))
            block.vector(mk("dve"))
            block.gpsimd(mk("pool"))
            block.sync(mk("sp"))

    def mm(self, out, lhsT, rhs, start, stop, r=(), w=()):
        return self.op("pe", lambda e: e.matmul(out, lhsT, rhs, start=start, stop=stop), r, w)

    def tr(self, out, in_, ident, r=(), w=()):
        return self.op("pe", lambda e: e.transpose(out, in_, ident), r, w)

    def act(self, out, in_, func, bias=None, scale=None, accum=None, r=(), w=()):
        kw = {}
        if bias is not None:
            kw["bias"] = bias
        if scale is not None:
            kw["scale"] = scale
        if accum is not None:
            kw["accum_out"] = accum
        return self.op("act", lambda e: e.activation(out, in_, func, **kw), r, w)

    def ts(self, E, out, in0, s1, s2, op0, op1=None, accum=None, r=(), w=()):
        kw = {}
        if op1 is not None:
            kw["op1"] = op1
        if accum is not None:
            kw["accum_out"] = accum
        return self.op(E, lambda e: e.tensor_scalar(out, in0, s1, s2, op0, **kw), r, w)

    def tt(self, E, out, in0, in1, op, r=(), w=()):
        return self.op(E, lambda e: e.tensor_tensor(out, in0, in1, op), r, w)

    def stt(self, out, in0, scalar, in1, op0, op1, r=(), w=()):
        return self.op("dve", lambda e: e.scalar_tensor_tensor(out, in0, scalar, in1, op0, op1), r, w)

    def cp(self, E, out, in_, r=(), w=()):
        if E == "act":
            return self.op("act", lambda e: e.copy(out, in_), r, w)
        return self.op(E, lambda e: e.tensor_copy(out, in_), r, w)

    def memset(self, E, ap, val, w=()):
        return self.op(E, lambda e: e.memset(ap, val), (), w)

    def dma(self, E, out, in_, r=(), w=(), slow=False):
        if slow:
            return self.op(E, lambda e: e.dma_start(out=out, in_=in_, allow_slow_non_contiguous=True), r, w, dma=True)
        return self.op(E, lambda e: e.dma_start(out=out, in_=in_), r, w, dma=True)


class Ctx:
    pass


def sb(nc, es, name, shape, dt):
    return es.enter_context(nc.sbuf_tensor(name, shape, dt))


def ps(nc, es, name, shape, dt):
    return es.enter_context(nc.psum_tensor(name, shape, dt))


def build_consts(C):
    nc, P, es = C.nc, C.P, C.es
    C.ident_f = sb(nc, es, "ident_f", [128, 128], F32)
    C.ident_b = sb(nc, es, "ident_b", [128, 128], BF16)
    C.b_ident = Buf("ident")
    P.memset("pool", C.ident_f[:], 0.0, w=[C.b_ident])
    P.op("pool", lambda e: e.affine_select(out=C.ident_f[:], in_=C.ident_f[:], pattern=[[-1, 128]],
                                           compare_op=ALU.not_equal, fill=1.0, base=0,
                                           channel_multiplier=1), [C.b_ident], [C.b_ident])
    P.cp("pool", C.ident_b[:], C.ident_f[:], r=[C.b_ident], w=[C.b_ident])
    C.eps_t = sb(nc, es, "eps_t", [128, 1], F32)
    C.b_eps = Buf("eps")
    P.memset("pool", C.eps_t[:], EPS, w=[C.b_eps])


def rms_tiles_to_hnT(C, src_ap_fn, hnT, hnT_bufs, blk, st):
    nc, P = C.nc, C.P
    for i in range(4):
        tt = blk * 4 + i
        s = st["xs_i"] % len(st["xs"])
        st["xs_i"] += 1
        xt, bx = st["xs"][s]
        P.dma("sp", xt[:], src_ap_fn(tt), w=[bx])
        ss, bss = st["ss"][s]
        junk, bj = st["junk"]
        P.act(junk[:], xt[:], AF.Square, accum=ss[:], r=[bx], w=[bj, bss])
        P.act(ss[:], ss[:], AF.Sqrt, bias=C.eps_t[:], scale=1.0 / D, r=[bss, C.b_eps], w=[bss])
        P.op("dve", lambda e, ss=ss: e.reciprocal(ss[:], ss[:]), [bss], [bss])
        xn, bxn = st["xn"][s % len(st["xn"])]
        P.ts("dve", xn[:], xt[:], ss[:], None, ALU.mult, r=[bx, bss], w=[bxn])
        pt, bpt = st["pst"][st["pst_i"] % len(st["pst"])]
        st["pst_i"] += 1
        for k in range(8):
            P.tr(pt[:, k, :], xn[:, k * 128:(k + 1) * 128], C.ident_b[:], r=[bxn, C.b_ident], w=[bpt])
        eng = "act" if (i % 2 == 0) else "dve"
        P.cp(eng, hnT[:, :, i * 128:(i + 1) * 128], pt[:], r=[bpt], w=[hnT_bufs])


def load_weight_bf16(C, wdst, bw, wsrc, ncols, g_t, bg, stage, col0=0, colmap=None):
    P = C.P
    for k in range(8):
        stg, bs = stage[k % len(stage)]
        P.dma("sp", stg[:, 0:ncols], wsrc[k * 128:(k + 1) * 128, :], w=[bs])
        eng = "dve" if k % 2 == 0 else "pool"
        P.ts(eng, wdst[:, k, col0:col0 + ncols], stg[:, 0:ncols], g_t[:, k:k + 1], None, ALU.mult,
             r=[bs, bg], w=[bw])


PW = [1] + [16 * (2 ** i) for i in range(8)]
PI = math.pi
CW1 = 6.28125
CW2 = 2 * math.pi - 6.28125


def s5_setup(C, ins, es):
    nc, P = C.nc, C.P
    S = Ctx()
    bS = Buf("s5setup")
    S.b = bS

    def t(name, shape, dt=F32):
        return sb(nc, es, "s5_" + name, shape, dt)

    lr, li, dt_ = t("lr", [128, 32]), t("li", [128, 32]), t("dt", [128, 32])
    for half in range(2):
        P.dma("sp", lr[half * 64:(half + 1) * 64, :], ins["e_lam_re"][0].rearrange("g n -> n g"), w=[bS], slow=True)
        P.dma("sp", li[half * 64:(half + 1) * 64, :], ins["e_lam_im"][0].rearrange("g n -> n g"), w=[bS], slow=True)
    P.dma("sp", dt_[:], ins["e_log_step"][0].partition_broadcast(128), w=[bS])
    bre, bim = t("bre", [128, 32, 16]), t("bim", [128, 32, 16])
    for half in range(2):
        P.dma("sp", bre[half * 64:(half + 1) * 64], ins["e_b_re"][0].rearrange("g n c -> n g c"), w=[bS])
        P.dma("sp", bim[half * 64:(half + 1) * 64], ins["e_b_im"][0].rearrange("g n c -> n g c"), w=[bS])
    cnat = t("cnat", [128, 4, 128])
    P.dma("sp", cnat[:, :, 0:64], ins["e_c_re"][0].rearrange("(t q) c n -> (q c) t n", q=8), w=[bS])
    P.dma("sp", cnat[:, :, 64:128], ins["e_c_im"][0].rearrange("(t q) c n -> (q c) t n", q=8), w=[bS])
    S.dskip = t("dskip", [128, 4])
    P.dma("sp", S.dskip[:], ins["e_d_skip"][0].rearrange("(t p) -> p t", p=128), w=[bS], slow=True)

    pidx_i = t("pidx_i", [128, 1], mybir.dt.int32)
    pidx = t("pidx", [128, 1])
    P.op("pool", lambda e: e.iota(pidx_i[:], pattern=[[0, 1]], base=0, channel_multiplier=1), (), [bS])
    P.cp("dve", pidx[:], pidx_i[:], r=[bS], w=[bS])
    mlo, mhi, sgn = t("mlo", [128, 1]), t("mhi", [128, 1]), t("sgn", [128, 1])
    P.ts("dve", mlo[:], pidx[:], 64.0, None, ALU.is_lt, r=[bS], w=[bS])
    P.ts("dve", mhi[:], mlo[:], -1.0, 1.0, ALU.mult, ALU.add, r=[bS], w=[bS])
    P.ts("dve", sgn[:], mlo[:], 2.0, -1.0, ALU.mult, ALU.add, r=[bS], w=[bS])
    mpar = t("mpar", [128, 4])
    pm64 = t("pm64", [128, 1])
    mtmp = t("mtmp", [128, 1])
    P.stt(pm64[:], mhi[:], -64.0, pidx[:], ALU.mult, ALU.add, r=[bS], w=[bS])
    for r4 in range(4):
        P.ts("dve", mtmp[:], pm64[:], 16.0 * r4 - 0.5, None, ALU.is_gt, r=[bS], w=[bS])
        P.ts("dve", mpar[:, r4:r4 + 1], pm64[:], 16.0 * r4 + 15.5, mtmp[:], ALU.is_lt, ALU.mult, r=[bS], w=[bS])
    negpi = t("negpi", [128, 1])
    P.memset("dve", negpi[:], -PI, w=[bS])
    S.sgn = sgn

    P.ts("dve", lr[:], lr[:], -1e-4, None, ALU.min, r=[bS], w=[bS])
    P.act(dt_[:], dt_[:], AF.Exp, r=[bS], w=[bS])
    lrdt, lidt = t("lrdt", [128, 32]), t("lidt", [128, 32])
    P.tt("dve", lrdt[:], lr[:], dt_[:], ALU.mult, r=[bS], w=[bS])
    P.tt("dve", lidt[:], li[:], dt_[:], ALU.mult, r=[bS], w=[bS])
    npw = len(PW)
    S.vr, S.vi = t("vr", [128, npw, 32]), t("vi", [128, npw, 32])
    mag, sn, cs, ph = t("mag", [128, 32]), t("sn", [128, 32]), t("cs", [128, 32]), t("ph", [128, 32])
    ai1 = t("ai1", [128, 32])
    ki_ = t("ki_", [128, 32], mybir.dt.int32)
    kf = t("kf", [128, 32])
    for ip, p in enumerate(PW):
        P.act(mag[:], lrdt[:], AF.Exp, scale=float(p), r=[bS], w=[bS])
        for dst, off in ((sn, 0.0), (cs, 0.5 * PI)):
            P.ts("dve", ph[:], lidt[:], float(p), off, ALU.mult, ALU.add, r=[bS], w=[bS])
            P.ts("dve", ki_[:], ph[:], 1.0 / (2 * PI), None, ALU.mult, r=[bS], w=[bS])
            P.cp("dve", kf[:], ki_[:], r=[bS], w=[bS])
            P.stt(ph[:], kf[:], -CW1, ph[:], ALU.mult, ALU.add, r=[bS], w=[bS])
            P.stt(ph[:], kf[:], -CW2, ph[:], ALU.mult, ALU.add, r=[bS], w=[bS])
            P.ts("dve", kf[:], ph[:], PI, None, ALU.is_gt, r=[bS], w=[bS])
            P.stt(ph[:], kf[:], -2 * PI, ph[:], ALU.mult, ALU.add, r=[bS], w=[bS])
            P.ts("dve", ph[:], ph[:], -PI, PI, ALU.max, ALU.min, r=[bS], w=[bS])
            P.act(dst[:], ph[:], AF.Sin, r=[bS], w=[bS])
        P.tt("dve", S.vr[:, ip, :], mag[:], cs[:], ALU.mult, r=[bS], w=[bS])
        if ip == 0:
            P.tt("dve", ai1[:], mag[:], sn[:], ALU.mult, r=[bS], w=[bS])
        P.stt(S.vi[:, ip, :], sn[:], sgn[:], mag[:], ALU.mult, ALU.mult, r=[bS], w=[bS])
    nr, den, kr, ki, tmp = t("nr", [128, 32]), t("den", [128, 32]), t("kr", [128, 32]), t("ki", [128, 32]), t("tmp", [128, 32])
    P.ts("dve", nr[:], S.vr[:, 0, :], -1.0, None, ALU.add, r=[bS], w=[bS])
    P.tt("dve", den[:], lr[:], lr[:], ALU.mult, r=[bS], w=[bS])
    P.tt("dve", tmp[:], li[:], li[:], ALU.mult, r=[bS], w=[bS])
    P.tt("dve", den[:], den[:], tmp[:], ALU.add, r=[bS], w=[bS])
    P.op("dve", lambda e: e.reciprocal(den[:], den[:]), [bS], [bS])
    P.tt("dve", kr[:], nr[:], lr[:], ALU.mult, r=[bS], w=[bS])
    P.tt("dve", tmp[:], ai1[:], li[:], ALU.mult, r=[bS], w=[bS])
    P.tt("dve", kr[:], kr[:], tmp[:], ALU.add, r=[bS], w=[bS])
    P.tt("dve", kr[:], kr[:], den[:], ALU.mult, r=[bS], w=[bS])
    P.tt("dve", ki[:], ai1[:], lr[:], ALU.mult, r=[bS], w=[bS])
    P.tt("dve", tmp[:], nr[:], li[:], ALU.mult, r=[bS], w=[bS])
    P.tt("dve", ki[:], ki[:], tmp[:], ALU.subtract, r=[bS], w=[bS])
    P.tt("dve", ki[:], ki[:], den[:], ALU.mult, r=[bS], w=[bS])
    KA, KB = t("KA", [128, 32]), t("KB", [128, 32])
    P.ts("dve", KA[:], kr[:], mlo[:], None, ALU.mult, r=[bS], w=[bS])
    P.stt(KA[:], ki[:], mhi[:], KA[:], ALU.mult, ALU.add, r=[bS], w=[bS])
    P.ts("dve", KB[:], kr[:], mhi[:], None, ALU.mult, r=[bS], w=[bS])
    P.ts("dve", tmp[:], ki[:], mlo[:], None, ALU.mult, r=[bS], w=[bS])
    P.tt("dve", KB[:], KB[:], tmp[:], ALU.subtract, r=[bS], w=[bS])
    bbar, btmp = t("bbar", [128, 32, 16]), t("btmp", [128, 32, 16])
    P.tt("dve", bbar[:], bre[:], KA[:].unsqueeze(2).broadcast_to([128, 32, 16]), ALU.mult, r=[bS], w=[bS])
    P.tt("dve", btmp[:], bim[:], KB[:].unsqueeze(2).broadcast_to([128, 32, 16]), ALU.mult, r=[bS], w=[bS])
    P.tt("dve", bbar[:], bbar[:], btmp[:], ALU.add, r=[bS], w=[bS])
    S.BT = t("BT", [128, 4, 4, 128], BF16)
    S.Cpad = t("Cpad", [128, 32, 128], BF16)
    P.memset("pool", S.Cpad[:], 0.0, w=[bS])
    pst = ps(nc, es, "s5_pst", [128, 128], F32)
    CSt = t("CSt", [128, 128])
    for T in range(4):
        P.tr(pst[:], bbar[:, 8 * T:8 * T + 8, :].rearrange("p g c -> p (g c)"), C.ident_f[:], r=[bS, C.b_ident], w=[bS])
        for par in range(4):
            P.ts("dve", S.BT[:, T, par, :], pst[:], mpar[:, par:par + 1], None, ALU.mult, r=[bS], w=[bS])
        P.tr(pst[:], cnat[:, T, :], C.ident_f[:], r=[bS, C.b_ident], w=[bS])
        P.ts("dve", CSt[:], pst[:], sgn[:], None, ALU.mult, r=[bS], w=[bS])
        for q in range(8):
            P.cp("dve", S.Cpad[:, 8 * T + q, 16 * q:16 * q + 16], CSt[:, 16 * q:16 * q + 16], r=[bS], w=[bS])
    S.J = t("J", [128, 128])
    P.cp("dve", S.J[:, 64:128], C.ident_f[:, 0:64], r=[C.b_ident], w=[bS])
    P.cp("dve", S.J[:, 0:64], C.ident_f[:, 64:128], r=[C.b_ident], w=[bS])
    return S


def s5_main(C, S, U, bU, G, bG, es, dbg):
    nc, P = C.nc, C.P
    bS = S.b
    pstep = [(ps(nc, es, "s5_pp%d" % i, [128, 256], F32), PBuf()) for i in range(5)]
    py = [(ps(nc, es, "s5_py%d" % i, [128, 256], F32), PBuf()) for i in range(2)]
    Sst = [[(sb(nc, es, "s5_S%d_%d" % (q, i), [128, 256], BF16), Buf()) for i in range(2)] for q in range(8)]
    Epp = [[(sb(nc, es, "s5_E%d_%d" % (q, i), [128, 256], F32), Buf()) for i in range(2)] for q in range(8)]
    Eprev = [(sb(nc, es, "s5_Ep%d" % q, [128, 256], BF16), Buf()) for q in range(8)]
    A1 = [(sb(nc, es, "s5_A1_%d" % q, [128, 2, 128], BF16), Buf()) for q in range(8)]
    Af = [(sb(nc, es, "s5_Af%d" % i, [128, 128], F32), Buf()) for i in range(4)]
    Atmp = (sb(nc, es, "s5_Atmp", [128, 128], F32), Buf())
    Ahi32 = (sb(nc, es, "s5_Ahi32", [128, 128], F32), Buf())
    yat = [(sb(nc, es, "s5_ya%d" % i, [128, 256], F32), Buf()) for i in range(2)]
    ppi = 0
    afi = 0
    yi = 0

    def gen_A(dst, bd, ip, g, eng):
        P.ts(eng, dst[:], C.ident_f[:], S.vr[:, ip, g:g + 1], None, ALU.mult, r=[C.b_ident, bS], w=[bd])
        P.stt(dst[:], S.J[:], S.vi[:, ip, g:g + 1], dst[:], ALU.mult, ALU.add, r=[bS, bd], w=[bd])

    for T in range(4):
        for q in range(8):
            g = 8 * T + q
            a1, ba = A1[q]
            gen_A(Atmp[0], Atmp[1], 0, g, "dve")
            P.cp("dve", a1[:, 0, :], Atmp[0][:], r=[Atmp[1]], w=[ba])
            P.cp("dve", Ahi32[0][:], a1[:, 0, :], r=[ba], w=[Ahi32[1]])
            P.tt("dve", a1[:, 1, :], Atmp[0][:], Ahi32[0][:], ALU.subtract, r=[Atmp[1], Ahi32[1]], w=[ba])
        for pas in range(2):
            for j in range(16):
                for q in range(8):
                    g = 8 * T + q
                    pr = q // 4
                    pp, bp = pstep[ppi % len(pstep)]
                    ppi += 1
                    a1, ba = A1[q]
                    sprev = None
                    if j > 0:
                        sprev = Sst[q][(j - 1) % 2]
                    elif pas == 1:
                        sprev = Eprev[q]
                    P.mm(pp[:], S.BT[64 * pr:64 * pr + 64, T, q % 4, :], U[64 * pr:64 * pr + 64, T, j, :],
                         True, sprev is None, r=[bS, bU[T]], w=[bp])
                    if sprev is not None:
                        P.mm(pp[:], a1[:, 0, :], sprev[0][:], False, False, r=[ba, sprev[1]], w=[bp])
                        P.mm(pp[:], a1[:, 1, :], sprev[0][:], False, True, r=[ba, sprev[1]], w=[bp])
                    sc, bs = Sst[q][j % 2]
                    if pas == 0 and j == 15:
                        P.cp("act", Epp[q][0][0][:], pp[:], r=[bp], w=[Epp[q][0][1]])
                    else:
                        eng = "act" if (q % 2 == 0) else "dve"
                        P.cp(eng, sc[:], pp[:], r=[bp], w=[bs])
                if pas == 1:
                    pyt, bpy = py[yi % 2]
                    ya, bya = yat[yi % 2]
                    yi += 1
                    for q in range(8):
                        sc, bs = Sst[q][j % 2]
                        P.mm(pyt[:], S.Cpad[:, 8 * T + q, :], sc[:], q == 0, q == 7, r=[bS, bs], w=[bpy])
                    P.stt(ya[:], U[:, T, j, :], S.dskip[:, T:T + 1], pyt[:], ALU.mult, ALU.add,
                          r=[bU[T], bS, bpy], w=[bya])
                    P.act(G[:, T, j, :], ya[:], AF.Gelu, r=[bya], w=[bG[T]])
                    if dbg is not None and dbg.get("stage") == "S5" and T == 0:
                        P.dma("sp", dbg["out32"][0:128, j * 256:(j + 1) * 256], ya[:], r=[bya])
            if pas == 0:
                for q in range(8):
                    g = 8 * T + q
                    cur = 0
                    for i in range(8):
                        d = 2 ** i
                        am, bam = Af[afi % len(Af)]
                        afi += 1
                        gen_A(am, bam, 1 + i, g, "pool" if False else "dve")
                        Ec, bEc = Epp[q][cur]
                        En, bEn = Epp[q][1 - cur]
                        pp, bp = pstep[ppi % len(pstep)]
                        ppi += 1
                        P.mm(pp[:, 0:256 - d], am[:], Ec[:, 0:256 - d], True, True, r=[bam, bEc], w=[bp])
                        P.tt("dve", En[:, d:256], Ec[:, d:256], pp[:, 0:256 - d], ALU.add, r=[bEc, bp], w=[bEn])
                        P.cp("act", En[:, 0:d], Ec[:, 0:d], r=[bEc], w=[bEn])
                        cur = 1 - cur
                    Ef, bEf = Epp[q][cur]
                    ep, bep = Eprev[q]
                    P.memset("pool", ep[:, 0:1], 0.0, w=[bep])
                    P.cp("act", ep[:, 1:256], Ef[:, 0:255], r=[bEf], w=[bep])


def load_cast(C, wdst, bw, wsrc_fn, nk, ncols, stage):
    P = C.P
    for k in range(nk):
        stg, bs = stage[k % len(stage)]
        P.dma("sp", stg[:, 0:ncols], wsrc_fn(k), w=[bs])
        P.cp("dve" if k % 2 == 0 else "pool", wdst[:, k, 0:ncols], stg[:, 0:ncols], r=[bs], w=[bw])


def glu_stage(C, ins, G, bG, za_d, ycat_d, es):
    nc, P = C.nc, C.P
    wg = sb(nc, es, "wglu", [128, 4, 512], BF16)
    bwg = Buf()
    stage = [(sb(nc, es, "gst%d" % i, [128, 512], F32), Buf()) for i in range(2)]
    load_cast(C, wg, bwg, lambda k: ins["e_w_glu"][0][k * 128:(k + 1) * 128, :], 4, 512, stage)
    bgl = sb(nc, es, "bglu", [128, 4], F32)
    bb = Buf()
    P.dma("sp", bgl[:], ins["e_b_glu"][0].rearrange("(t p) -> p t", p=128), w=[bb], slow=True)
    YAn = sb(nc, es, "YAn", [128, 4, L], BF16)
    bY = [Buf() for _ in range(4)]
    pg = [(ps(nc, es, "pglu%d" % i, [128, 512], F32), PBuf()) for i in range(3)]
    sg = [(sb(nc, es, "gsig%d" % i, [128, 512], F32), Buf()) for i in range(2)]
    zt = [(sb(nc, es, "gza%d" % i, [128, 512], BF16), Buf()) for i in range(3)]
    tm = [(sb(nc, es, "gtm%d" % i, [128, 512], F32), Buf()) for i in range(2)]
    n = 0
    for b in range(8):
        for T in range(4):
            p, bp = pg[n % 3]
            s, bs = sg[n % 2]
            z, bz = zt[n % 3]
            t, bt = tm[n % 2]
            n += 1
            P.dma("sp", z[:], za_d[T][:, b * 512:(b + 1) * 512], w=[bz])
            for kc in range(4):
                P.mm(p[:], wg[:, kc, T * 128:(T + 1) * 128],
                     G[:, kc, 2 * b:2 * b + 2, :].rearrange("p j m -> p (j m)"), kc == 0, kc == 3,
                     r=[bwg, bG[kc]], w=[bp])
            P.act(s[:], p[:], AF.Sigmoid, bias=bgl[:, T:T + 1], r=[bp, bb], w=[bs])
            P.tt("dve", t[:], G[:, T, 2 * b:2 * b + 2, :].rearrange("p j m -> p (j m)"), s[:], ALU.mult,
                 r=[bG[T], bs], w=[bt])
            P.tt("pool", YAn[:, T].rearrange("p (m j) -> p j m", j=16)[:, 2 * b:2 * b + 2, :],
                 t[:].rearrange("p (j m) -> p j m", j=2), z[:].rearrange("p (j m) -> p j m", j=2), ALU.mult,
                 r=[bt, bz], w=[bY[T]])
    for T in range(4):
        P.dma("sp", ycat_d[T], YAn[:, T], r=[bY[T]])


def conv_stage(C, ins, H, bH, zb_d, ycat_d, es):
    nc, P = C.nc, C.P
    bc = Buf("convsetup")
    cwn = sb(nc, es, "cwn", [31, 512], F32)
    P.dma("sp", cwn[:], ins["e_conv_w"][0], w=[bc])
    cw = sb(nc, es, "cw", [128, 4, 31], F32)
    pcw = ps(nc, es, "pcw", [128, 31], F32)
    for T in range(4):
        P.tr(pcw[:], cwn[0:31, T * 128:(T + 1) * 128], C.ident_f[0:31, 0:31], r=[bc, C.b_ident], w=[bc])
        P.cp("dve", cw[:, T, :], pcw[:], r=[bc], w=[bc])
    vecs = {}
    for nm in ("e_conv_b", "e_ln_g", "e_ln_b"):
        v = sb(nc, es, "cv_" + nm, [128, 4], F32)
        P.dma("sp", v[:], ins[nm][0].rearrange("(t p) -> p t", p=128), w=[bc], slow=True)
        vecs[nm] = v
    Dg = sb(nc, es, "Dg", [128, 4, 31, 128], BF16)
    for T in range(4):
        for k in range(31):
            P.ts("dve" if (k % 2 == 0) else "pool", Dg[:, T, k, :], C.ident_b[:], cw[:, T, k:k + 1], None, ALU.mult,
                 r=[bc, C.b_ident], w=[bc])
    ones = sb(nc, es, "ones_f", [128, 128], F32)
    P.memset("pool", ones[:], 1.0, w=[bc])
    pc = [(ps(nc, es, "pconv%d" % i, [128, 512], F32), PBuf()) for i in range(3)]
    pmu = (ps(nc, es, "pmu", [128, 512], F32), PBuf())
    psq = (ps(nc, es, "psq", [128, 512], F32), PBuf())
    HC = [[(sb(nc, es, "HC%d_%d" % (i, T), [128, 512], F32), Buf()) for T in range(4)] for i in range(2)]
    SQ = [(sb(nc, es, "SQ%d" % i, [128, 512], F32), Buf()) for i in range(2)]
    mu = (sb(nc, es, "c_mu", [128, 512], F32), Buf())
    rs = (sb(nc, es, "c_rs", [128, 512], F32), Buf())
    t1 = [(sb(nc, es, "c_t1%d" % i, [128, 512], F32), Buf()) for i in range(2)]
    zb = [(sb(nc, es, "c_zb%d" % i, [128, 512], BF16), Buf()) for i in range(3)]
    yo = [(sb(nc, es, "c_yo%d" % i, [128, 512], BF16), Buf()) for i in range(3)]
    n = 0
    nq = 0
    for b in range(8):
        hc = HC[b % 2]
        for T in range(4):
            p, bp = pc[n % 3]
            n += 1
            for k in range(31):
                P.mm(p[:], Dg[:, T, k, :], H[:, T, b * 512 + k:b * 512 + k + 512], k == 0, k == 30,
                     r=[bc, bH[T]], w=[bp])
            P.act(hc[T][0][:], p[:], AF.Identity, bias=vecs["e_conv_b"][:, T:T + 1], r=[bp, bc], w=[hc[T][1]])
            sq, bsq = SQ[nq % 2]
            nq += 1
            P.act(sq[:], p[:], AF.Square, bias=vecs["e_conv_b"][:, T:T + 1], r=[bp, bc], w=[bsq])
            P.mm(pmu[0][:], ones[:], hc[T][0][:], T == 0, T == 3, r=[bc, hc[T][1]], w=[pmu[1]])
            P.mm(psq[0][:], ones[:], sq[:], T == 0, T == 3, r=[bc, bsq], w=[psq[1]])
        P.act(mu[0][:], pmu[0][:], AF.Copy, scale=1.0 / 512, r=[pmu[1]], w=[mu[1]])
        P.act(rs[0][:], pmu[0][:], AF.Square, scale=1.0 / 512, r=[pmu[1]], w=[rs[1]])
        P.stt(rs[0][:], psq[0][:], 1.0 / 512, rs[0][:], ALU.mult, ALU.subtract, r=[psq[1], rs[1]], w=[rs[1]])
        P.act(rs[0][:], rs[0][:], AF.Sqrt, bias=C.eps_t[:], r=[rs[1], C.b_eps], w=[rs[1]])
        P.op("dve", lambda e: e.reciprocal(rs[0][:], rs[0][:]), [rs[1]], [rs[1]])
        for T in range(4):
            t, bt = t1[T % 2]
            z, bz = zb[(b * 4 + T) % 3]
            y, by = yo[(b * 4 + T) % 3]
            P.dma("sp", z[:], zb_d[T][:, b * 512:(b + 1) * 512], w=[bz])
            P.tt("dve", t[:], hc[T][0][:], mu[0][:], ALU.subtract, r=[hc[T][1], mu[1]], w=[bt])
            P.tt("pool", t[:], t[:], rs[0][:], ALU.mult, r=[bt, rs[1]], w=[bt])
            P.act(t[:], t[:], AF.Silu, bias=vecs["e_ln_b"][:, T:T + 1], scale=vecs["e_ln_g"][:, T:T + 1],
                  r=[bt, bc], w=[bt])
            P.tt("dve", y[:], t[:], z[:], ALU.mult, r=[bt, bz], w=[by])
            P.dma("sp", ycat_d[4 + T][:, b * 512:(b + 1) * 512], y[:], r=[by])


def outproj_stage(C, w_ap, ycat_d, x_ap, o_ap, es, final_g=None):
    nc, P = C.nc, C.P
    wo = sb(nc, es, "wout", [128, 8, D], BF16)
    bwo = Buf()
    stage = [(sb(nc, es, "ost%d" % i, [128, D], F32), Buf()) for i in range(2)]
    load_cast(C, wo, bwo, lambda k: w_ap[k * 128:(k + 1) * 128, :], 8, D, stage)
    yc = [(sb(nc, es, "o_yc%d" % i, [128, 8, 128], BF16), Buf()) for i in range(3)]
    xs = [(sb(nc, es, "o_xs%d" % i, [128, D], F32), Buf()) for i in range(3)]
    ho = [(sb(nc, es, "o_ho%d" % i, [128, D], F32), Buf()) for i in range(2)]
    po = [(ps(nc, es, "o_ps%d" % i, [128, 512], F32), PBuf()) for i in range(4)]
    if final_g is not None:
        fg = sb(nc, es, "o_fg", [128, D], F32)
        bfg = Buf()
        P.dma("sp", fg[:], final_g.partition_broadcast(128), w=[bfg])
        ss = [(sb(nc, es, "o_ss%d" % i, [128, 1], F32), Buf()) for i in range(2)]
        junk = (sb(nc, es, "o_junk", [128, D], BF16), Buf())
    n = 0
    for tt in range(NT):
        y, by = yc[tt % 3]
        x, bx = xs[tt % 3]
        h, bh = ho[tt % 2]
        P.dma("sp", y[:], ycat_d[:, :, tt * 128:(tt + 1) * 128].rearrange("c p t -> p c t"), w=[by])
        P.dma("sp", x[:], x_ap[tt * 128:(tt + 1) * 128, :], w=[bx])
        for half in range(2):
            p, bp = po[n % 4]
            n += 1
            for c in range(8):
                P.mm(p[:], y[:, c, :], wo[:, c, half * 512:(half + 1) * 512], c == 0, c == 7, r=[by, bwo], w=[bp])
            P.tt("dve", h[:, half * 512:(half + 1) * 512], p[:], x[:, half * 512:(half + 1) * 512], ALU.add,
                 r=[bp, bx], w=[bh])
        if final_g is not None:
            s, bs = ss[tt % 2]
            P.act(junk[0][:], h[:], AF.Square, accum=s[:], r=[bh], w=[junk[1], bs])
            P.act(s[:], s[:], AF.Sqrt, bias=C.eps_t[:], scale=1.0 / D, r=[bs, C.b_eps], w=[bs])
            P.op("dve", lambda e, s=s: e.reciprocal(s[:], s[:]), [bs], [bs])
            P.stt(h[:], h[:], s[:], fg[:], ALU.mult, ALU.mult, r=[bh, bs, bfg], w=[bh])
        P.dma("sp", o_ap[tt * 128:(tt + 1) * 128, :], h[:], r=[bh])


def layer0(C, x_ap, h1_ap, ins, dbg=None):
    nc, P = C.nc, C.P
    with ExitStack() as es:
        U = sb(nc, es, "U", [128, 4, 16, 256], BF16)
        bU = [Buf("U%d" % t) for t in range(4)]
        H = sb(nc, es, "H", [128, 4, 30 + L], BF16)
        bH = [Buf("H%d" % t) for t in range(4)]
        for t in range(4):
            P.memset("pool", H[:, t, 0:30], 0.0, w=[bH[t]])
        za_d = nc.dram_tensor("za_d", [4, 128, L], BF16, kind="Internal").ap()
        zb_d = nc.dram_tensor("zb_d", [4, 128, L], BF16, kind="Internal").ap()

        with ExitStack() as ea:
            W = sb(nc, ea, "W0", [128, 8, E_IN], BF16)
            bW = Buf("W0")
            g_t = sb(nc, ea, "g0", [128, 8], F32)
            bg = Buf("g0")
            P.dma("sp", g_t[:], ins["norm_g"][0].rearrange("(k p) -> p k", p=128), w=[bg], slow=True)
            stage = [(sb(nc, ea, "wst%d" % i, [128, E_IN], F32), Buf()) for i in range(2)]
            load_weight_bf16(C, W, bW, ins["e_w_in"][0], E_IN, g_t, bg, stage)
            st = {
                "xs": [(sb(nc, ea, "xs%d" % i, [128, D], F32), Buf()) for i in range(3)],
                "ss": [(sb(nc, ea, "ss%d" % i, [128, 1], F32), Buf()) for i in range(3)],
                "xn": [(sb(nc, ea, "xn%d" % i, [128, D], BF16), Buf()) for i in range(3)],
                "junk": (sb(nc, ea, "junk", [128, D], BF16), Buf()),
                "pst": [(ps(nc, ea, "pst%d" % i, [128, 8, 128], BF16), PBuf()) for i in range(2)],
                "xs_i": 0, "pst_i": 0,
            }
            hnTs = [(sb(nc, ea, "hnT%d" % i, [128, 8, 512], BF16), Buf()) for i in range(2)]
            pmm = [(ps(nc, ea, "pmm%d" % i, [128, 512], F32), PBuf()) for i in range(4)]
            sig = [(sb(nc, ea, "sig%d" % i, [128, 512], F32), Buf()) for i in range(2)]
            zst = [(sb(nc, ea, "zst%d" % i, [128, 512], BF16), Buf()) for i in range(4)]
            pi = 0
            zi = 0
            order = [0, 1, 2, 3, 4, 5, 6, 7, 12, 8, 13, 9, 14, 10, 15, 11, 16, 17, 18, 19]
            for blk in range(NB):
                hnT, bh = hnTs[blk % 2]
                rms_tiles_to_hnT(C, lambda tt: x_ap[tt * 128:(tt + 1) * 128, :], hnT, bh, blk, st)
                for ct in order:
                    pm, bp = pmm[pi % 4]
                    pi += 1
                    for k in range(8):
                        P.mm(pm[:], W[:, k, ct * 128:(ct + 1) * 128], hnT[:, k, :], k == 0, k == 7,
                             r=[bW, bh], w=[bp])
                    m0 = blk * 32
                    if ct < 4:
                        P.cp("dve", U[:, ct, :, m0:m0 + 32],
                             pm[:].rearrange("p (m j) -> p j m", j=16), r=[bp], w=[bU[ct]])
                    elif ct < 8:
                        z, bz = zst[zi % 4]
                        zi += 1
                        P.act(z[:].rearrange("p (j m) -> p j m", j=16),
                              pm[:].rearrange("p (m j) -> p j m", j=16), AF.Silu, r=[bp], w=[bz])
                        P.dma("sp", za_d[ct - 4].rearrange("p (j m) -> p j m", j=16)[:, :, m0:m0 + 32],
                              z[:].rearrange("p (j m) -> p j m", j=16), r=[bz])
                    elif ct < 12:
                        sg, bs = sig[(ct - 8) % 2]
                        P.tt("dve", H[:, ct - 8, 30 + blk * 512:30 + (blk + 1) * 512], pm[:], sg[:], ALU.mult,
                             r=[bp, bs], w=[bH[ct - 8]])
                    elif ct < 16:
                        sg, bs = sig[(ct - 12) % 2]
                        P.act(sg[:], pm[:], AF.Sigmoid, r=[bp], w=[bs])
                    else:
                        z, bz = zst[zi % 4]
                        zi += 1
                        P.act(z[:], pm[:], AF.Silu, r=[bp], w=[bz])
                        P.dma("sp", zb_d[ct - 16][:, blk * 512:(blk + 1) * 512], z[:], r=[bz])
            if dbg is not None and dbg.get("stage") == "A":
                P.dma("sp", dbg["out"][0:128, 0:L], U[:, 0].rearrange("p j m -> p (j m)"), r=[bU[0]])
                P.dma("sp", dbg["out"][128:256, 0:L], H[:, 0, 30:30 + L], r=[bH[0]])
            P.barrier()
        if dbg is not None and dbg.get("stage") == "A":
            return

        G = sb(nc, es, "G", [128, 4, 16, 256], BF16)
        bG = [Buf("G%d" % t) for t in range(4)]
        with ExitStack() as e5:
            S5 = s5_setup(C, ins, e5)
            s5_main(C, S5, U, bU, G, bG, e5, dbg)
            P.barrier()
        if dbg is not None and dbg.get("stage") == "S5":
            return
        ycat_d = nc.dram_tensor("ycat_d", [8, 128, L], BF16, kind="Internal").ap()
        with ExitStack() as eg:
            glu_stage(C, ins, G, bG, za_d, ycat_d, eg)
            P.barrier()
        with ExitStack() as ec:
            conv_stage(C, ins, H, bH, zb_d, ycat_d, ec)
            P.barrier()
        with ExitStack() as eo:
            outproj_stage(C, ins["e_w_out"][0], ycat_d, x_ap, h1_ap, eo)
            P.barrier()
        if dbg is not None and dbg.get("stage") == "L0":
            return


NIT = 18
cQ, cZ, cK, cQI, cKI, cV, cWI, W1C = 0, 1024, 2048, 2560, 3072, 3200, 3456, 3464
AX = mybir.AxisListType


def rope_tables(C, es, cosT, sinT, bT):
    nc, P = C.nc, C.P
    b = Buf("ropesetup")
    I32 = mybir.dt.int32

    def t(name, shape, dt=F32):
        return sb(nc, es, "rp_" + name, shape, dt)

    pidx_i, tmp_i = t("pidx_i", [128, 1], I32), t("tmp_i", [128, 1], I32)
    P.op("pool", lambda e: e.iota(pidx_i[:], pattern=[[0, 1]], base=0, channel_multiplier=1), (), [b])
    invp, m16, sg, f = t("invp", [128, 1]), t("m16", [128, 1]), t("sg", [128, 1]), t("f", [128, 1])
    P.ts("dve", tmp_i[:], pidx_i[:], 7, None, ALU.bitwise_and, r=[b], w=[b])
    P.cp("dve", f[:], tmp_i[:], r=[b], w=[b])
    P.act(invp[:], f[:], AF.Exp, scale=-math.log(500000.0) / 8.0, r=[b], w=[b])
    P.ts("dve", tmp_i[:], pidx_i[:], 48, None, ALU.bitwise_and, r=[b], w=[b])
    P.cp("dve", f[:], tmp_i[:], r=[b], w=[b])
    P.ts("dve", m16[:], f[:], 0.5, None, ALU.is_lt, r=[b], w=[b])
    P.tt("dve", invp[:], invp[:], m16[:], ALU.mult, r=[b], w=[b])
    P.ts("dve", tmp_i[:], pidx_i[:], 56, None, ALU.bitwise_and, r=[b], w=[b])
    P.cp("dve", f[:], tmp_i[:], r=[b], w=[b])
    P.ts("dve", sg[:], f[:], 0.5, None, ALU.is_lt, r=[b], w=[b])
    P.ts("dve", sg[:], sg[:], -2.0, 1.0, ALU.mult, ALU.add, r=[b], w=[b])
    CH = 1024
    pos_i, pos, ang, ph, kf = t("pos_i", [128, CH], I32), t("pos", [128, CH]), t("ang", [128, CH]), t("ph", [128, CH]), t("kf", [128, CH])
    ki_ = t("ki", [128, CH], I32)
    for c in range(L // CH):
        P.op("pool", lambda e, c=c: e.iota(pos_i[:], pattern=[[1, CH]], base=c * CH, channel_multiplier=0), (), [b])
        P.cp("dve", pos[:], pos_i[:], r=[b], w=[b])
        P.ts("dve", ang[:], pos[:], invp[:], None, ALU.mult, r=[b], w=[b])
        for dst, off in ((sinT, 0.0), (cosT, 0.5 * PI)):
            P.ts("dve", ph[:], ang[:], off, None, ALU.add, r=[b], w=[b])
            P.ts("dve", ki_[:], ph[:], 1.0 / (2 * PI), None, ALU.mult, r=[b], w=[b])
            P.cp("dve", kf[:], ki_[:], r=[b], w=[b])
            P.stt(ph[:], kf[:], -CW1, ph[:], ALU.mult, ALU.add, r=[b], w=[b])
            P.stt(ph[:], kf[:], -CW2, ph[:], ALU.mult, ALU.add, r=[b], w=[b])
            P.ts("dve", kf[:], ph[:], PI, None, ALU.is_gt, r=[b], w=[b])
            P.stt(ph[:], kf[:], -2 * PI, ph[:], ALU.mult, ALU.add, r=[b], w=[b])
            P.ts("dve", ph[:], ph[:], -PI, PI, ALU.max, ALU.min, r=[b], w=[b])
            P.act(ph[:], ph[:], AF.Sin, r=[b], w=[b])
            if dst is sinT:
                P.ts("dve", dst[:, c * CH:(c + 1) * CH], ph[:], sg[:], None, ALU.mult, r=[b], w=[b, bT])
            else:
                P.cp("dve", dst[:, c * CH:(c + 1) * CH], ph[:], r=[b], w=[b, bT])


def layer1(C, x_ap, o_ap, ins):
    nc, P = C.nc, C.P
    with ExitStack() as es:
        KT2 = sb(nc, es, "KT2", [128, 4, L], BF16)
        bKT = Buf("KT2")
        Vtok = sb(nc, es, "Vtok", [128, NT, 4, 66], BF16)
        bV = Buf("Vtok")
        KI2 = sb(nc, es, "KI2", [128, L], BF16)
        bKI = Buf("KI2")
        WI = sb(nc, es, "WI", [128, NT, 8], F32)
        bWI = Buf("WI")
        P.memset("pool", Vtok[:, :, :, 64:65], 1.0, w=[bV])
        q_d = nc.dram_tensor("q_d", [8, 128, L], BF16, kind="Internal").ap()
        z_d = nc.dram_tensor("z_d", [8, 128, L], BF16, kind="Internal").ap()
        qi_d = nc.dram_tensor("qi_d", [4, 128, L], BF16, kind="Internal").ap()
        w_in = ins["o_w_in"][0]

        with ExitStack() as ea:
            cosT = sb(nc, ea, "cosT", [128, L], BF16)
            sinT = sb(nc, ea, "sinT", [128, L], BF16)
            bT = Buf("ropeT")
            with ExitStack() as er:
                rope_tables(C, er, cosT, sinT, bT)
                P.barrier()
            if getattr(C, "stop", None) == "R":
                return
            Pm = sb(nc, ea, "Pm", [128, 128], BF16)
            bPm = Buf("Pm")
            P.memset("pool", Pm[:], 0.0, w=[bPm])
            for base in (0, 64):
                P.cp("dve", Pm[:, base:base + 8], C.ident_b[:, base + 8:base + 16], r=[C.b_ident], w=[bPm])
                P.cp("dve", Pm[:, base + 8:base + 16], C.ident_b[:, base:base + 8], r=[C.b_ident], w=[bPm])
            W = sb(nc, ea, "W1", [128, 8, W1C], BF16)
            bW = Buf("W1")
            g_t = sb(nc, ea, "g1", [128, 8], F32)
            bg = Buf("g1")
            P.dma("sp", g_t[:], ins["norm_g"][1].rearrange("(k p) -> p k", p=128), w=[bg], slow=True)
            stg = sb(nc, ea, "w1st", [128, W1C], F32)
            bst = Buf("w1st")
            for k in range(8):
                rows = w_in[k * 128:(k + 1) * 128, :]
                P.dma("sp", stg[:, 0:2048], rows[:, 0:2048], w=[bst])
                for a in range(2):
                    P.dma("sp", stg[:, cK:cK + 512].rearrange("p (g two d) -> p g two d", two=2, d=64)[:, :, a, :],
                          rows[:, 2048:2304].rearrange("p (g d) -> p g d", d=64), w=[bst])
                    P.dma("sp", stg[:, cKI + 64 * a:cKI + 64 * a + 64], rows[:, 3072:3136], w=[bst])
                P.dma("sp", stg[:, cQI:cQI + 512], rows[:, 2560:3072], w=[bst])
                P.dma("sp", stg[:, cV:cV + 256], rows[:, 2304:2560], w=[bst])
                P.dma("sp", stg[:, cWI:cWI + 8], rows[:, 3136:3144], w=[bst])
                P.ts("dve", W[:, k, 0:1792], stg[:, 0:1792], g_t[:, k:k + 1], None, ALU.mult, r=[bst, bg], w=[bW])
                P.ts("pool", W[:, k, 1792:W1C], stg[:, 1792:W1C], g_t[:, k:k + 1], None, ALU.mult, r=[bst, bg], w=[bW])
            if getattr(C, "stop", None) == "W":
                return
            st = {
                "xs": [(sb(nc, ea, "b_xs%d" % i, [128, D], F32), Buf()) for i in range(2)],
                "ss": [(sb(nc, ea, "b_ss%d" % i, [128, 1], F32), Buf()) for i in range(2)],
                "xn": [(sb(nc, ea, "b_xn%d" % i, [128, D], BF16), Buf()) for i in range(2)],
                "junk": (sb(nc, ea, "b_junk", [128, D], BF16), Buf()),
                "pst": [(ps(nc, ea, "b_pst%d" % i, [128, 8, 128], BF16), PBuf()) for i in range(2)],
                "xs_i": 0, "pst_i": 0,
            }
            hnTs = [(sb(nc, ea, "b_hnT%d" % i, [128, 8, 512], BF16), Buf()) for i in range(2)]
            pmm = [(ps(nc, ea, "b_pmm%d" % i, [128, 512], F32), PBuf()) for i in range(3)]
            prr = [(ps(nc, ea, "b_prr%d" % i, [128, 512], F32), PBuf()) for i in range(2)]
            pvv = (ps(nc, ea, "b_pvv", [128, 512], F32), PBuf())
            xb = [(sb(nc, ea, "b_xb%d" % i, [128, 512], BF16), Buf()) for i in range(2)]
            ta = [(sb(nc, ea, "b_ta%d" % i, [128, 512], F32), Buf()) for i in range(2)]
            tb = [(sb(nc, ea, "b_tb%d" % i, [128, 512], F32), Buf()) for i in range(2)]
            so = [(sb(nc, ea, "b_so%d" % i, [128, 512], BF16), Buf()) for i in range(4)]
            n_mm = n_r = n_so = 0
            tiles = [("q", cQ + 128 * i, i) for i in range(8)] + [("z", cZ + 128 * i, i) for i in range(8)] + \
                    [("k", cK + 128 * i, i) for i in range(4)] + [("qi", cQI + 128 * i, i) for i in range(4)] + \
                    [("ki", cKI, 0)]
            sub = getattr(C, "stop", None)
            nblk = NB
            do_v = True
            if sub is not None and sub.startswith("B1") and len(sub) > 2:
                nblk = 1
                kinds = {"q": ["q"], "z": ["z"], "k": ["k"], "i": ["ki"], "v": []}[sub[2]]
                tiles = [t_ for t_ in tiles if t_[0] in kinds]
                do_v = sub[2] == "v"
            for blk in range(nblk):
                hnT, bh = hnTs[blk % 2]
                rms_tiles_to_hnT(C, lambda tt: x_ap[tt * 128:(tt + 1) * 128, :], hnT, bh, blk, st)
                sl = slice(blk * 512, (blk + 1) * 512)
                for kind, c0, idx in tiles:
                    pm, bp = pmm[n_mm % 3]
                    n_mm += 1
                    for k in range(8):
                        P.mm(pm[:], W[:, k, c0:c0 + 128], hnT[:, k, :], k == 0, k == 7, r=[bW, bh], w=[bp])
                    if kind == "z":
                        s, bs = so[n_so % 4]
                        n_so += 1
                        P.act(s[:], pm[:], AF.Silu, r=[bp], w=[bs])
                        P.dma("sp", z_d[idx][:, sl], s[:], r=[bs])
                        continue
                    x2, bx2 = xb[n_r % 2]
                    pr, bpr = prr[n_r % 2]
                    a, ba = ta[n_r % 2]
                    b2, bb2 = tb[n_r % 2]
                    n_r += 1
                    P.cp("act", x2[:], pm[:], r=[bp], w=[bx2])
                    P.mm(pr[:], Pm[:], x2[:], True, True, r=[bPm, bx2], w=[bpr])
                    P.tt("dve", a[:], pm[:], cosT[:, sl], ALU.mult, r=[bp, bT], w=[ba])
                    P.tt("dve", b2[:], pr[:], sinT[:, sl], ALU.mult, r=[bpr, bT], w=[bb2])
                    if kind == "k":
                        P.tt("pool", KT2[:, idx, sl], a[:], b2[:], ALU.add, r=[ba, bb2], w=[bKT])
                    elif kind == "ki":
                        P.tt("pool", KI2[:, sl], a[:], b2[:], ALU.add, r=[ba, bb2], w=[bKI])
                    else:
                        s, bs = so[n_so % 4]
                        n_so += 1
                        P.tt("pool", s[:], a[:], b2[:], ALU.add, r=[ba, bb2], w=[bs])
                        dst = q_d if kind == "q" else qi_d
                        P.dma("sp", dst[idx][:, sl], s[:], r=[bs])
                for i in range(4 if do_v else 0):
                    tt_ = blk * 4 + i
                    pv, bpv = pvv
                    for k in range(8):
                        P.mm(pv[:, 0:264], hnT[:, k, i * 128:(i + 1) * 128], W[:, k, cV:cV + 264], k == 0, k == 7,
                             r=[bh, bW], w=[bpv])
                    import os
                    vv = os.environ.get("VVAR", "")
                    if vv != "2":
                        P.cp("act", Vtok[:, tt_, :, 0:64], pv[:, 0:256].rearrange("p (g d) -> p g d", d=64), r=[bpv], w=[bV])
                    if vv != "1":
                        P.ts("dve", WI[:, tt_, :], pv[:, 256:264], (8 ** -0.5) * (64 ** -0.5), None, ALU.mult,
                             r=[bpv], w=[bWI])
            P.barrier()

        if getattr(C, "stop", "").startswith("B1"):
            return
        with ExitStack() as eb:
            w_out = ins["o_w_out"][0]
            wo = sb(nc, eb, "wo64", [64, 16, D], BF16)
            bwo = Buf("wo64")
            wst = [(sb(nc, eb, "wo_st%d" % i, [64, 1, D], F32), Buf()) for i in range(2)]
            for hp in range(16):
                s, bs = wst[hp % 2]
                P.dma("sp", s[:], w_out[hp * 64:(hp + 1) * 64, :].rearrange("(h d) c -> d h c", d=64), w=[bs])
                P.cp("dve" if hp % 2 == 0 else "pool", wo[:, hp:hp + 1, :], s[:], r=[bs], w=[bwo])
            fg = sb(nc, eb, "fg", [128, D], F32)
            bfg = Buf("fg")
            P.dma("sp", fg[:], ins["final_g"].partition_broadcast(128), w=[bfg])
            ones = sb(nc, eb, "Esel", [128, 128], F32)
            bon = Buf("Esel")
            P.memset("pool", ones[:], 0.0, w=[bon])
            P.memset("pool", ones[64:65, :], 1.0, w=[bon])
            cpow = sb(nc, eb, "cpow", [128, NIT + 1], F32)
            bcp = Buf("cpow")
            for n in range(NIT + 1):
                P.memset("pool", cpow[:, n:n + 1], 2.0 ** -(n + 1), w=[bcp])
            SC = [(sb(nc, eb, "SC%d" % i, [128, L], F32), Buf()) for i in range(1)]
            Mq = (sb(nc, eb, "Mq", [128, L], BF16), Buf())
            junk = Mq
            MT = [(sb(nc, eb, "MT%d" % i, [128, NT, 128], BF16), Buf()) for i in range(2)]
            qq = [(sb(nc, eb, "qq%d" % i, [128, 8, 128], BF16), Buf()) for i in range(2)]
            zq = [(sb(nc, eb, "zq%d" % i, [64, 16, 128], BF16), Buf()) for i in range(2)]
            qiq = [(sb(nc, eb, "qiq%d" % i, [128, 4, 128], BF16), Buf()) for i in range(2)]
            xs = [(sb(nc, eb, "c_xs%d" % i, [128, D], F32), Buf()) for i in range(2)]
            rl = [(sb(nc, eb, "rl%d" % i, [128, 512], F32), Buf()) for i in range(2)]
            sm = {nm: (sb(nc, eb, "sm_" + nm, [128, 1], F32), Buf()) for nm in ("rmax", "rmin", "w0", "mid", "cnt", "t", "thr")}
            hs = (sb(nc, eb, "hs", [128, NIT + 1], F32), Buf())
            pe_ = [(sb(nc, eb, "pexp%d" % i, [128, 512], BF16), Buf()) for i in range(3)]
            pmk = [(sb(nc, eb, "pmk%d" % i, [128, 512], BF16), Buf()) for i in range(3)]
            osb = [(sb(nc, eb, "osb%d" % i, [65, 512], F32), Buf()) for i in range(2)]
            on_ = [(sb(nc, eb, "on%d" % i, [64, 512], F32), Buf()) for i in range(2)]
            rbc = [(sb(nc, eb, "rbc%d" % i, [64, 512], F32), Buf()) for i in range(2)]
            og = [(sb(nc, eb, "og%d" % i, [64, 16, 128], BF16), Buf()) for i in range(2)]
            ho = [(sb(nc, eb, "c_ho%d" % i, [128, D], F32), Buf()) for i in range(2)]
            ss = [(sb(nc, eb, "c_ss%d" % i, [128, 1], F32), Buf()) for i in range(2)]
            fjunk = (sb(nc, eb, "c_fjunk", [128, D], BF16), Buf())
            psc = [(ps(nc, eb, "psc%d" % i, [128, 512], F32), PBuf()) for i in range(1)]
            ptr = [(ps(nc, eb, "ptr%d" % i, [128, 4, 128], BF16), PBuf()) for i in range(1)]
            pl_ = [(ps(nc, eb, "pl%d" % i, [128, 512], F32), PBuf()) for i in range(4)]
            po_ = [(ps(nc, eb, "po%d" % i, [128, 512], F32), PBuf()) for i in range(1)]
            pop = (ps(nc, eb, "pop", [128, 512], F32), PBuf())
            pbc = (pop[0][0:64, :], pop[1])
            cn = {"sc": 0, "rl": 0, "l": 0, "e": 0, "g": 0}

            def gen_S(qt):
                nk = (qt + 1) * 128
                nkb = (nk + 511) // 512
                tsl = slice(qt * 128, (qt + 1) * 128)
                q_t, bq = qq[qt % 2]
                z_t, bz = zq[qt % 2]
                qi_t, bqi = qiq[qt % 2]
                x_t, bx = xs[qt % 2]
                P.dma("sp", q_t[:], q_d[:, :, tsl].rearrange("t p l -> p t l"), w=[bq])
                P.dma("sp", qi_t[:], qi_d[:, :, tsl].rearrange("t p l -> p t l"), w=[bqi])
                P.dma("sp", z_t[:], z_d.rearrange("t (a d) l -> d (t a) l", a=2)[:, :, tsl], w=[bz])
                P.dma("sp", x_t[:], x_ap[tsl, :], w=[bx])
                sc, bsc = SC[0]
                for hi in range(8):
                    til, half = hi // 2, hi % 2
                    for kb in range(nkb):
                        w_ = min(512, nk - 512 * kb)
                        ksl = slice(512 * kb, 512 * kb + w_)
                        p, bp = psc[cn["sc"] % len(psc)]
                        cn["sc"] += 1
                        P.mm(p[:, 0:w_], qi_t[64 * half:64 * half + 64, til, :], KI2[64 * half:64 * half + 64, ksl],
                             True, True, r=[bqi, bKI], w=[bp])
                        if hi == 0:
                            P.ts("dve", sc[:, ksl], p[:, 0:w_], 0.0, WI[:, qt, 0:1], ALU.max, ALU.mult,
                                 r=[bp, bWI], w=[bsc])
                        else:
                            r_, br = rl[cn["rl"] % 2]
                            cn["rl"] += 1
                            P.act(r_[:, 0:w_], p[:, 0:w_], AF.Relu, r=[bp], w=[br])
                            P.stt(sc[:, ksl], r_[:, 0:w_], WI[:, qt, hi:hi + 1], sc[:, ksl], ALU.mult, ALU.add,
                                  r=[br, bWI, bsc], w=[bsc])
                        yield
                rmax, rmin, w0, mid, cnt, t_, thr = (sm[k_] for k_ in ("rmax", "rmin", "w0", "mid", "cnt", "t", "thr"))
                P.op("dve", lambda e: e.tensor_reduce(rmax[0][:], sc[:, 0:nk], AX.X, ALU.max), [bsc], [rmax[1]])
                P.op("dve", lambda e: e.tensor_reduce(rmin[0][:], sc[:, 0:nk], AX.X, ALU.min), [bsc], [rmin[1]])
                P.memset("pool", sc[0:64, nk - 64:nk], -1e30, w=[bsc])
                P.tt("dve", w0[0][:], rmax[0][:], rmin[0][:], ALU.subtract, r=[rmax[1], rmin[1]], w=[w0[1]])
                P.tt("dve", hs[0][:], cpow[:], w0[0][:].broadcast_to([128, NIT + 1]), ALU.mult, r=[bcp, w0[1]], w=[hs[1]])
                P.tt("dve", mid[0][:], rmin[0][:], hs[0][:, 0:1], ALU.add, r=[rmin[1], hs[1]], w=[mid[1]])
                yield
                for n in range(NIT):
                    P.ts("dve", junk[0][:, 0:nk], sc[:, 0:nk], mid[0][:], 0.0, ALU.is_ge, ALU.add, accum=cnt[0][:],
                         r=[bsc, mid[1]], w=[junk[1], cnt[1]])
                    P.ts("dve", t_[0][:], cnt[0][:], 255.5, hs[0][:, n:n + 1], ALU.is_gt, ALU.mult,
                         r=[cnt[1], hs[1]], w=[t_[1]])
                    P.stt(mid[0][:], t_[0][:], hs[0][:, n + 1:n + 2], mid[0][:], ALU.subtract, ALU.add,
                          r=[t_[1], hs[1], mid[1]], w=[mid[1]])
                    yield
                P.tt("dve", thr[0][:], mid[0][:], hs[0][:, NIT:NIT + 1], ALU.subtract, r=[mid[1], hs[1]], w=[thr[1]])
                P.ts("dve", Mq[0][:, 0:nk], sc[:, 0:nk], thr[0][:], None, ALU.is_ge, r=[bsc, thr[1]], w=[Mq[1]])
                mt, bmt = MT[qt % 2]
                for k0 in range(0, qt + 1, 4):
                    kn = min(4, qt + 1 - k0)
                    pt, bpt = ptr[0]
                    for kk in range(kn):
                        kt = k0 + kk
                        P.tr(pt[:, kk, :], Mq[0][:, kt * 128:(kt + 1) * 128], C.ident_b[:], r=[Mq[1], C.b_ident], w=[bpt])
                    P.cp("act", mt[:, k0:k0 + kn, :], pt[:, 0:kn, :], r=[bpt], w=[bmt])
                    yield

            def n_S(qt):
                nk = (qt + 1) * 128
                return 8 * ((nk + 511) // 512) + 1 + NIT + (qt + 4) // 4

            def gen_A(qt):
                tsl = slice(qt * 128, (qt + 1) * 128)
                q_t, bq = qq[qt % 2]
                z_t, bz = zq[qt % 2]
                x_t, bx = xs[qt % 2]
                mt, bmt = MT[qt % 2]
                og_t, bog = og[qt % 2]
                for g in range(4):
                    po, bpo = po_[0]
                    for kt in range(qt + 1):
                        pl, bpl = pl_[(2 * cn["l"]) % 4]
                        pl2, bpl2 = pl_[(2 * cn["l"] + 1) % 4]
                        cn["l"] += 1
                        ks = slice(kt * 128, (kt + 1) * 128)
                        P.mm(pl[:, 0:256], KT2[0:64, g, ks], q_t[0:64, 2 * g:2 * g + 2, :], True, True, r=[bKT, bq], w=[bpl])
                        P.mm(pl2[:, 0:256], KT2[64:128, g, ks], q_t[64:128, 2 * g:2 * g + 2, :], True, True, r=[bKT, bq], w=[bpl2])
                        pe, bpe = pe_[cn["e"] % 3]
                        pk, bpk = pmk[cn["e"] % 3]
                        cn["e"] += 1
                        P.act(pe[:, 0:256], pl[:, 0:256], AF.Exp, scale=0.125, r=[bpl], w=[bpe])
                        P.act(pe[:, 256:512], pl2[:, 0:256], AF.Exp, scale=0.125, r=[bpl2], w=[bpe])
                        P.tt("pool", pk[:].rearrange("p (s t) -> p s t", s=4),
                             pe[:].rearrange("p (s t) -> p s t", s=4),
                             mt[:, kt, :].unsqueeze(1).broadcast_to([128, 4, 128]), ALU.mult, r=[bpe, bmt], w=[bpk])
                        P.mm(po[0:65, :], Vtok[:, kt, g, 0:65], pk[:], kt == 0, kt == qt, r=[bV, bpk], w=[bpo])
                        yield
                    o_s, bos = osb[cn["g"] % 2]
                    o_n, bonn = on_[cn["g"] % 2]
                    r_b, brb = rbc[cn["g"] % 2]
                    cn["g"] += 1
                    P.cp("act", o_s[:], po[0:65, :], r=[bpo], w=[bos])
                    P.mm(pop[0][:], ones[0:65, :], o_s[0:65, :], True, True, r=[bon, bos], w=[pop[1]])
                    P.op("dve", lambda e, r_b=r_b: e.reciprocal(r_b[:], pop[0][0:64, :]), [pop[1]], [brb])
                    P.tt("dve", o_n[:], o_s[0:64, :], r_b[:], ALU.mult, r=[bos, brb], w=[bonn])
                    P.tt("pool", og_t[:, 4 * g:4 * g + 4, :].rearrange("p (i1 i0) t -> p i0 i1 t", i0=2),
                         o_n[:].rearrange("p (s1 s0 t) -> p s1 s0 t", s1=2, s0=2),
                         z_t[:, 4 * g:4 * g + 4, :].rearrange("p (i1 i0) t -> p i0 i1 t", i0=2), ALU.mult,
                         r=[bonn, bz], w=[bog])
                    yield
                h, bh = ho[qt % 2]
                for half in range(2):
                    for hh in range(16):
                        P.mm(pop[0][:], og_t[:, hh, :], wo[:, hh, half * 512:(half + 1) * 512], hh == 0, hh == 15,
                             r=[bog, bwo], w=[pop[1]])
                    P.tt("dve", h[:, half * 512:(half + 1) * 512], pop[0][:], x_t[:, half * 512:(half + 1) * 512], ALU.add,
                         r=[pop[1], bx], w=[bh])
                    yield
                s_, bs_ = ss[qt % 2]
                P.act(fjunk[0][:], h[:], AF.Square, accum=s_[:], r=[bh], w=[fjunk[1], bs_])
                P.act(s_[:], s_[:], AF.Sqrt, bias=C.eps_t[:], scale=1.0 / D, r=[bs_, C.b_eps], w=[bs_])
                P.op("dve", lambda e: e.reciprocal(s_[:], s_[:]), [bs_], [bs_])
                P.stt(h[:], h[:], s_[:], fg[:], ALU.mult, ALU.mult, r=[bh, bs_, bfg], w=[bh])
                P.dma("sp", o_ap[tsl, :], h[:], r=[bh])
                yield

            def n_A(qt):
                return 4 * (qt + 1) + 4 + 2 + 1

            for _ in gen_S(0):
                pass
            for qt in range(NT):
                ga = gen_A(qt)
                gs = gen_S(qt + 1) if qt + 1 < NT else None
                na, ns = n_A(qt), (n_S(qt + 1) if gs is not None else 0)
                ia = is_ = 0
                a_done = False
                s_done = gs is None
                while not (a_done and s_done):
                    if not s_done and (a_done or is_ * na <= ia * ns):
                        try:
                            next(gs)
                            is_ += 1
                        except StopIteration:
                            s_done = True
                    else:
                        try:
                            next(ga)
                            ia += 1
                        except StopIteration:
                            a_done = True
            P.barrier()


def build(mode="full"):
    nc = bass.Bass("TRN2", target_bir_lowering=False)
    ins = {}

    def din(name, shape):
        ins[name] = nc.dram_tensor(name, list(shape), F32, kind="ExternalInput").ap()

    need0 = mode in ("full", "L0", "A", "S5")
    need1 = mode in ("full",) + ("L1", "B1", "R", "W", "B1q", "B1z", "B1k", "B1i", "B1v")
    din("x", (L, D))
    din("norm_g", (2, D))
    if need0:
        for nm in L0_NAMES:
            din(nm, SHAPES[nm])
    if need1:
        for nm in L1_NAMES:
            din(nm, SHAPES[nm])
    out = nc.dram_tensor("out", [L, D], F32, kind="ExternalOutput").ap()
    dbg = None
    if mode in ("A", "S5"):
        dbg = {"stage": mode,
               "out": nc.dram_tensor("dbg", [512, L], BF16, kind="ExternalOutput").ap(),
               "out32": nc.dram_tensor("dbg32", [512, L], F32, kind="ExternalOutput").ap()}
    with ExitStack() as es:
        C = Ctx()
        C.nc = nc
        C.es = es
        C.P = Prog(nc, es)
        build_consts(C)
        if mode == "full":
            h1 = nc.dram_tensor("h1_d", [L, D], F32, kind="Internal").ap()
            layer0(C, ins["x"], h1, ins, None)
            layer1(C, h1, out, ins)
        elif mode in ("L1", "B1", "R", "W", "B1q", "B1z", "B1k", "B1i", "B1v"):
            C.stop = mode if mode != "L1" else ""
            layer1(C, ins["x"], out, ins)
        else:
            layer0(C, ins["x"], out, ins, dbg)
        C.P.emit()
    return nc


SHAPES = {"e_w_in": (1, D, E_IN), "e_lam_re": (1, 32, 64), "e_lam_im": (1, 32, 64), "e_log_step": (1, 32),
          "e_b_re": (1, 32, 64, 16), "e_b_im": (1, 32, 64, 16), "e_c_re": (1, 32, 16, 64), "e_c_im": (1, 32, 16, 64),
          "e_d_skip": (1, 512), "e_w_glu": (1, 512, 512), "e_b_glu": (1, 512), "e_conv_w": (1, 31, 512),
          "e_conv_b": (1, 512), "e_ln_g": (1, 512), "e_ln_b": (1, 512), "e_w_out": (1, D, D),
          "o_w_in": (1, D, O_IN), "o_w_out": (1, D, D), "final_g": (D,)}
L0_NAMES = ["e_w_in", "e_lam_re", "e_lam_im", "e_log_step", "e_b_re", "e_b_im", "e_c_re", "e_c_im", "e_d_skip",
            "e_w_glu", "e_b_glu", "e_conv_w", "e_conv_b", "e_ln_g", "e_ln_b", "e_w_out"]
L1_NAMES = ["o_w_in", "o_w_out", "final_g"]


def run(inputs, mode="full", x_override=None, trace=False):
    nc = build(mode)
    names = ["norm_g"]
    if mode in ("full", "L0", "A", "S5"):
        names += L0_NAMES
    if mode in ("full",) + ("L1", "B1", "R", "W", "B1q", "B1z", "B1k", "B1i", "B1v"):
        names += L1_NAMES
    shared = {k: np.ascontiguousarray(np.asarray(inputs[k], dtype=np.float32)) for k in names}
    x = x_override if x_override is not None else inputs["x"]
    in_maps = []
    for c in range(8):
        m = dict(shared)
        m["x"] = np.ascontiguousarray(np.asarray(x[c], dtype=np.float32))
        in_maps.append(m)
    return run_bass_kernel_spmd(nc, in_maps, core_ids=list(range(8)), trace=trace)


MODE = "full"


def kernel(**inputs):
    if MODE == "full":
        res = run(inputs, "full")
        return np.stack([np.asarray(r["out"]) for r in res.results], axis=0).astype(np.float32)
    r0 = run(inputs, "L0")
    h1 = [np.asarray(r["out"]) for r in r0.results]
    r1 = run(inputs, "L1", x_override=h1)
    return np.stack([np.asarray(r["out"]) for r in r1.results], axis=0).astype(np.float32)
```

```python
import math
from contextlib import ExitStack

import numpy as np
import concourse.bass as bass
import concourse.mybir as mybir
from concourse.bass_utils import run_bass_kernel_spmd

F32 = mybir.dt.float32
BF16 = mybir.dt.bfloat16
ALU = mybir.AluOpType
AF = mybir.ActivationFunctionType

L = 4096
D = 1024
NT = L // 128
NB = L // 512
EPS = 1e-6
E_IN = 2560
O_IN = 3144

ENGS = ["pe", "act", "dve", "pool", "sp"]
EPOCH_LEN = 3000


class Buf:
    __slots__ = ("lw", "rd", "name")

    def __init__(self, name=""):
        self.lw = None
        self.rd = {}
        self.name = name


class PBuf(Buf):
    __slots__ = ()


class Prog:
    def __init__(self, nc, es, n_dma_sems=24):
        self.nc = nc
        self.es = es
        self.sem = {}
        self.epoch = {e: 0 for e in ENGS}
        self.cnt = {e: 0 for e in ENGS}
        for e in ENGS:
            self.sem[e + "#0"] = es.enter_context(nc.semaphore("s_%s_0" % e))
        self.ops = {e: [] for e in ENGS}
        self.known = {e: {} for e in ENGS}
        self.pending = {e: {} for e in ENGS}
        self.dsem = [es.enter_context(nc.semaphore("d%d" % i)) for i in range(n_dma_sems)]
        self.dval = [0] * n_dma_sems
        self.dnext = 0

    def semh(self, key):
        if isinstance(key, tuple):
            return self.dsem[key[1]]
        return self.sem[key]

    def op(self, E, fn, r=(), w=(), dma=False):
        waits = {}
        known = self.known[E]

        def need(k, v):
            if E == "pe" and isinstance(k, str) and k.startswith("pe#"):
                return
            if known.get(k, 0) >= v:
                return
            if waits.get(k, 0) < v:
                waits[k] = v

        for k, v in self.pending[E].items():
            need(k, v)
        self.pending[E] = {}
        for b in r:
            if b.lw is not None:
                need(*b.lw)
            if isinstance(b, PBuf):
                for k, v in b.rd.items():
                    if not (isinstance(k, str) and k.split("#")[0] == E):
                        need(k, v)
        for b in w:
            if b.lw is not None:
                need(*b.lw)
            for k, v in b.rd.items():
                need(k, v)
        if dma:
            i = self.dnext
            self.dnext = (i + 1) % len(self.dsem)
            if self.dval[i] > 0:
                need(("d", i), self.dval[i])
            self.dval[i] += 16
            tok = (("d", i), self.dval[i])
            inc = 16
        else:
            if self.cnt[E] >= EPOCH_LEN:
                self.epoch[E] += 1
                self.cnt[E] = 0
                self.sem["%s#%d" % (E, self.epoch[E])] = self.es.enter_context(
                    self.nc.semaphore("s_%s_%d" % (E, self.epoch[E])))
            self.cnt[E] += 1
            tok = ("%s#%d" % (E, self.epoch[E]), self.cnt[E])
            inc = 1
        for k, v in waits.items():
            known[k] = v
        self.ops[E].append((list(waits.items()), fn, tok[0], inc))
        for b in r:
            if b.rd.get(tok[0], 0) < tok[1]:
                b.rd[tok[0]] = tok[1]
        for b in w:
            b.lw = tok
            b.rd = {}
        return tok

    def barrier(self):
        snap = {"%s#%d" % (e, self.epoch[e]): self.cnt[e] for e in ENGS if self.cnt[e] > 0}
        for i, v in enumerate(self.dval):
            if v > 0:
                snap[("d", i)] = v
        for e in ENGS:
            p = self.pending[e]
            for k, v in snap.items():
                if p.get(k, 0) < v:
                    p[k] = v

    def emit(self):
        self.barrier()
        final = {e: [(k, v) for k, v in self.pending[e].items()
                     if not (e == "pe" and isinstance(k, str) and k.startswith("pe#"))
                     and self.known[e].get(k, 0) < v] for e in ENGS}
        nc = self.nc
        with nc.Block() as block:
            def mk(E):
                def body(eng):
                    for waits, fn, key, inc in self.ops[E]:
                        for k, v in waits:
                            eng.wait_ge(self.semh(k), v)
                        ins = fn(eng)
                        ins.then_inc(self.semh(key), inc)
                    for k, v in final[E]:
                        eng.wait_ge(self.semh(k), v)
                return body
            block.tensor(mk("pe"))
            block.scalar(mk("act"))
            block.vector(mk("dve"))
            block.gpsimd(mk("pool"))
            block.sync(mk("sp"))

    def mm(self, out, lhsT, rhs, start, stop, r=(), w=()):
        return self.op("pe", lambda e: e.matmul(out, lhsT, rhs, start=start, stop=stop), r, w)

    def tr(self, out, in_, ident, r=(), w=()):
        return self.op("pe", lambda e: e.transpose(out, in_, ident), r, w)

    def act(self, out, in_, func, bias=None, scale=None, accum=None, r=(), w=()):
        kw = {}
        if bias is not None:
            kw["bias"] = bias
        if scale is not None:
            kw["scale"] = scale
        if accum is not None:
            kw["accum_out"] = accum
        return self.op("act", lambda e: e.activation(out, in_, func, **kw), r, w)

    def ts(self, E, out, in0, s1, s2, op0, op1=None, accum=None, r=(), w=()):
        kw = {}
        if op1 is not None:
            kw["op1"] = op1
        if accum is not None:
            kw["accum_out"] = accum
        return self.op(E, lambda e: e.tensor_scalar(out, in0, s1, s2, op0, **kw), r, w)

    def tt(self, E, out, in0, in1, op, r=(), w=()):
        return self.op(E, lambda e: e.tensor_tensor(out, in0, in1, op), r, w)

    def stt(self, out, in0, scalar, in1, op0, op1, r=(), w=()):
        return self.op("dve", lambda e: e.scalar_tensor_tensor(out, in0, scalar, in1, op0, op1), r, w)

    def cp(self, E, out, in_, r=(), w=()):
        if E == "act":
            return self.op("act", lambda e: e.copy(out, in_), r, w)
        return self.op(E, lambda e: e.tensor_copy(out, in_), r, w)

    def memset(self, E, ap, val, w=()):
        return self.op(E, lambda e: e.memset(ap, val), (), w)

    def dma(self, E, out, in_, r=(), w=(), slow=False):
        if slow:
            return self.op(E, lambda e: e.dma_start(out=out, in_=in_, allow_slow_non_contiguous=True), r, w, dma=True)
        return self.op(E, lambda e: e.dma_start(out=out, in_=in_), r, w, dma=True)


class Ctx:
    pass


def sb(nc, es, name, shape, dt):
    return es.enter_context(nc.sbuf_tensor(name, shape, dt))


def ps(nc, es, name, shape, dt):
    return es.enter_context(nc.psum_tensor(name, shape, dt))


def build_consts(C):
    nc, P, es = C.nc, C.P, C.es
    C.ident_f = sb(nc, es, "ident_f", [128, 128], F32)
    C.ident_b = sb(nc, es, "ident_b", [128, 128], BF16)
    C.b_ident = Buf("ident")
    P.memset("pool", C.ident_f[:], 0.0, w=[C.b_ident])
    P.op("pool", lambda e: e.affine_select(out=C.ident_f[:], in_=C.ident_f[:], pattern=[[-1, 128]],
                                           compare_op=ALU.not_equal, fill=1.0, base=0,
                                           channel_multiplier=1), [C.b_ident], [C.b_ident])
    P.cp("pool", C.ident_b[:], C.ident_f[:], r=[C.b_ident], w=[C.b_ident])
    C.eps_t = sb(nc, es, "eps_t", [128, 1], F32)
    C.b_eps = Buf("eps")
    P.memset("pool", C.eps_t[:], EPS, w=[C.b_eps])


def rms_tiles_to_hnT(C, src_ap_fn, hnT, hnT_bufs, blk, st):
    nc, P = C.nc, C.P
    for i in range(4):
        tt = blk * 4 + i
        s = st["xs_i"] % len(st["xs"])
        st["xs_i"] += 1
        xt, bx = st["xs"][s]
        P.dma("sp", xt[:], src_ap_fn(tt), w=[bx])
        ss, bss = st["ss"][s]
        junk, bj = st["junk"]
        P.act(junk[:], xt[:], AF.Square, accum=ss[:], r=[bx], w=[bj, bss])
        P.act(ss[:], ss[:], AF.Sqrt, bias=C.eps_t[:], scale=1.0 / D, r=[bss, C.b_eps], w=[bss])
        P.op("dve", lambda e, ss=ss: e.reciprocal(ss[:], ss[:]), [bss], [bss])
        xn, bxn = st["xn"][s % len(st["xn"])]
        P.ts("dve", xn[:], xt[:], ss[:], None, ALU.mult, r=[bx, bss], w=[bxn])
        pt, bpt = st["pst"][st["pst_i"] % len(st["pst"])]
        st["pst_i"] += 1
        for k in range(8):
            P.tr(pt[:, k, :], xn[:, k * 128:(k + 1) * 128], C.ident_b[:], r=[bxn, C.b_ident], w=[bpt])
        eng = "act" if (i % 2 == 0) else "dve"
        P.cp(eng, hnT[:, :, i * 128:(i + 1) * 128], pt[:], r=[bpt], w=[hnT_bufs])


def load_weight_bf16(C, wdst, bw, wsrc, ncols, g_t, bg, stage, col0=0, colmap=None):
    P = C.P
    for k in range(8):
        stg, bs = stage[k % len(stage)]
        P.dma("sp", stg[:, 0:ncols], wsrc[k * 128:(k + 1) * 128, :], w=[bs])
        eng = "dve" if k % 2 == 0 else "pool"
        P.ts(eng, wdst[:, k, col0:col0 + ncols], stg[:, 0:ncols], g_t[:, k:k + 1], None, ALU.mult,
             r=[bs, bg], w=[bw])


PW = [1] + [16 * (2 ** i) for i in range(8)]
PI = math.pi
CW1 = 6.28125
CW2 = 2 * math.pi - 6.28125


def s5_setup(C, ins, es):
    nc, P = C.nc, C.P
    S = Ctx()
    bS = Buf("s5setup")
    S.b = bS

    def t(name, shape, dt=F32):
        return sb(nc, es, "s5_" + name, shape, dt)

    lr, li, dt_ = t("lr", [128, 32]), t("li", [128, 32]), t("dt", [128, 32])
    for half in range(2):
        P.dma("sp", lr[half * 64:(half + 1) * 64, :], ins["e_lam_re"][0].rearrange("g n -> n g"), w=[bS], slow=True)
        P.dma("sp", li[half * 64:(half + 1) * 64, :], ins["e_lam_im"][0].rearrange("g n -> n g"), w=[bS], slow=True)
    P.dma("sp", dt_[:], ins["e_log_step"][0].partition_broadcast(128), w=[bS])
    bre, bim = t("bre", [128, 32, 16]), t("bim", [128, 32, 16])
    for half in range(2):
        P.dma("sp", bre[half * 64:(half + 1) * 64], ins["e_b_re"][0].rearrange("g n c -> n g c"), w=[bS])
        P.dma("sp", bim[half * 64:(half + 1) * 64], ins["e_b_im"][0].rearrange("g n c -> n g c"), w=[bS])
    cnat = t("cnat", [128, 4, 128])
    P.dma("sp", cnat[:, :, 0:64], ins["e_c_re"][0].rearrange("(t q) c n -> (q c) t n", q=8), w=[bS])
    P.dma("sp", cnat[:, :, 64:128], ins["e_c_im"][0].rearrange("(t q) c n -> (q c) t n", q=8), w=[bS])
    S.dskip = t("dskip", [128, 4])
    P.dma("sp", S.dskip[:], ins["e_d_skip"][0].rearrange("(t p) -> p t", p=128), w=[bS], slow=True)

    pidx_i = t("pidx_i", [128, 1], mybir.dt.int32)
    pidx = t("pidx", [128, 1])
    P.op("pool", lambda e: e.iota(pidx_i[:], pattern=[[0, 1]], base=0, channel_multiplier=1), (), [bS])
    P.cp("dve", pidx[:], pidx_i[:], r=[bS], w=[bS])
    mlo, mhi, sgn = t("mlo", [128, 1]), t("mhi", [128, 1]), t("sgn", [128, 1])
    P.ts("dve", mlo[:], pidx[:], 64.0, None, ALU.is_lt, r=[bS], w=[bS])
    P.ts("dve", mhi[:], mlo[:], -1.0, 1.0, ALU.mult, ALU.add, r=[bS], w=[bS])
    P.ts("dve", sgn[:], mlo[:], 2.0, -1.0, ALU.mult, ALU.add, r=[bS], w=[bS])
    mpar = t("mpar", [128, 4])
    pm64 = t("pm64", [128, 1])
    mtmp = t("mtmp", [128, 1])
    P.stt(pm64[:], mhi[:], -64.0, pidx[:], ALU.mult, ALU.add, r=[bS], w=[bS])
    for r4 in range(4):
        P.ts("dve", mtmp[:], pm64[:], 16.0 * r4 - 0.5, None, ALU.is_gt, r=[bS], w=[bS])
        P.ts("dve", mpar[:, r4:r4 + 1], pm64[:], 16.0 * r4 + 15.5, mtmp[:], ALU.is_lt, ALU.mult, r=[bS], w=[bS])
    negpi = t("negpi", [128, 1])
    P.memset("dve", negpi[:], -PI, w=[bS])
    S.sgn = sgn

    P.ts("dve", lr[:], lr[:], -1e-4, None, ALU.min, r=[bS], w=[bS])
    P.act(dt_[:], dt_[:], AF.Exp, r=[bS], w=[bS])
    lrdt, lidt = t("lrdt", [128, 32]), t("lidt", [128, 32])
    P.tt("dve", lrdt[:], lr[:], dt_[:], ALU.mult, r=[bS], w=[bS])
    P.tt("dve", lidt[:], li[:], dt_[:], ALU.mult, r=[bS], w=[bS])
    npw = len(PW)
    S.vr, S.vi = t("vr", [128, npw, 32]), t("vi", [128, npw, 32])
    mag, sn, cs, ph = t("mag", [128, 32]), t("sn", [128, 32]), t("cs", [128, 32]), t("ph", [128, 32])
    ai1 = t("ai1", [128, 32])
    ki_ = t("ki_", [128, 32], mybir.dt.int32)
    kf = t("kf", [128, 32])
    for ip, p in enumerate(PW):
        P.act(mag[:], lrdt[:], AF.Exp, scale=float(p), r=[bS], w=[bS])
        for dst, off in ((sn, 0.0), (cs, 0.5 * PI)):
            P.ts("dve", ph[:], lidt[:], float(p), off, ALU.mult, ALU.add, r=[bS], w=[bS])
            P.ts("dve", ki_[:], ph[:], 1.0 / (2 * PI), None, ALU.mult, r=[bS], w=[bS])
            P.cp("dve", kf[:], ki_[:], r=[bS], w=[bS])
            P.stt(ph[:], kf[:], -CW1, ph[:], ALU.mult, ALU.add, r=[bS], w=[bS])
            P.stt(ph[:], kf[:], -CW2, ph[:], ALU.mult, ALU.add, r=[bS], w=[bS])
            P.ts("dve", kf[:], ph[:], PI, None, ALU.is_gt, r=[bS], w=[bS])
            P.stt(ph[:], kf[:], -2 * PI, ph[:], ALU.mult, ALU.add, r=[bS], w=[bS])
            P.ts("dve", ph[:], ph[:], -PI, PI, ALU.max, ALU.min, r=[bS], w=[bS])
            P.act(dst[:], ph[:], AF.Sin, r=[bS], w=[bS])
        P.tt("dve", S.vr[:, ip, :], mag[:], cs[:], ALU.mult, r=[bS], w=[bS])
        if ip == 0:
            P.tt("dve", ai1[:], mag[:], sn[:], ALU.mult, r=[bS], w=[bS])
        P.stt(S.vi[:, ip, :], sn[:], sgn[:], mag[:], ALU.mult, ALU.mult, r=[bS], w=[bS])
    nr, den, kr, ki, tmp = t("nr", [128, 32]), t("den", [128, 32]), t("kr", [128, 32]), t("ki", [128, 32]), t("tmp", [128, 32])
    P.ts("dve", nr[:], S.vr[:, 0, :], -1.0, None, ALU.add, r=[bS], w=[bS])
    P.tt("dve", den[:], lr[:], lr[:], ALU.mult, r=[bS], w=[bS])
    P.tt("dve", tmp[:], li[:], li[:], ALU.mult, r=[bS], w=[bS])
    P.tt("dve", den[:], den[:], tmp[:], ALU.add, r=[bS], w=[bS])
    P.op("dve", lambda e: e.reciprocal(den[:], den[:]), [bS], [bS])
    P.tt("dve", kr[:], nr[:], lr[:], ALU.mult, r=[bS], w=[bS])
    P.tt("dve", tmp[:], ai1[:], li[:], ALU.mult, r=[bS], w=[bS])
    P.tt("dve", kr[:], kr[:], tmp[:], ALU.add, r=[bS], w=[bS])
    P.tt("dve", kr[:], kr[:], den[:], ALU.mult, r=[bS], w=[bS])
    P.tt("dve", ki[:], ai1[:], lr[:], ALU.mult, r=[bS], w=[bS])
    P.tt("dve", tmp[:], nr[:], li[:], ALU.mult, r=[bS], w=[bS])
    P.tt("dve", ki[:], ki[:], tmp[:], ALU.subtract, r=[bS], w=[bS])
    P.tt("dve", ki[:], ki[:], den[:], ALU.mult, r=[bS], w=[bS])
    KA, KB = t("KA", [128, 32]), t("KB", [128, 32])
    P.ts("dve", KA[:], kr[:], mlo[:], None, ALU.mult, r=[bS], w=[bS])
    P.stt(KA[:], ki[:], mhi[:], KA[:], ALU.mult, ALU.add, r=[bS], w=[bS])
    P.ts("dve", KB[:], kr[:], mhi[:], None, ALU.mult, r=[bS], w=[bS])
    P.ts("dve", tmp[:], ki[:], mlo[:], None, ALU.mult, r=[bS], w=[bS])
    P.tt("dve", KB[:], KB[:], tmp[:], ALU.subtract, r=[bS], w=[bS])
    bbar, btmp = t("bbar", [128, 32, 16]), t("btmp", [128, 32, 16])
    P.tt("dve", bbar[:], bre[:], KA[:].unsqueeze(2).broadcast_to([128, 32, 16]), ALU.mult, r=[bS], w=[bS])
    P.tt("dve", btmp[:], bim[:], KB[:].unsqueeze(2).broadcast_to([128, 32, 16]), ALU.mult, r=[bS], w=[bS])
    P.tt("dve", bbar[:], bbar[:], btmp[:], ALU.add, r=[bS], w=[bS])
    S.BT = t("BT", [128, 4, 4, 128], BF16)
    S.Cpad = t("Cpad", [128, 32, 128], BF16)
    P.memset("pool", S.Cpad[:], 0.0, w=[bS])
    pst = ps(nc, es, "s5_pst", [128, 128], F32)
    CSt = t("CSt", [128, 128])
    for T in range(4):
        P.tr(pst[:], bbar[:, 8 * T:8 * T + 8, :].rearrange("p g c -> p (g c)"), C.ident_f[:], r=[bS, C.b_ident], w=[bS])
        for par in range(4):
            P.ts("dve", S.BT[:, T, par, :], pst[:], mpar[:, par:par + 1], None, ALU.mult, r=[bS], w=[bS])
        P.tr(pst[:], cnat[:, T, :], C.ident_f[:], r=[bS, C.b_ident], w=[bS])
        P.ts("dve", CSt[:], pst[:], sgn[:], None, ALU.mult, r=[bS], w=[bS])
        for q in range(8):
            P.cp("dve", S.Cpad[:, 8 * T + q, 16 * q:16 * q + 16], CSt[:, 16 * q:16 * q + 16], r=[bS], w=[bS])
    S.J = t("J", [128, 128])
    P.cp("dve", S.J[:, 64:128], C.ident_f[:, 0:64], r=[C.b_ident], w=[bS])
    P.cp("dve", S.J[:, 0:64], C.ident_f[:, 64:128], r=[C.b_ident], w=[bS])
    return S


def s5_main(C, S, U, bU, G, bG, es, dbg):
    nc, P = C.nc, C.P
    bS = S.b
    pstep = [(ps(nc, es, "s5_pp%d" % i, [128, 256], F32), PBuf()) for i in range(5)]
    py = [(ps(nc, es, "s5_py%d" % i, [128, 256], F32), PBuf()) for i in range(2)]
    Sst = [[(sb(nc, es, "s5_S%d_%d" % (q, i), [128, 256], BF16), Buf()) for i in range(2)] for q in range(8)]
    Epp = [[(sb(nc, es, "s5_E%d_%d" % (q, i), [128, 256], F32), Buf()) for i in range(2)] for q in range(8)]
    Eprev = [(sb(nc, es, "s5_Ep%d" % q, [128, 256], BF16), Buf()) for q in range(8)]
    A1 = [(sb(nc, es, "s5_A1_%d" % q, [128, 2, 128], BF16), Buf()) for q in range(8)]
    Af = [(sb(nc, es, "s5_Af%d" % i, [128, 128], F32), Buf()) for i in range(4)]
    Atmp = (sb(nc, es, "s5_Atmp", [128, 128], F32), Buf())
    Ahi32 = (sb(nc, es, "s5_Ahi32", [128, 128], F32), Buf())
    yat = [(sb(nc, es, "s5_ya%d" % i, [128, 256], F32), Buf()) for i in range(2)]
    ppi = 0
    afi = 0
    yi = 0

    def gen_A(dst, bd, ip, g, eng):
        P.ts(eng, dst[:], C.ident_f[:], S.vr[:, ip, g:g + 1], None, ALU.mult, r=[C.b_ident, bS], w=[bd])
        P.stt(dst[:], S.J[:], S.vi[:, ip, g:g + 1], dst[:], ALU.mult, ALU.add, r=[bS, bd], w=[bd])

    for T in range(4):
        for q in range(8):
            g = 8 * T + q
            a1, ba = A1[q]
            gen_A(Atmp[0], Atmp[1], 0, g, "dve")
            P.cp("dve", a1[:, 0, :], Atmp[0][:], r=[Atmp[1]], w=[ba])
            P.cp("dve", Ahi32[0][:], a1[:, 0, :], r=[ba], w=[Ahi32[1]])
            P.tt("dve", a1[:, 1, :], Atmp[0][:], Ahi32[0][:], ALU.subtract, r=[Atmp[1], Ahi32[1]], w=[ba])
        for pas in range(2):
            for j in range(16):
                for q in range(8):
                    g = 8 * T + q
                    pr = q // 4
                    pp, bp = pstep[ppi % len(pstep)]
                    ppi += 1
                    a1, ba = A1[q]
                    sprev = None
                    if j > 0:
                        sprev = Sst[q][(j - 1) % 2]
                    elif pas == 1:
                        sprev = Eprev[q]
                    P.mm(pp[:], S.BT[64 * pr:64 * pr + 64, T, q % 4, :], U[64 * pr:64 * pr + 64, T, j, :],
                         True, sprev is None, r=[bS, bU[T]], w=[bp])
                    if sprev is not None:
                        P.mm(pp[:], a1[:, 0, :], sprev[0][:], False, False, r=[ba, sprev[1]], w=[bp])
                        P.mm(pp[:], a1[:, 1, :], sprev[0][:], False, True, r=[ba, sprev[1]], w=[bp])
                    sc, bs = Sst[q][j % 2]
                    if pas == 0 and j == 15:
                        P.cp("act", Epp[q][0][0][:], pp[:], r=[bp], w=[Epp[q][0][1]])
                    else:
                        eng = "act" if (q % 2 == 0) else "dve"
                        P.cp(eng, sc[:], pp[:], r=[bp], w=[bs])
                if pas == 1:
                    pyt, bpy = py[yi % 2]
                    ya, bya = yat[yi % 2]
                    yi += 1
                    for q in range(8):
                        sc, bs = Sst[q][j % 2]
                        P.mm(pyt[:], S.Cpad[:, 8 * T + q, :], sc[:], q == 0, q == 7, r=[bS, bs], w=[bpy])
                    P.stt(ya[:], U[:, T, j, :], S.dskip[:, T:T + 1], pyt[:], ALU.mult, ALU.add,
                          r=[bU[T], bS, bpy], w=[bya])
                    P.act(G[:, T, j, :], ya[:], AF.Gelu, r=[bya], w=[bG[T]])
                    if dbg is not None and dbg.get("stage") == "S5" and T == 0:
                        P.dma("sp", dbg["out32"][0:128, j * 256:(j + 1) * 256], ya[:], r=[bya])
            if pas == 0:
                for q in range(8):
                    g = 8 * T + q
                    cur = 0
                    for i in range(8):
                        d = 2 ** i
                        am, bam = Af[afi % len(Af)]
                        afi += 1
                        gen_A(am, bam, 1 + i, g, "pool" if False else "dve")
                        Ec, bEc = Epp[q][cur]
                        En, bEn = Epp[q][1 - cur]
                        pp, bp = pstep[ppi % len(pstep)]
                        ppi += 1
                        P.mm(pp[:, 0:256 - d], am[:], Ec[:, 0:256 - d], True, True, r=[bam, bEc], w=[bp])
                        P.tt("dve", En[:, d:256], Ec[:, d:256], pp[:, 0:256 - d], ALU.add, r=[bEc, bp], w=[bEn])
                        P.cp("act", En[:, 0:d], Ec[:, 0:d], r=[bEc], w=[bEn])
                        cur = 1 - cur
                    Ef, bEf = Epp[q][cur]
                    ep, bep = Eprev[q]
                    P.memset("pool", ep[:, 0:1], 0.0, w=[bep])
                    P.cp("act", ep[:, 1:256], Ef[:, 0:255], r=[bEf], w=[bep])


def load_cast(C, wdst, bw, wsrc_fn, nk, ncols, stage):
    P = C.P
    for k in range(nk):
        stg, bs = stage[k % len(stage)]
        P.dma("sp", stg[:, 0:ncols], wsrc_fn(k), w=[bs])
        P.cp("dve" if k % 2 == 0 else "pool", wdst[:, k, 0:ncols], stg[:, 0:ncols], r=[bs], w=[bw])


def glu_stage(C, ins, G, bG, za_d, ycat_d, es):
    nc, P = C.nc, C.P
    wg = sb(nc, es, "wglu", [128, 4, 512], BF16)
    bwg = Buf()
    stage = [(sb(nc, es, "gst%d" % i, [128, 512], F32), Buf()) for i in range(2)]
    load_cast(C, wg, bwg, lambda k: ins["e_w_glu"][0][k * 128:(k + 1) * 128, :], 4, 512, stage)
    bgl = sb(nc, es, "bglu", [128, 4], F32)
    bb = Buf()
    P.dma("sp", bgl[:], ins["e_b_glu"][0].rearrange("(t p) -> p t", p=128), w=[bb], slow=True)
    YAn = sb(nc, es, "YAn", [128, 4, L], BF16)
    bY = [Buf() for _ in range(4)]
    pg = [(ps(nc, es, "pglu%d" % i, [128, 512], F32), PBuf()) for i in range(3)]
    sg = [(sb(nc, es, "gsig%d" % i, [128, 512], F32), Buf()) for i in range(2)]
    zt = [(sb(nc, es, "gza%d" % i, [128, 512], BF16), Buf()) for i in range(3)]
    tm = [(sb(nc, es, "gtm%d" % i, [128, 512], F32), Buf()) for i in range(2)]
    n = 0
    for b in range(8):
        for T in range(4):
            p, bp = pg[n % 3]
            s, bs = sg[n % 2]
            z, bz = zt[n % 3]
            t, bt = tm[n % 2]
            n += 1
            P.dma("sp", z[:], za_d[T][:, b * 512:(b + 1) * 512], w=[bz])
            for kc in range(4):
                P.mm(p[:], wg[:, kc, T * 128:(T + 1) * 128],
                     G[:, kc, 2 * b:2 * b + 2, :].rearrange("p j m -> p (j m)"), kc == 0, kc == 3,
                     r=[bwg, bG[kc]], w=[bp])
            P.act(s[:], p[:], AF.Sigmoid, bias=bgl[:, T:T + 1], r=[bp, bb], w=[bs])
            P.tt("dve", t[:], G[:, T, 2 * b:2 * b + 2, :].rearrange("p j m -> p (j m)"), s[:], ALU.mult,
                 r=[bG[T], bs], w=[bt])
            P.tt("pool", YAn[:, T].rearrange("p (m j) -> p j m", j=16)[:, 2 * b:2 * b + 2, :],
                 t[:].rearrange("p (j m) -> p j m", j=2), z[:].rearrange("p (j m) -> p j m", j=2), ALU.mult,
                 r=[bt, bz], w=[bY[T]])
    for T in range(4):
        P.dma("sp", ycat_d[T], YAn[:, T], r=[bY[T]])


def conv_stage(C, ins, H, bH, zb_d, ycat_d, es):
    nc, P = C.nc, C.P
    bc = Buf("convsetup")
    cwn = sb(nc, es, "cwn", [31, 512], F32)
    P.dma("sp", cwn[:], ins["e_conv_w"][0], w=[bc])
    cw = sb(nc, es, "cw", [128, 4, 31], F32)
    pcw = ps(nc, es, "pcw", [128, 31], F32)
    for T in range(4):
        P.tr(pcw[:], cwn[0:31, T * 128:(T + 1) * 128], C.ident_f[0:31, 0:31], r=[bc, C.b_ident], w=[bc])
        P.cp("dve", cw[:, T, :], pcw[:], r=[bc], w=[bc])
    vecs = {}
    for nm in ("e_conv_b", "e_ln_g", "e_ln_b"):
        v = sb(nc, es, "cv_" + nm, [128, 4], F32)
        P.dma("sp", v[:], ins[nm][0].rearrange("(t p) -> p t", p=128), w=[bc], slow=True)
        vecs[nm] = v
    Dg = sb(nc, es, "Dg", [128, 4, 31, 128], BF16)
    for T in range(4):
        for k in range(31):
            P.ts("dve" if (k % 2 == 0) else "pool", Dg[:, T, k, :], C.ident_b[:], cw[:, T, k:k + 1], None, ALU.mult,
                 r=[bc, C.b_ident], w=[bc])
    ones = sb(nc, es, "ones_f", [128, 128], F32)
    P.memset("pool", ones[:], 1.0, w=[bc])
    pc = [(ps(nc, es, "pconv%d" % i, [128, 512], F32), PBuf()) for i in range(3)]
    pmu = (ps(nc, es, "pmu", [128, 512], F32), PBuf())
    psq = (ps(nc, es, "psq", [128, 512], F32), PBuf())
    HC = [[(sb(nc, es, "HC%d_%d" % (i, T), [128, 512], F32), Buf()) for T in range(4)] for i in range(2)]
    SQ = [(sb(nc, es, "SQ%d" % i, [128, 512], F32), Buf()) for i in range(2)]
    mu = (sb(nc, es, "c_mu", [128, 512], F32), Buf())
    rs = (sb(nc, es, "c_rs", [128, 512], F32), Buf())
    t1 = [(sb(nc, es, "c_t1%d" % i, [128, 512], F32), Buf()) for i in range(2)]
    zb = [(sb(nc, es, "c_zb%d" % i, [128, 512], BF16), Buf()) for i in range(3)]
    yo = [(sb(nc, es, "c_yo%d" % i, [128, 512], BF16), Buf()) for i in range(3)]
    n = 0
    nq = 0
    for b in range(8):
        hc = HC[b % 2]
        for T in range(4):
            p, bp = pc[n % 3]
            n += 1
            for k in range(31):
                P.mm(p[:], Dg[:, T, k, :], H[:, T, b * 512 + k:b * 512 + k + 512], k == 0, k == 30,
                     r=[bc, bH[T]], w=[bp])
            P.act(hc[T][0][:], p[:], AF.Identity, bias=vecs["e_conv_b"][:, T:T + 1], r=[bp, bc], w=[hc[T][1]])
            sq, bsq = SQ[nq % 2]
            nq += 1
            P.act(sq[:], p[:], AF.Square, bias=vecs["e_conv_b"][:, T:T + 1], r=[bp, bc], w=[bsq])
            P.mm(pmu[0][:], ones[:], hc[T][0][:], T == 0, T == 3, r=[bc, hc[T][1]], w=[pmu[1]])
            P.mm(psq[0][:], ones[:], sq[:], T == 0, T == 3, r=[bc, bsq], w=[psq[1]])
        P.act(mu[0][:], pmu[0][:], AF.Copy, scale=1.0 / 512, r=[pmu[1]], w=[mu[1]])
        P.act(rs[0][:], pmu[0][:], AF.Square, scale=1.0 / 512, r=[pmu[1]], w=[rs[1]])
        P.stt(rs[0][:], psq[0][:], 1.0 / 512, rs[0][:], ALU.mult, ALU.subtract, r=[psq[1], rs[1]], w=[rs[1]])
        P.act(rs[0][:], rs[0][:], AF.Sqrt, bias=C.eps_t[:], r=[rs[1], C.b_eps], w=[rs[1]])
        P.op("dve", lambda e: e.reciprocal(rs[0][:], rs[0][:]), [rs[1]], [rs[1]])
        for T in range(4):
            t, bt = t1[T % 2]
            z, bz = zb[(b * 4 + T) % 3]
            y, by = yo[(b * 4 + T) % 3]
            P.dma("sp", z[:], zb_d[T][:, b * 512:(b + 1) * 512], w=[bz])
            P.tt("dve", t[:], hc[T][0][:], mu[0][:], ALU.subtract, r=[hc[T][1], mu[1]], w=[bt])
            P.tt("pool", t[:], t[:], rs[0][:], ALU.mult, r=[bt, rs[1]], w=[bt])
            P.act(t[:], t[:], AF.Silu, bias=vecs["e_ln_b"][:, T:T + 1], scale=vecs["e_ln_g"][:, T:T + 1],
                  r=[bt, bc], w=[bt])
            P.tt("dve", y[:], t[:], z[:], ALU.mult, r=[bt, bz], w=[by])
            P.dma("sp", ycat_d[4 + T][:, b * 512:(b + 1) * 512], y[:], r=[by])


def outproj_stage(C, w_ap, ycat_d, x_ap, o_ap, es, final_g=None):
    nc, P = C.nc, C.P
    wo = sb(nc, es, "wout", [128, 8, D], BF16)
    bwo = Buf()
    stage = [(sb(nc, es, "ost%d" % i, [128, D], F32), Buf()) for i in range(2)]
    load_cast(C, wo, bwo, lambda k: w_ap[k * 128:(k + 1) * 128, :], 8, D, stage)
    yc = [(sb(nc, es, "o_yc%d" % i, [128, 8, 128], BF16), Buf()) for i in range(3)]
    xs = [(sb(nc, es, "o_xs%d" % i, [128, D], F32), Buf()) for i in range(3)]
    ho = [(sb(nc, es, "o_ho%d" % i, [128, D], F32), Buf()) for i in range(2)]
    po = [(ps(nc, es, "o_ps%d" % i, [128, 512], F32), PBuf()) for i in range(4)]
    if final_g is not None:
        fg = sb(nc, es, "o_fg", [128, D], F32)
        bfg = Buf()
        P.dma("sp", fg[:], final_g.partition_broadcast(128), w=[bfg])
        ss = [(sb(nc, es, "o_ss%d" % i, [128, 1], F32), Buf()) for i in range(2)]
        junk = (sb(nc, es, "o_junk", [128, D], BF16), Buf())
    n = 0
    for tt in range(NT):
        y, by = yc[tt % 3]
        x, bx = xs[tt % 3]
        h, bh = ho[tt % 2]
        P.dma("sp", y[:], ycat_d[:, :, tt * 128:(tt + 1) * 128].rearrange("c p t -> p c t"), w=[by])
        P.dma("sp", x[:], x_ap[tt * 128:(tt + 1) * 128, :], w=[bx])
        for half in range(2):
            p, bp = po[n % 4]
            n += 1
            for c in range(8):
                P.mm(p[:], y[:, c, :], wo[:, c, half * 512:(half + 1) * 512], c == 0, c == 7, r=[by, bwo], w=[bp])
            P.tt("dve", h[:, half * 512:(half + 1) * 512], p[:], x[:, half * 512:(half + 1) * 512], ALU.add,
                 r=[bp, bx], w=[bh])
        if final_g is not None:
            s, bs = ss[tt % 2]
            P.act(junk[0][:], h[:], AF.Square, accum=s[:], r=[bh], w=[junk[1], bs])
            P.act(s[:], s[:], AF.Sqrt, bias=C.eps_t[:], scale=1.0 / D, r=[bs, C.b_eps], w=[bs])
            P.op("dve", lambda e, s=s: e.reciprocal(s[:], s[:]), [bs], [bs])
            P.stt(h[:], h[:], s[:], fg[:], ALU.mult, ALU.mult, r=[bh, bs, bfg], w=[bh])
        P.dma("sp", o_ap[tt * 128:(tt + 1) * 128, :], h[:], r=[bh])


def layer0(C, x_ap, h1_ap, ins, dbg=None):
    nc, P = C.nc, C.P
    with ExitStack() as es:
        U = sb(nc, es, "U", [128, 4, 16, 256], BF16)
        bU = [Buf("U%d" % t) for t in range(4)]
        H = sb(nc, es, "H", [128, 4, 30 + L], BF16)
        bH = [Buf("H%d" % t) for t in range(4)]
        for t in range(4):
            P.memset("pool", H[:, t, 0:30], 0.0, w=[bH[t]])
        za_d = nc.dram_tensor("za_d", [4, 128, L], BF16, kind="Internal").ap()
        zb_d = nc.dram_tensor("zb_d", [4, 128, L], BF16, kind="Internal").ap()

        with ExitStack() as ea:
            W = sb(nc, ea, "W0", [128, 8, E_IN], BF16)
            bW = Buf("W0")
            g_t = sb(nc, ea, "g0", [128, 8], F32)
            bg = Buf("g0")
            P.dma("sp", g_t[:], ins["norm_g"][0].rearrange("(k p) -> p k", p=128), w=[bg], slow=True)
            stage = [(sb(nc, ea, "wst%d" % i, [128, E_IN], F32), Buf()) for i in range(2)]
            load_weight_bf16(C, W, bW, ins["e_w_in"][0], E_IN, g_t, bg, stage)
            st = {
                "xs": [(sb(nc, ea, "xs%d" % i, [128, D], F32), Buf()) for i in range(3)],
                "ss": [(sb(nc, ea, "ss%d" % i, [128, 1], F32), Buf()) for i in range(3)],
                "xn": [(sb(nc, ea, "xn%d" % i, [128, D], BF16), Buf()) for i in range(3)],
                "junk": (sb(nc, ea, "junk", [128, D], BF16), Buf()),
                "pst": [(ps(nc, ea, "pst%d" % i, [128, 8, 128], BF16), PBuf()) for i in range(2)],
                "xs_i": 0, "pst_i": 0,
            }
            hnTs = [(sb(nc, ea, "hnT%d" % i, [128, 8, 512], BF16), Buf()) for i in range(2)]
            pmm = [(ps(nc, ea, "pmm%d" % i, [128, 512], F32), PBuf()) for i in range(4)]
            sig = [(sb(nc, ea, "sig%d" % i, [128, 512], F32), Buf()) for i in range(2)]
            zst = [(sb(nc, ea, "zst%d" % i, [128, 512], BF16), Buf()) for i in range(4)]
            pi = 0
            zi = 0
            order = [0, 1, 2, 3, 4, 5, 6, 7, 12, 8, 13, 9, 14, 10, 15, 11, 16, 17, 18, 19]
            for blk in range(NB):
                hnT, bh = hnTs[blk % 2]
                rms_tiles_to_hnT(C, lambda tt: x_ap[tt * 128:(tt + 1) * 128, :], hnT, bh, blk, st)
                for ct in order:
                    pm, bp = pmm[pi % 4]
                    pi += 1
                    for k in range(8):
                        P.mm(pm[:], W[:, k, ct * 128:(ct + 1) * 128], hnT[:, k, :], k == 0, k == 7,
                             r=[bW, bh], w=[bp])
                    m0 = blk * 32
                    if ct < 4:
                        P.cp("dve", U[:, ct, :, m0:m0 + 32],
                             pm[:].rearrange("p (m j) -> p j m", j=16), r=[bp], w=[bU[ct]])
                    elif ct < 8:
                        z, bz = zst[zi % 4]
                        zi += 1
                        P.act(z[:].rearrange("p (j m) -> p j m", j=16),
                              pm[:].rearrange("p (m j) -> p j m", j=16), AF.Silu, r=[bp], w=[bz])
                        P.dma("sp", za_d[ct - 4].rearrange("p (j m) -> p j m", j=16)[:, :, m0:m0 + 32],
                              z[:].rearrange("p (j m) -> p j m", j=16), r=[bz])
                    elif ct < 12:
                        sg, bs = sig[(ct - 8) % 2]
                        P.tt("dve", H[:, ct - 8, 30 + blk * 512:30 + (blk + 1) * 512], pm[:], sg[:], ALU.mult,
                             r=[bp, bs], w=[bH[ct - 8]])
                    elif ct < 16:
                        sg, bs = sig[(ct - 12) % 2]
                        P.act(sg[:], pm[:], AF.Sigmoid, r=[bp], w=[bs])
                    else:
                        z, bz = zst[zi % 4]
                        zi += 1
                        P.act(z[:], pm[:], AF.Silu, r=[bp], w=[bz])
                        P.dma("sp", zb_d[ct - 16][:, blk * 512:(blk + 1) * 512], z[:], r=[bz])
            if dbg is not None and dbg.get("stage") == "A":
                P.dma("sp", dbg["out"][0:128, 0:L], U[:, 0].rearrange("p j m -> p (j m)"), r=[bU[0]])
                P.dma("sp", dbg["out"][128:256, 0:L], H[:, 0, 30:30 + L], r=[bH[0]])
            P.barrier()
        if dbg is not None and dbg.get("stage") == "A":
            return

        G = sb(nc, es, "G", [128, 4, 16, 256], BF16)
        bG = [Buf("G%d" % t) for t in range(4)]
        with ExitStack() as e5:
            S5 = s5_setup(C, ins, e5)
            s5_main(C, S5, U, bU, G, bG, e5, dbg)
            P.barrier()
        if dbg is not None and dbg.get("stage") == "S5":
            return
        ycat_d = nc.dram_tensor("ycat_d", [8, 128, L], BF16, kind="Internal").ap()
        with ExitStack() as eg:
            glu_stage(C, ins, G, bG, za_d, ycat_d, eg)
            P.barrier()
        with ExitStack() as ec:
            conv_stage(C, ins, H, bH, zb_d, ycat_d, ec)
            P.barrier()
        with ExitStack() as eo:
            outproj_stage(C, ins["e_w_out"][0], ycat_d, x_ap, h1_ap, eo)
            P.barrier()
        if dbg is not None and dbg.get("stage") == "L0":
            return


NIT = 18
cQ, cZ, cK, cQI, cKI, cV, cWI, W1C = 0, 1024, 2048, 2560, 3072, 3200, 3456, 3464
AX = mybir.AxisListType


def rope_tables(C, es, cosT, sinT, bT):
    nc, P = C.nc, C.P
    b = Buf("ropesetup")
    I32 = mybir.dt.int32

    def t(name, shape, dt=F32):
        return sb(nc, es, "rp_" + name, shape, dt)

    pidx_i, tmp_i = t("pidx_i", [128, 1], I32), t("tmp_i", [128, 1], I32)
    P.op("pool", lambda e: e.iota(pidx_i[:], pattern=[[0, 1]], base=0, channel_multiplier=1), (), [b])
    invp, m16, sg, f = t("invp", [128, 1]), t("m16", [128, 1]), t("sg", [128, 1]), t("f", [128, 1])
    P.ts("dve", tmp_i[:], pidx_i[:], 7, None, ALU.bitwise_and, r=[b], w=[b])
    P.cp("dve", f[:], tmp_i[:], r=[b], w=[b])
    P.act(invp[:], f[:], AF.Exp, scale=-math.log(500000.0) / 8.0, r=[b], w=[b])
    P.ts("dve", tmp_i[:], pidx_i[:], 48, None, ALU.bitwise_and, r=[b], w=[b])
    P.cp("dve", f[:], tmp_i[:], r=[b], w=[b])
    P.ts("dve", m16[:], f[:], 0.5, None, ALU.is_lt, r=[b], w=[b])
    P.tt("dve", invp[:], invp[:], m16[:], ALU.mult, r=[b], w=[b])
    P.ts("dve", tmp_i[:], pidx_i[:], 56, None, ALU.bitwise_and, r=[b], w=[b])
    P.cp("dve", f[:], tmp_i[:], r=[b], w=[b])
    P.ts("dve", sg[:], f[:], 0.5, None, ALU.is_lt, r=[b], w=[b])
    P.ts("dve", sg[:], sg[:], -2.0, 1.0, ALU.mult, ALU.add, r=[b], w=[b])
    CH = 1024
    pos_i, pos, ang, ph, kf = t("pos_i", [128, CH], I32), t("pos", [128, CH]), t("ang", [128, CH]), t("ph", [128, CH]), t("kf", [128, CH])
    ki_ = t("ki", [128, CH], I32)
    for c in range(L // CH):
        P.op("pool", lambda e, c=c: e.iota(pos_i[:], pattern=[[1, CH]], base=c * CH, channel_multiplier=0), (), [b])
        P.cp("dve", pos[:], pos_i[:], r=[b], w=[b])
        P.ts("dve", ang[:], pos[:], invp[:], None, ALU.mult, r=[b], w=[b])
        for dst, off in ((sinT, 0.0), (cosT, 0.5 * PI)):
            P.ts("dve", ph[:], ang[:], off, None, ALU.add, r=[b], w=[b])
            P.ts("dve", ki_[:], ph[:], 1.0 / (2 * PI), None, ALU.mult, r=[b], w=[b])
            P.cp("dve", kf[:], ki_[:], r=[b], w=[b])
            P.stt(ph[:], kf[:], -CW1, ph[:], ALU.mult, ALU.add, r=[b], w=[b])
            P.stt(ph[:], kf[:], -CW2, ph[:], ALU.mult, ALU.add, r=[b], w=[b])
            P.ts("dve", kf[:], ph[:], PI, None, ALU.is_gt, r=[b], w=[b])
            P.stt(ph[:], kf[:], -2 * PI, ph[:], ALU.mult, ALU.add, r=[b], w=[b])
            P.ts("dve", ph[:], ph[:], -PI, PI, ALU.max, ALU.min, r=[b], w=[b])
            P.act(ph[:], ph[:], AF.Sin, r=[b], w=[b])
            if dst is sinT:
                P.ts("dve", dst[:, c * CH:(c + 1) * CH], ph[:], sg[:], None, ALU.mult, r=[b], w=[b, bT])
            else:
                P.cp("dve", dst[:, c * CH:(c + 1) * CH], ph[:], r=[b], w=[b, bT])


def layer1(C, x_ap, o_ap, ins):
    nc, P = C.nc, C.P
    with ExitStack() as es:
        KT2 = sb(nc, es, "KT2", [128, 4, L], BF16)
        bKT = Buf("KT2")
        Vtok = sb(nc, es, "Vtok", [128, NT, 4, 66], BF16)
        bV = Buf("Vtok")
        KI2 = sb(nc, es, "KI2", [128, L], BF16)
        bKI = Buf("KI2")
        WI = sb(nc, es, "WI", [128, NT, 8], F32)
        bWI = Buf("WI")
        P.memset("pool", Vtok[:, :, :, 64:65], 1.0, w=[bV])
        q_d = nc.dram_tensor("q_d", [8, 128, L], BF16, kind="Internal").ap()
        z_d = nc.dram_tensor("z_d", [8, 128, L], BF16, kind="Internal").ap()
        qi_d = nc.dram_tensor("qi_d", [4, 128, L], BF16, kind="Internal").ap()
        w_in = ins["o_w_in"][0]

        with ExitStack() as ea:
            cosT = sb(nc, ea, "cosT", [128, L], BF16)
            sinT = sb(nc, ea, "sinT", [128, L], BF16)
            bT = Buf("ropeT")
            with ExitStack() as er:
                rope_tables(C, er, cosT, sinT, bT)
                P.barrier()
            if getattr(C, "stop", None) == "R":
                return
            Pm = sb(nc, ea, "Pm", [128, 128], BF16)
            bPm = Buf("Pm")
            P.memset("pool", Pm[:], 0.0, w=[bPm])
            for base in (0, 64):
                P.cp("dve", Pm[:, base:base + 8], C.ident_b[:, base + 8:base + 16], r=[C.b_ident], w=[bPm])
                P.cp("dve", Pm[:, base + 8:base + 16], C.ident_b[:, base:base + 8], r=[C.b_ident], w=[bPm])
            W = sb(nc, ea, "W1", [128, 8, W1C], BF16)
            bW = Buf("W1")
            g_t = sb(nc, ea, "g1", [128, 8], F32)
            bg = Buf("g1")
            P.dma("sp", g_t[:], ins["norm_g"][1].rearrange("(k p) -> p k", p=128), w=[bg], slow=True)
            stg = sb(nc, ea, "w1st", [128, W1C], F32)
            bst = Buf("w1st")
            for k in range(8):
                rows = w_in[k * 128:(k + 1) * 128, :]
                P.dma("sp", stg[:, 0:2048], rows[:, 0:2048], w=[bst])
                for a in range(2):
                    P.dma("sp", stg[:, cK:cK + 512].rearrange("p (g two d) -> p g two d", two=2, d=64)[:, :, a, :],
                          rows[:, 2048:2304].rearrange("p (g d) -> p g d", d=64), w=[bst])
                    P.dma("sp", stg[:, cKI + 64 * a:cKI + 64 * a + 64], rows[:, 3072:3136], w=[bst])
                P.dma("sp", stg[:, cQI:cQI + 512], rows[:, 2560:3072], w=[bst])
                P.dma("sp", stg[:, cV:cV + 256], rows[:, 2304:2560], w=[bst])
                P.dma("sp", stg[:, cWI:cWI + 8], rows[:, 3136:3144], w=[bst])
                P.ts("dve", W[:, k, 0:1792], stg[:, 0:1792], g_t[:, k:k + 1], None, ALU.mult, r=[bst, bg], w=[bW])
                P.ts("pool", W[:, k, 1792:W1C], stg[:, 1792:W1C], g_t[:, k:k + 1], None, ALU.mult, r=[bst, bg], w=[bW])
            if getattr(C, "stop", None) == "W":
                return
            st = {
                "xs": [(sb(nc, ea, "b_xs%d" % i, [128, D], F32), Buf()) for i in range(2)],
                "ss": [(sb(nc, ea, "b_ss%d" % i, [128, 1], F32), Buf()) for i in range(2)],
                "xn": [(sb(nc, ea, "b_xn%d" % i, [128, D], BF16), Buf()) for i in range(2)],
                "junk": (sb(nc, ea, "b_junk", [128, D], BF16), Buf()),
                "pst": [(ps(nc, ea, "b_pst%d" % i, [128, 8, 128], BF16), PBuf()) for i in range(2)],
                "xs_i": 0, "pst_i": 0,
            }
            hnTs = [(sb(nc, ea, "b_hnT%d" % i, [128, 8, 512], BF16), Buf()) for i in range(2)]
            pmm = [(ps(nc, ea, "b_pmm%d" % i, [128, 512], F32), PBuf()) for i in range(3)]
            prr = [(ps(nc, ea, "b_prr%d" % i, [128, 512], F32), PBuf()) for i in range(2)]
            pvv = (ps(nc, ea, "b_pvv", [128, 512], F32), PBuf())
            xb = [(sb(nc, ea, "b_xb%d" % i, [128, 512], BF16), Buf()) for i in range(2)]
            ta = [(sb(nc, ea, "b_ta%d" % i, [128, 512], F32), Buf()) for i in range(2)]
            tb = [(sb(nc, ea, "b_tb%d" % i, [128, 512], F32), Buf()) for i in range(2)]
            so = [(sb(nc, ea, "b_so%d" % i, [128, 512], BF16), Buf()) for i in range(4)]
            n_mm = n_r = n_so = 0
            tiles = [("q", cQ + 128 * i, i) for i in range(8)] + [("z", cZ + 128 * i, i) for i in range(8)] + \
                    [("k", cK + 128 * i, i) for i in range(4)] + [("qi", cQI + 128 * i, i) for i in range(4)] + \
                    [("ki", cKI, 0)]
            sub = getattr(C, "stop", None)
            nblk = NB
            do_v = True
            if sub is not None and sub.startswith("B1") and len(sub) > 2:
                nblk = 1
                kinds = {"q": ["q"], "z": ["z"], "k": ["k"], "i": ["ki"], "v": []}[sub[2]]
                tiles = [t_ for t_ in tiles if t_[0] in kinds]
                do_v = sub[2] == "v"
            for blk in range(nblk):
                hnT, bh = hnTs[blk % 2]
                rms_tiles_to_hnT(C, lambda tt: x_ap[tt * 128:(tt + 1) * 128, :], hnT, bh, blk, st)
                sl = slice(blk * 512, (blk + 1) * 512)
                for kind, c0, idx in tiles:
                    pm, bp = pmm[n_mm % 3]
                    n_mm += 1
                    for k in range(8):
                        P.mm(pm[:], W[:, k, c0:c0 + 128], hnT[:, k, :], k == 0, k == 7, r=[bW, bh], w=[bp])
                    if kind == "z":
                        s, bs = so[n_so % 4]
                        n_so += 1
                        P.act(s[:], pm[:], AF.Silu, r=[bp], w=[bs])
                        P.dma("sp", z_d[idx][:, sl], s[:], r=[bs])
                        continue
                    x2, bx2 = xb[n_r % 2]
                    pr, bpr = prr[n_r % 2]
                    a, ba = ta[n_r % 2]
                    b2, bb2 = tb[n_r % 2]
                    n_r += 1
                    P.cp("act", x2[:], pm[:], r=[bp], w=[bx2])
                    P.mm(pr[:], Pm[:], x2[:], True, True, r=[bPm, bx2], w=[bpr])
                    P.tt("dve", a[:], pm[:], cosT[:, sl], ALU.mult, r=[bp, bT], w=[ba])
                    P.tt("dve", b2[:], pr[:], sinT[:, sl], ALU.mult, r=[bpr, bT], w=[bb2])
                    if kind == "k":
                        P.tt("pool", KT2[:, idx, sl], a[:], b2[:], ALU.add, r=[ba, bb2], w=[bKT])
                    elif kind == "ki":
                        P.tt("pool", KI2[:, sl], a[:], b2[:], ALU.add, r=[ba, bb2], w=[bKI])
                    else:
                        s, bs = so[n_so % 4]
                        n_so += 1
                        P.tt("pool", s[:], a[:], b2[:], ALU.add, r=[ba, bb2], w=[bs])
                        dst = q_d if kind == "q" else qi_d
                        P.dma("sp", dst[idx][:, sl], s[:], r=[bs])
                for i in range(4 if do_v else 0):
                    tt_ = blk * 4 + i
                    pv, bpv = pvv
                    for k in range(8):
                        P.mm(pv[:, 0:264], hnT[:, k, i * 128:(i + 1) * 128], W[:, k, cV:cV + 264], k == 0, k == 7,
                             r=[bh, bW], w=[bpv])
                    import os
                    vv = os.environ.get("VVAR", "")
                    if vv != "2":
                        P.cp("act", Vtok[:, tt_, :, 0:64], pv[:, 0:256].rearrange("p (g d) -> p g d", d=64), r=[bpv], w=[bV])
                    if vv != "1":
                        P.ts("dve", WI[:, tt_, :], pv[:, 256:264], (8 ** -0.5) * (64 ** -0.5), None, ALU.mult,
                             r=[bpv], w=[bWI])
            P.barrier()

        if getattr(C, "stop", "").startswith("B1"):
            return
        with ExitStack() as eb:
            w_out = ins["o_w_out"][0]
            wo = sb(nc, eb, "wo64", [64, 16, D], BF16)
            bwo = Buf("wo64")
            wst = [(sb(nc, eb, "wo_st%d" % i, [64, 1, D], F32), Buf()) for i in range(2)]
            for hp in range(16):
                s, bs = wst[hp % 2]
                P.dma("sp", s[:], w_out[hp * 64:(hp + 1) * 64, :].rearrange("(h d) c -> d h c", d=64), w=[bs])
                P.cp("dve" if hp % 2 == 0 else "pool", wo[:, hp:hp + 1, :], s[:], r=[bs], w=[bwo])
            fg = sb(nc, eb, "fg", [128, D], F32)
            bfg = Buf("fg")
            P.dma("sp", fg[:], ins["final_g"].partition_broadcast(128), w=[bfg])
            ones = sb(nc, eb, "Esel", [128, 128], F32)
            bon = Buf("Esel")
            P.memset("pool", ones[:], 0.0, w=[bon])
            P.memset("pool", ones[64:65, :], 1.0, w=[bon])
            cpow = sb(nc, eb, "cpow", [128, NIT + 1], F32)
            bcp = Buf("cpow")
            for n in range(NIT + 1):
                P.memset("pool", cpow[:, n:n + 1], 2.0 ** -(n + 1), w=[bcp])
            SC = [(sb(nc, eb, "SC%d" % i, [128, L], F32), Buf()) for i in range(1)]
            Mq = (sb(nc, eb, "Mq", [128, L], BF16), Buf())
            junk = Mq
            MT = [(sb(nc, eb, "MT%d" % i, [128, NT, 128], BF16), Buf()) for i in range(2)]
            qq = [(sb(nc, eb, "qq%d" % i, [128, 8, 128], BF16), Buf()) for i in range(2)]
            zq = [(sb(nc, eb, "zq%d" % i, [64, 16, 128], BF16), Buf()) for i in range(2)]
            qiq = [(sb(nc, eb, "qiq%d" % i, [128, 4, 128], BF16), Buf()) for i in range(2)]
            xs = [(sb(nc, eb, "c_xs%d" % i, [128, D], F32), Buf()) for i in range(2)]
            rl = [(sb(nc, eb, "rl%d" % i, [128, 512], F32), Buf()) for i in range(2)]
            sm = {nm: (sb(nc, eb, "sm_" + nm, [128, 1], F32), Buf()) for nm in ("rmax", "rmin", "w0", "mid", "cnt", "t", "thr")}
            hs = (sb(nc, eb, "hs", [128, NIT + 1], F32), Buf())
            pe_ = [(sb(nc, eb, "pexp%d" % i, [128, 512], BF16), Buf()) for i in range(3)]
            pmk = [(sb(nc, eb, "pmk%d" % i, [128, 512], BF16), Buf()) for i in range(3)]
            osb = [(sb(nc, eb, "osb%d" % i, [65, 512], F32), Buf()) for i in range(2)]
            on_ = [(sb(nc, eb, "on%d" % i, [64, 512], F32), Buf()) for i in range(2)]
            rbc = [(sb(nc, eb, "rbc%d" % i, [64, 512], F32), Buf()) for i in range(2)]
            og = [(sb(nc, eb, "og%d" % i, [64, 16, 128], BF16), Buf()) for i in range(2)]
            ho = [(sb(nc, eb, "c_ho%d" % i, [128, D], F32), Buf()) for i in range(2)]
            ss = [(sb(nc, eb, "c_ss%d" % i, [128, 1], F32), Buf()) for i in range(2)]
            fjunk = (sb(nc, eb, "c_fjunk", [128, D], BF16), Buf())
            psc = [(ps(nc, eb, "psc%d" % i, [128, 512], F32), PBuf()) for i in range(1)]
            ptr = [(ps(nc, eb, "ptr%d" % i, [128, 4, 128], BF16), PBuf()) for i in range(1)]
            pl_ = [(ps(nc, eb, "pl%d" % i, [128, 512], F32), PBuf()) for i in range(4)]
            po_ = [(ps(nc, eb, "po%d" % i, [128, 512], F32), PBuf()) for i in range(1)]
            pop = (ps(nc, eb, "pop", [128, 512], F32), PBuf())
            pbc = (pop[0][0:64, :], pop[1])
            cn = {"sc": 0, "rl": 0, "l": 0, "e": 0, "g": 0}

            def gen_S(qt):
                nk = (qt + 1) * 128
                nkb = (nk + 511) // 512
                tsl = slice(qt * 128, (qt + 1) * 128)
                q_t, bq = qq[qt % 2]
                z_t, bz = zq[qt % 2]
                qi_t, bqi = qiq[qt % 2]
                x_t, bx = xs[qt % 2]
                P.dma("sp", q_t[:], q_d[:, :, tsl].rearrange("t p l -> p t l"), w=[bq])
                P.dma("sp", qi_t[:], qi_d[:, :, tsl].rearrange("t p l -> p t l"), w=[bqi])
                P.dma("sp", z_t[:], z_d.rearrange("t (a d) l -> d (t a) l", a=2)[:, :, tsl], w=[bz])
                P.dma("sp", x_t[:], x_ap[tsl, :], w=[bx])
                sc, bsc = SC[0]
                for hi in range(8):
                    til, half = hi // 2, hi % 2
                    for kb in range(nkb):
                        w_ = min(512, nk - 512 * kb)
                        ksl = slice(512 * kb, 512 * kb + w_)
                        p, bp = psc[cn["sc"] % len(psc)]
                        cn["sc"] += 1
                        P.mm(p[:, 0:w_], qi_t[64 * half:64 * half + 64, til, :], KI2[64 * half:64 * half + 64, ksl],
                             True, True, r=[bqi, bKI], w=[bp])
                        if hi == 0:
                            P.ts("dve", sc[:, ksl], p[:, 0:w_], 0.0, WI[:, qt, 0:1], ALU.max, ALU.mult,
                                 r=[bp, bWI], w=[bsc])
                        else:
                            r_, br = rl[cn["rl"] % 2]
                            cn["rl"] += 1
                            P.act(r_[:, 0:w_], p[:, 0:w_], AF.Relu, r=[bp], w=[br])
                            P.stt(sc[:, ksl], r_[:, 0:w_], WI[:, qt, hi:hi + 1], sc[:, ksl], ALU.mult, ALU.add,
                                  r=[br, bWI, bsc], w=[bsc])
                        yield
                rmax, rmin, w0, mid, cnt, t_, thr = (sm[k_] for k_ in ("rmax", "rmin", "w0", "mid", "cnt", "t", "thr"))
                P.op("dve", lambda e: e.tensor_reduce(rmax[0][:], sc[:, 0:nk], AX.X, ALU.max), [bsc], [rmax[1]])
                P.op("dve", lambda e: e.tensor_reduce(rmin[0][:], sc[:, 0:nk], AX.X, ALU.min), [bsc], [rmin[1]])
                P.memset("pool", sc[0:64, nk - 64:nk], -1e30, w=[bsc])
                P.tt("dve", w0[0][:], rmax[0][:], rmin[0][:], ALU.subtract, r=[rmax[1], rmin[1]], w=[w0[1]])
                P.tt("dve", hs[0][:], cpow[:], w0[0][:].broadcast_to([128, NIT + 1]), ALU.mult, r=[bcp, w0[1]], w=[hs[1]])
                P.tt("dve", mid[0][:], rmin[0][:], hs[0][:, 0:1], ALU.add, r=[rmin[1], hs[1]], w=[mid[1]])
                yield
                for n in range(NIT):
                    P.ts("dve", junk[0][:, 0:nk], sc[:, 0:nk], mid[0][:], 0.0, ALU.is_ge, ALU.add, accum=cnt[0][:],
                         r=[bsc, mid[1]], w=[junk[1], cnt[1]])
                    P.ts("dve", t_[0][:], cnt[0][:], 255.5, hs[0][:, n:n + 1], ALU.is_gt, ALU.mult,
                         r=[cnt[1], hs[1]], w=[t_[1]])
                    P.stt(mid[0][:], t_[0][:], hs[0][:, n + 1:n + 2], mid[0][:], ALU.subtract, ALU.add,
                          r=[t_[1], hs[1], mid[1]], w=[mid[1]])
                    yield
                P.tt("dve", thr[0][:], mid[0][:], hs[0][:, NIT:NIT + 1], ALU.subtract, r=[mid[1], hs[1]], w=[thr[1]])
                P.ts("dve", Mq[0][:, 0:nk], sc[:, 0:nk], thr[0][:], None, ALU.is_ge, r=[bsc, thr[1]], w=[Mq[1]])
                mt, bmt = MT[qt % 2]
                for k0 in range(0, qt + 1, 4):
                    kn = min(4, qt + 1 - k0)
                    pt, bpt = ptr[0]
                    for kk in range(kn):
                        kt = k0 + kk
                        P.tr(pt[:, kk, :], Mq[0][:, kt * 128:(kt + 1) * 128], C.ident_b[:], r=[Mq[1], C.b_ident], w=[bpt])
                    P.cp("act", mt[:, k0:k0 + kn, :], pt[:, 0:kn, :], r=[bpt], w=[bmt])
                    yield

            def n_S(qt):
                nk = (qt + 1) * 128
                return 8 * ((nk + 511) // 512) + 1 + NIT + (qt + 4) // 4

            def gen_A(qt):
                tsl = slice(qt * 128, (qt + 1) * 128)
                q_t, bq = qq[qt % 2]
                z_t, bz = zq[qt % 2]
                x_t, bx = xs[qt % 2]
                mt, bmt = MT[qt % 2]
                og_t, bog = og[qt % 2]
                po, bpo = po_[0]
                iters = [(g, kt) for g in range(4) for kt in range(qt + 1)]
                pend = {}

                def stage1(i):
                    g, kt = iters[i]
                    pl, bpl = pl_[(2 * cn["l"]) % 4]
                    pl2, bpl2 = pl_[(2 * cn["l"] + 1) % 4]
                    cn["l"] += 1
                    ks = slice(kt * 128, (kt + 1) * 128)
                    P.mm(pl[:, 0:256], KT2[0:64, g, ks], q_t[0:64, 2 * g:2 * g + 2, :], True, True, r=[bKT, bq], w=[bpl])
                    P.mm(pl2[:, 0:256], KT2[64:128, g, ks], q_t[64:128, 2 * g:2 * g + 2, :], True, True, r=[bKT, bq], w=[bpl2])
                    pe, bpe = pe_[cn["e"] % 3]
                    pk, bpk = pmk[cn["e"] % 3]
                    cn["e"] += 1
                    P.act(pe[:, 0:256], pl[:, 0:256], AF.Exp, scale=0.125, r=[bpl], w=[bpe])
                    P.act(pe[:, 256:512], pl2[:, 0:256], AF.Exp, scale=0.125, r=[bpl2], w=[bpe])
                    P.tt("pool", pk[:].rearrange("p (s t) -> p s t", s=4),
                         pe[:].rearrange("p (s t) -> p s t", s=4),
                         mt[:, kt, :].unsqueeze(1).broadcast_to([128, 4, 128]), ALU.mult, r=[bpe, bmt], w=[bpk])
                    pend[i] = (pk, bpk)

                def stage2(i):
                    g, kt = iters[i]
                    pk, bpk = pend.pop(i)
                    P.mm(po[0:65, :], Vtok[:, kt, g, 0:65], pk[:], kt == 0, kt == qt, r=[bV, bpk], w=[bpo])
                    if kt != qt:
                        return
                    o_s, bos = osb[cn["g"] % 2]
                    o_n, bonn = on_[cn["g"] % 2]
                    r_b, brb = rbc[cn["g"] % 2]
                    cn["g"] += 1
                    P.cp("act", o_s[:], po[0:65, :], r=[bpo], w=[bos])
                    P.mm(pop[0][:], ones[0:65, :], o_s[0:65, :], True, True, r=[bon, bos], w=[pop[1]])
                    P.op("dve", lambda e, r_b=r_b: e.reciprocal(r_b[:], pop[0][0:64, :]), [pop[1]], [brb])
                    P.tt("dve", o_n[:], o_s[0:64, :], r_b[:], ALU.mult, r=[bos, brb], w=[bonn])
                    P.tt("pool", og_t[:, 4 * g:4 * g + 4, :].rearrange("p (i1 i0) t -> p i0 i1 t", i0=2),
                         o_n[:].rearrange("p (s1 s0 t) -> p s1 s0 t", s1=2, s0=2),
                         z_t[:, 4 * g:4 * g + 4, :].rearrange("p (i1 i0) t -> p i0 i1 t", i0=2), ALU.mult,
                         r=[bonn, bz], w=[bog])

                LOOK = 2
                for i in range(min(LOOK, len(iters))):
                    stage1(i)
                for i in range(len(iters)):
                    if i + LOOK < len(iters):
                        stage1(i + LOOK)
                    stage2(i)
                    yield
                h, bh = ho[qt % 2]
                for half in range(2):
                    for hh in range(16):
                        P.mm(pop[0][:], og_t[:, hh, :], wo[:, hh, half * 512:(half + 1) * 512], hh == 0, hh == 15,
                             r=[bog, bwo], w=[pop[1]])
                    P.tt("dve", h[:, half * 512:(half + 1) * 512], pop[0][:], x_t[:, half * 512:(half + 1) * 512], ALU.add,
                         r=[pop[1], bx], w=[bh])
                    yield
                s_, bs_ = ss[qt % 2]
                P.act(fjunk[0][:], h[:], AF.Square, accum=s_[:], r=[bh], w=[fjunk[1], bs_])
                P.act(s_[:], s_[:], AF.Sqrt, bias=C.eps_t[:], scale=1.0 / D, r=[bs_, C.b_eps], w=[bs_])
                P.op("dve", lambda e: e.reciprocal(s_[:], s_[:]), [bs_], [bs_])
                P.stt(h[:], h[:], s_[:], fg[:], ALU.mult, ALU.mult, r=[bh, bs_, bfg], w=[bh])
                P.dma("sp", o_ap[tsl, :], h[:], r=[bh])
                yield

            def n_A(qt):
                return 4 * (qt + 1) + 2 + 1

            for _ in gen_S(0):
                pass
            for qt in range(NT):
                ga = gen_A(qt)
                gs = gen_S(qt + 1) if qt + 1 < NT else None
                na, ns = n_A(qt), (n_S(qt + 1) if gs is not None else 0)
                ia = is_ = 0
                a_done = False
                s_done = gs is None
                while not (a_done and s_done):
                    if not s_done and (a_done or is_ * na <= ia * ns):
                        try:
                            next(gs)
                            is_ += 1
                        except StopIteration:
                            s_done = True
                    else:
                        try:
                            next(ga)
                            ia += 1
                        except StopIteration:
                            a_done = True
            P.barrier()


def build(mode="full"):
    nc = bass.Bass("TRN2", target_bir_lowering=False)
    ins = {}

    def din(name, shape):
        ins[name] = nc.dram_tensor(name, list(shape), F32, kind="ExternalInput").ap()

    need0 = mode in ("full", "L0", "A", "S5")
    need1 = mode in ("full",) + ("L1", "B1", "R", "W", "B1q", "B1z", "B1k", "B1i", "B1v")
    din("x", (L, D))
    din("norm_g", (2, D))
    if need0:
        for nm in L0_NAMES:
            din(nm, SHAPES[nm])
    if need1:
        for nm in L1_NAMES:
            din(nm, SHAPES[nm])
    out = nc.dram_tensor("out", [L, D], F32, kind="ExternalOutput").ap()
    dbg = None
    if mode in ("A", "S5"):
        dbg = {"stage": mode,
               "out": nc.dram_tensor("dbg", [512, L], BF16, kind="ExternalOutput").ap(),
               "out32": nc.dram_tensor("dbg32", [512, L], F32, kind="ExternalOutput").ap()}
    with ExitStack() as es:
        C = Ctx()
        C.nc = nc
        C.es = es
        C.P = Prog(nc, es)
        build_consts(C)
        if mode == "full":
            h1 = nc.dram_tensor("h1_d", [L, D], F32, kind="Internal").ap()
            layer0(C, ins["x"], h1, ins, None)
            layer1(C, h1, out, ins)
        elif mode in ("L1", "B1", "R", "W", "B1q", "B1z", "B1k", "B1i", "B1v"):
            C.stop = mode if mode != "L1" else ""
            layer1(C, ins["x"], out, ins)
        else:
            layer0(C, ins["x"], out, ins, dbg)
        C.P.emit()
    return nc


SHAPES = {"e_w_in": (1, D, E_IN), "e_lam_re": (1, 32, 64), "e_lam_im": (1, 32, 64), "e_log_step": (1, 32),
          "e_b_re": (1, 32, 64, 16), "e_b_im": (1, 32, 64, 16), "e_c_re": (1, 32, 16, 64), "e_c_im": (1, 32, 16, 64),
          "e_d_skip": (1, 512), "e_w_glu": (1, 512, 512), "e_b_glu": (1, 512), "e_conv_w": (1, 31, 512),
          "e_conv_b": (1, 512), "e_ln_g": (1, 512), "e_ln_b": (1, 512), "e_w_out": (1, D, D),
          "o_w_in": (1, D, O_IN), "o_w_out": (1, D, D), "final_g": (D,)}
L0_NAMES = ["e_w_in", "e_lam_re", "e_lam_im", "e_log_step", "e_b_re", "e_b_im", "e_c_re", "e_c_im", "e_d_skip",
            "e_w_glu", "e_b_glu", "e_conv_w", "e_conv_b", "e_ln_g", "e_ln_b", "e_w_out"]
L1_NAMES = ["o_w_in", "o_w_out", "final_g"]


def run(inputs, mode="full", x_override=None, trace=False):
    nc = build(mode)
    names = ["norm_g"]
    if mode in ("full", "L0", "A", "S5"):
        names += L0_NAMES
    if mode in ("full",) + ("L1", "B1", "R", "W", "B1q", "B1z", "B1k", "B1i", "B1v"):
        names += L1_NAMES
    shared = {k: np.ascontiguousarray(np.asarray(inputs[k], dtype=np.float32)) for k in names}
    x = x_override if x_override is not None else inputs["x"]
    in_maps = []
    for c in range(8):
        m = dict(shared)
        m["x"] = np.ascontiguousarray(np.asarray(x[c], dtype=np.float32))
        in_maps.append(m)
    return run_bass_kernel_spmd(nc, in_maps, core_ids=list(range(8)), trace=trace)


MODE = "full"


def kernel(**inputs):
    if MODE == "full":
        res = run(inputs, "full")
        return np.stack([np.asarray(r["out"]) for r in res.results], axis=0).astype(np.float32)
    r0 = run(inputs, "L0")
    h1 = [np.asarray(r["out"]) for r in r0.results]
    r1 = run(inputs, "L1", x_override=h1)
    return np.stack([np.asarray(r["out"]) for r in r1.results], axis=0).astype(np.float32)
```

```python
import math
from contextlib import ExitStack

import numpy as np
import concourse.bass as bass
import concourse.mybir as mybir
from concourse.bass_utils import run_bass_kernel_spmd

F32 = mybir.dt.float32
BF16 = mybir.dt.bfloat16
ALU = mybir.AluOpType
AF = mybir.ActivationFunctionType

L = 4096
D = 1024
NT = L // 128
NB = L // 512
EPS = 1e-6
E_IN = 2560
O_IN = 3144

ENGS = ["pe", "act", "dve", "pool", "sp"]
EPOCH_LEN = 3000


class Buf:
    __slots__ = ("lw", "rd", "name")

    def __init__(self, name=""):
        self.lw = None
        self.rd = {}
        self.name = name


class PBuf(Buf):
    __slots__ = ()


class Prog:
    def __init__(self, nc, es, n_dma_sems=24):
        self.nc = nc
        self.es = es
        self.sem = {}
        self.epoch = {e: 0 for e in ENGS}
        self.cnt = {e: 0 for e in ENGS}
        for e in ENGS:
            self.sem[e + "#0"] = es.enter_context(nc.semaphore("s_%s_0" % e))
        self.ops = {e: [] for e in ENGS}
        self.known = {e: {} for e in ENGS}
        self.pending = {e: {} for e in ENGS}
        self.dsem = [es.enter_context(nc.semaphore("d%d" % i)) for i in range(n_dma_sems)]
        self.dval = [0] * n_dma_sems
        self.dnext = 0

    def semh(self, key):
        if isinstance(key, tuple):
            return self.dsem[key[1]]
        return self.sem[key]

    def op(self, E, fn, r=(), w=(), dma=False):
        waits = {}
        known = self.known[E]

        def need(k, v):
            if E == "pe" and isinstance(k, str) and k.startswith("pe#"):
                return
            if known.get(k, 0) >= v:
                return
            if waits.get(k, 0) < v:
                waits[k] = v

        for k, v in self.pending[E].items():
            need(k, v)
        self.pending[E] = {}
        for b in r:
            if b.lw is not None:
                need(*b.lw)
            if isinstance(b, PBuf):
                for k, v in b.rd.items():
                    if not (isinstance(k, str) and k.split("#")[0] == E):
                        need(k, v)
        for b in w:
            if b.lw is not None:
                need(*b.lw)
            for k, v in b.rd.items():
                need(k, v)
        if dma:
            i = self.dnext
            self.dnext = (i + 1) % len(self.dsem)
            if self.dval[i] > 0:
                need(("d", i), self.dval[i])
            self.dval[i] += 16
            tok = (("d", i), self.dval[i])
            inc = 16
        else:
            if self.cnt[E] >= EPOCH_LEN:
                self.epoch[E] += 1
                self.cnt[E] = 0
                self.sem["%s#%d" % (E, self.epoch[E])] = self.es.enter_context(
                    self.nc.semaphore("s_%s_%d" % (E, self.epoch[E])))
            self.cnt[E] += 1
            tok = ("%s#%d" % (E, self.epoch[E]), self.cnt[E])
            inc = 1
        for k, v in waits.items():
            known[k] = v
        self.ops[E].append((list(waits.items()), fn, tok[0], inc))
        for b in r:
            if b.rd.get(tok[0], 0) < tok[1]:
                b.rd[tok[0]] = tok[1]
        for b in w:
            b.lw = tok
            b.rd = {}
        return tok

    def barrier(self):
        snap = {"%s#%d" % (e, self.epoch[e]): self.cnt[e] for e in ENGS if self.cnt[e] > 0}
        for i, v in enumerate(self.dval):
            if v > 0:
                snap[("d", i)] = v
        for e in ENGS:
            p = self.pending[e]
            for k, v in snap.items():
                if p.get(k, 0) < v:
                    p[k] = v

    def emit(self):
        self.barrier()
        final = {e: [(k, v) for k, v in self.pending[e].items()
                     if not (e == "pe" and isinstance(k, str) and k.startswith("pe#"))
                     and self.known[e].get(k, 0) < v] for e in ENGS}
        nc = self.nc
        with nc.Block() as block:
            def mk(E):
                def body(eng):
                    for waits, fn, key, inc in self.ops[E]:
                        for k, v in waits:
                            eng.wait_ge(self.semh(k), v)
                        ins = fn(eng)
                        ins.then_inc(self.semh(key), inc)
                    for k, v in final[E]:
                        eng.wait_ge(self.semh(k), v)
                return body
            block.tensor(mk("pe"))
            block.scalar(mk("act"))
            block.vector(mk("dve"))
            block.gpsimd(mk("pool"))
            block.sync(mk("sp"))

    def mm(self, out, lhsT, rhs, start, stop, r=(), w=()):
        return self.op("pe", lambda e: e.matmul(out, lhsT, rhs, start=start, stop=stop), r, w)

    def tr(self, out, in_, ident, r=(), w=()):
        return self.op("pe", lambda e: e.transpose(out, in_, ident), r, w)

    def act(self, out, in_, func, bias=None, scale=None, accum=None, r=(), w=()):
        kw = {}
        if bias is not None:
            kw["bias"] = bias
        if scale is not None:
            kw["scale"] = scale
        if accum is not None:
            kw["accum_out"] = accum
        return self.op("act", lambda e: e.activation(out, in_, func, **kw), r, w)

    def ts(self, E, out, in0, s1, s2, op0, op1=None, accum=None, r=(), w=()):
        kw = {}
        if op1 is not None:
            kw["op1"] = op1
        if accum is not None:
            kw["accum_out"] = accum
        return self.op(E, lambda e: e.tensor_scalar(out, in0, s1, s2, op0, **kw), r, w)

    def tt(self, E, out, in0, in1, op, r=(), w=()):
        return self.op(E, lambda e: e.tensor_tensor(out, in0, in1, op), r, w)

    def stt(self, out, in0, scalar, in1, op0, op1, r=(), w=()):
        return self.op("dve", lambda e: e.scalar_tensor_tensor(out, in0, scalar, in1, op0, op1), r, w)

    def cp(self, E, out, in_, r=(), w=()):
        if E == "act":
            return self.op("act", lambda e: e.copy(out, in_), r, w)
        return self.op(E, lambda e: e.tensor_copy(out, in_), r, w)

    def memset(self, E, ap, val, w=()):
        return self.op(E, lambda e: e.memset(ap, val), (), w)

    def dma(self, E, out, in_, r=(), w=(), slow=False):
        if slow:
            return self.op(E, lambda e: e.dma_start(out=out, in_=in_, allow_slow_non_contiguous=True), r, w, dma=True)
        return self.op(E, lambda e: e.dma_start(out=out, in_=in_), r, w, dma=True)


class Ctx:
    pass


def sb(nc, es, name, shape, dt):
    return es.enter_context(nc.sbuf_tensor(name, shape, dt))


def ps(nc, es, name, shape, dt):
    return es.enter_context(nc.psum_tensor(name, shape, dt))


def build_consts(C):
    nc, P, es = C.nc, C.P, C.es
    C.ident_f = sb(nc, es, "ident_f", [128, 128], F32)
    C.ident_b = sb(nc, es, "ident_b", [128, 128], BF16)
    C.b_ident = Buf("ident")
    P.memset("pool", C.ident_f[:], 0.0, w=[C.b_ident])
    P.op("pool", lambda e: e.affine_select(out=C.ident_f[:], in_=C.ident_f[:], pattern=[[-1, 128]],
                                           compare_op=ALU.not_equal, fill=1.0, base=0,
                                           channel_multiplier=1), [C.b_ident], [C.b_ident])
    P.cp("pool", C.ident_b[:], C.ident_f[:], r=[C.b_ident], w=[C.b_ident])
    C.eps_t = sb(nc, es, "eps_t", [128, 1], F32)
    C.b_eps = Buf("eps")
    P.memset("pool", C.eps_t[:], EPS, w=[C.b_eps])


def rms_tiles_to_hnT(C, src_ap_fn, hnT, hnT_bufs, blk, st):
    nc, P = C.nc, C.P
    for i in range(4):
        tt = blk * 4 + i
        s = st["xs_i"] % len(st["xs"])
        st["xs_i"] += 1
        xt, bx = st["xs"][s]
        P.dma("sp", xt[:], src_ap_fn(tt), w=[bx])
        ss, bss = st["ss"][s]
        junk, bj = st["junk"]
        P.act(junk[:], xt[:], AF.Square, accum=ss[:], r=[bx], w=[bj, bss])
        P.act(ss[:], ss[:], AF.Sqrt, bias=C.eps_t[:], scale=1.0 / D, r=[bss, C.b_eps], w=[bss])
        P.op("dve", lambda e, ss=ss: e.reciprocal(ss[:], ss[:]), [bss], [bss])
        xn, bxn = st["xn"][s % len(st["xn"])]
        P.ts("dve", xn[:], xt[:], ss[:], None, ALU.mult, r=[bx, bss], w=[bxn])
        pt, bpt = st["pst"][st["pst_i"] % len(st["pst"])]
        st["pst_i"] += 1
        for k in range(8):
            P.tr(pt[:, k, :], xn[:, k * 128:(k + 1) * 128], C.ident_b[:], r=[bxn, C.b_ident], w=[bpt])
        eng = "act" if (i % 2 == 0) else "dve"
        P.cp(eng, hnT[:, :, i * 128:(i + 1) * 128], pt[:], r=[bpt], w=[hnT_bufs])


def load_weight_bf16(C, wdst, bw, wsrc, ncols, g_t, bg, stage, col0=0, colmap=None):
    P = C.P
    for k in range(8):
        stg, bs = stage[k % len(stage)]
        P.dma("sp", stg[:, 0:ncols], wsrc[k * 128:(k + 1) * 128, :], w=[bs])
        eng = "dve" if k % 2 == 0 else "pool"
        P.ts(eng, wdst[:, k, col0:col0 + ncols], stg[:, 0:ncols], g_t[:, k:k + 1], None, ALU.mult,
             r=[bs, bg], w=[bw])


PW = [1] + [16 * (2 ** i) for i in range(8)]
PI = math.pi
CW1 = 6.28125
CW2 = 2 * math.pi - 6.28125


def s5_setup(C, ins, es):
    nc, P = C.nc, C.P
    S = Ctx()
    bS = Buf("s5setup")
    S.b = bS

    def t(name, shape, dt=F32):
        return sb(nc, es, "s5_" + name, shape, dt)

    lr, li, dt_ = t("lr", [128, 32]), t("li", [128, 32]), t("dt", [128, 32])
    for half in range(2):
        P.dma("sp", lr[half * 64:(half + 1) * 64, :], ins["e_lam_re"][0].rearrange("g n -> n g"), w=[bS], slow=True)
        P.dma("sp", li[half * 64:(half + 1) * 64, :], ins["e_lam_im"][0].rearrange("g n -> n g"), w=[bS], slow=True)
    P.dma("sp", dt_[:], ins["e_log_step"][0].partition_broadcast(128), w=[bS])
    bre, bim = t("bre", [128, 32, 16]), t("bim", [128, 32, 16])
    for half in range(2):
        P.dma("sp", bre[half * 64:(half + 1) * 64], ins["e_b_re"][0].rearrange("g n c -> n g c"), w=[bS])
        P.dma("sp", bim[half * 64:(half + 1) * 64], ins["e_b_im"][0].rearrange("g n c -> n g c"), w=[bS])
    cnat = t("cnat", [128, 4, 128])
    P.dma("sp", cnat[:, :, 0:64], ins["e_c_re"][0].rearrange("(t q) c n -> (q c) t n", q=8), w=[bS])
    P.dma("sp", cnat[:, :, 64:128], ins["e_c_im"][0].rearrange("(t q) c n -> (q c) t n", q=8), w=[bS])
    S.dskip = t("dskip", [128, 4])
    P.dma("sp", S.dskip[:], ins["e_d_skip"][0].rearrange("(t p) -> p t", p=128), w=[bS], slow=True)

    pidx_i = t("pidx_i", [128, 1], mybir.dt.int32)
    pidx = t("pidx", [128, 1])
    P.op("pool", lambda e: e.iota(pidx_i[:], pattern=[[0, 1]], base=0, channel_multiplier=1), (), [bS])
    P.cp("dve", pidx[:], pidx_i[:], r=[bS], w=[bS])
    mlo, mhi, sgn = t("mlo", [128, 1]), t("mhi", [128, 1]), t("sgn", [128, 1])
    P.ts("dve", mlo[:], pidx[:], 64.0, None, ALU.is_lt, r=[bS], w=[bS])
    P.ts("dve", mhi[:], mlo[:], -1.0, 1.0, ALU.mult, ALU.add, r=[bS], w=[bS])
    P.ts("dve", sgn[:], mlo[:], 2.0, -1.0, ALU.mult, ALU.add, r=[bS], w=[bS])
    mpar = t("mpar", [128, 4])
    pm64 = t("pm64", [128, 1])
    mtmp = t("mtmp", [128, 1])
    P.stt(pm64[:], mhi[:], -64.0, pidx[:], ALU.mult, ALU.add, r=[bS], w=[bS])
    for r4 in range(4):
        P.ts("dve", mtmp[:], pm64[:], 16.0 * r4 - 0.5, None, ALU.is_gt, r=[bS], w=[bS])
        P.ts("dve", mpar[:, r4:r4 + 1], pm64[:], 16.0 * r4 + 15.5, mtmp[:], ALU.is_lt, ALU.mult, r=[bS], w=[bS])
    negpi = t("negpi", [128, 1])
    P.memset("dve", negpi[:], -PI, w=[bS])
    S.sgn = sgn

    P.ts("dve", lr[:], lr[:], -1e-4, None, ALU.min, r=[bS], w=[bS])
    P.act(dt_[:], dt_[:], AF.Exp, r=[bS], w=[bS])
    lrdt, lidt = t("lrdt", [128, 32]), t("lidt", [128, 32])
    P.tt("dve", lrdt[:], lr[:], dt_[:], ALU.mult, r=[bS], w=[bS])
    P.tt("dve", lidt[:], li[:], dt_[:], ALU.mult, r=[bS], w=[bS])
    npw = len(PW)
    S.vr, S.vi = t("vr", [128, npw, 32]), t("vi", [128, npw, 32])
    mag, sn, cs, ph = t("mag", [128, 32]), t("sn", [128, 32]), t("cs", [128, 32]), t("ph", [128, 32])
    ai1 = t("ai1", [128, 32])
    ki_ = t("ki_", [128, 32], mybir.dt.int32)
    kf = t("kf", [128, 32])
    for ip, p in enumerate(PW):
        P.act(mag[:], lrdt[:], AF.Exp, scale=float(p), r=[bS], w=[bS])
        for dst, off in ((sn, 0.0), (cs, 0.5 * PI)):
            P.ts("dve", ph[:], lidt[:], float(p), off, ALU.mult, ALU.add, r=[bS], w=[bS])
            P.ts("dve", ki_[:], ph[:], 1.0 / (2 * PI), None, ALU.mult, r=[bS], w=[bS])
            P.cp("dve", kf[:], ki_[:], r=[bS], w=[bS])
            P.stt(ph[:], kf[:], -CW1, ph[:], ALU.mult, ALU.add, r=[bS], w=[bS])
            P.stt(ph[:], kf[:], -CW2, ph[:], ALU.mult, ALU.add, r=[bS], w=[bS])
            P.ts("dve", kf[:], ph[:], PI, None, ALU.is_gt, r=[bS], w=[bS])
            P.stt(ph[:], kf[:], -2 * PI, ph[:], ALU.mult, ALU.add, r=[bS], w=[bS])
            P.ts("dve", ph[:], ph[:], -PI, PI, ALU.max, ALU.min, r=[bS], w=[bS])
            P.act(dst[:], ph[:], AF.Sin, r=[bS], w=[bS])
        P.tt("dve", S.vr[:, ip, :], mag[:], cs[:], ALU.mult, r=[bS], w=[bS])
        if ip == 0:
            P.tt("dve", ai1[:], mag[:], sn[:], ALU.mult, r=[bS], w=[bS])
        P.stt(S.vi[:, ip, :], sn[:], sgn[:], mag[:], ALU.mult, ALU.mult, r=[bS], w=[bS])
    nr, den, kr, ki, tmp = t("nr", [128, 32]), t("den", [128, 32]), t("kr", [128, 32]), t("ki", [128, 32]), t("tmp", [128, 32])
    P.ts("dve", nr[:], S.vr[:, 0, :], -1.0, None, ALU.add, r=[bS], w=[bS])
    P.tt("dve", den[:], lr[:], lr[:], ALU.mult, r=[bS], w=[bS])
    P.tt("dve", tmp[:], li[:], li[:], ALU.mult, r=[bS], w=[bS])
    P.tt("dve", den[:], den[:], tmp[:], ALU.add, r=[bS], w=[bS])
    P.op("dve", lambda e: e.reciprocal(den[:], den[:]), [bS], [bS])
    P.tt("dve", kr[:], nr[:], lr[:], ALU.mult, r=[bS], w=[bS])
    P.tt("dve", tmp[:], ai1[:], li[:], ALU.mult, r=[bS], w=[bS])
    P.tt("dve", kr[:], kr[:], tmp[:], ALU.add, r=[bS], w=[bS])
    P.tt("dve", kr[:], kr[:], den[:], ALU.mult, r=[bS], w=[bS])
    P.tt("dve", ki[:], ai1[:], lr[:], ALU.mult, r=[bS], w=[bS])
    P.tt("dve", tmp[:], nr[:], li[:], ALU.mult, r=[bS], w=[bS])
    P.tt("dve", ki[:], ki[:], tmp[:], ALU.subtract, r=[bS], w=[bS])
    P.tt("dve", ki[:], ki[:], den[:], ALU.mult, r=[bS], w=[bS])
    KA, KB = t("KA", [128, 32]), t("KB", [128, 32])
    P.ts("dve", KA[:], kr[:], mlo[:], None, ALU.mult, r=[bS], w=[bS])
    P.stt(KA[:], ki[:], mhi[:], KA[:], ALU.mult, ALU.add, r=[bS], w=[bS])
    P.ts("dve", KB[:], kr[:], mhi[:], None, ALU.mult, r=[bS], w=[bS])
    P.ts("dve", tmp[:], ki[:], mlo[:], None, ALU.mult, r=[bS], w=[bS])
    P.tt("dve", KB[:], KB[:], tmp[:], ALU.subtract, r=[bS], w=[bS])
    bbar, btmp = t("bbar", [128, 32, 16]), t("btmp", [128, 32, 16])
    P.tt("dve", bbar[:], bre[:], KA[:].unsqueeze(2).broadcast_to([128, 32, 16]), ALU.mult, r=[bS], w=[bS])
    P.tt("dve", btmp[:], bim[:], KB[:].unsqueeze(2).broadcast_to([128, 32, 16]), ALU.mult, r=[bS], w=[bS])
    P.tt("dve", bbar[:], bbar[:], btmp[:], ALU.add, r=[bS], w=[bS])
    S.BT = t("BT", [128, 4, 4, 128], BF16)
    S.Cpad = t("Cpad", [128, 32, 128], BF16)
    P.memset("pool", S.Cpad[:], 0.0, w=[bS])
    pst = ps(nc, es, "s5_pst", [128, 128], F32)
    CSt = t("CSt", [128, 128])
    for T in range(4):
        P.tr(pst[:], bbar[:, 8 * T:8 * T + 8, :].rearrange("p g c -> p (g c)"), C.ident_f[:], r=[bS, C.b_ident], w=[bS])
        for par in range(4):
            P.ts("dve", S.BT[:, T, par, :], pst[:], mpar[:, par:par + 1], None, ALU.mult, r=[bS], w=[bS])
        P.tr(pst[:], cnat[:, T, :], C.ident_f[:], r=[bS, C.b_ident], w=[bS])
        P.ts("dve", CSt[:], pst[:], sgn[:], None, ALU.mult, r=[bS], w=[bS])
        for q in range(8):
            P.cp("dve", S.Cpad[:, 8 * T + q, 16 * q:16 * q + 16], CSt[:, 16 * q:16 * q + 16], r=[bS], w=[bS])
    S.J = t("J", [128, 128])
    P.cp("dve", S.J[:, 64:128], C.ident_f[:, 0:64], r=[C.b_ident], w=[bS])
    P.cp("dve", S.J[:, 0:64], C.ident_f[:, 64:128], r=[C.b_ident], w=[bS])
    return S


def s5_main(C, S, U, bU, G, bG, es, dbg):
    nc, P = C.nc, C.P
    bS = S.b
    pstep = [(ps(nc, es, "s5_pp%d" % i, [128, 256], F32), PBuf()) for i in range(5)]
    py = [(ps(nc, es, "s5_py%d" % i, [128, 256], F32), PBuf()) for i in range(2)]
    Sst = [[(sb(nc, es, "s5_S%d_%d" % (q, i), [128, 256], BF16), Buf()) for i in range(2)] for q in range(8)]
    Epp = [[(sb(nc, es, "s5_E%d_%d" % (q, i), [128, 256], F32), Buf()) for i in range(2)] for q in range(8)]
    Eprev = [(sb(nc, es, "s5_Ep%d" % q, [128, 256], BF16), Buf()) for q in range(8)]
    A1 = [(sb(nc, es, "s5_A1_%d" % q, [128, 2, 128], BF16), Buf()) for q in range(8)]
    Af = [(sb(nc, es, "s5_Af%d" % i, [128, 128], F32), Buf()) for i in range(4)]
    Atmp = (sb(nc, es, "s5_Atmp", [128, 128], F32), Buf())
    Ahi32 = (sb(nc, es, "s5_Ahi32", [128, 128], F32), Buf())
    yat = [(sb(nc, es, "s5_ya%d" % i, [128, 256], F32), Buf()) for i in range(2)]
    gtm = [(sb(nc, es, "s5_gt%d" % i, [128, 256], F32), Buf()) for i in range(2)]
    ppi = 0
    afi = 0
    yi = 0

    def gen_A(dst, bd, ip, g, eng):
        P.ts(eng, dst[:], C.ident_f[:], S.vr[:, ip, g:g + 1], None, ALU.mult, r=[C.b_ident, bS], w=[bd])
        P.stt(dst[:], S.J[:], S.vi[:, ip, g:g + 1], dst[:], ALU.mult, ALU.add, r=[bS, bd], w=[bd])

    for T in range(4):
        for q in range(8):
            g = 8 * T + q
            a1, ba = A1[q]
            gen_A(Atmp[0], Atmp[1], 0, g, "dve")
            P.cp("dve", a1[:, 0, :], Atmp[0][:], r=[Atmp[1]], w=[ba])
            P.cp("dve", Ahi32[0][:], a1[:, 0, :], r=[ba], w=[Ahi32[1]])
            P.tt("dve", a1[:, 1, :], Atmp[0][:], Ahi32[0][:], ALU.subtract, r=[Atmp[1], Ahi32[1]], w=[ba])
        for pas in range(2):
            for j in range(16):
                for q in range(8):
                    g = 8 * T + q
                    pr = q // 4
                    pp, bp = pstep[ppi % len(pstep)]
                    ppi += 1
                    a1, ba = A1[q]
                    sprev = None
                    if j > 0:
                        sprev = Sst[q][(j - 1) % 2]
                    elif pas == 1:
                        sprev = Eprev[q]
                    P.mm(pp[:], S.BT[64 * pr:64 * pr + 64, T, q % 4, :], U[64 * pr:64 * pr + 64, T, j, :],
                         True, sprev is None, r=[bS, bU[T]], w=[bp])
                    if sprev is not None:
                        P.mm(pp[:], a1[:, 0, :], sprev[0][:], False, False, r=[ba, sprev[1]], w=[bp])
                        P.mm(pp[:], a1[:, 1, :], sprev[0][:], False, True, r=[ba, sprev[1]], w=[bp])
                    sc, bs = Sst[q][j % 2]
                    if pas == 0 and j == 15:
                        P.cp("act", Epp[q][0][0][:], pp[:], r=[bp], w=[Epp[q][0][1]])
                    else:
                        eng = "act" if (q % 2 == 0) else "dve"
                        P.cp(eng, sc[:], pp[:], r=[bp], w=[bs])
                if pas == 1:
                    pyt, bpy = py[yi % 2]
                    ya, bya = yat[yi % 2]
                    yi += 1
                    for q in range(8):
                        sc, bs = Sst[q][j % 2]
                        P.mm(pyt[:], S.Cpad[:, 8 * T + q, :], sc[:], q == 0, q == 7, r=[bS, bs], w=[bpy])
                    P.stt(ya[:], U[:, T, j, :], S.dskip[:, T:T + 1], pyt[:], ALU.mult, ALU.add,
                          r=[bU[T], bS, bpy], w=[bya])
                    gt, bgt = gtm[yi % 2]
                    P.act(gt[:], ya[:], AF.Square, r=[bya], w=[bgt])
                    P.ts("dve", gt[:], gt[:], 0.0713548162726, 1.5957691216057308, ALU.mult, ALU.add, r=[bgt], w=[bgt])
                    P.tt("pool", gt[:], gt[:], ya[:], ALU.mult, r=[bgt, bya], w=[bgt])
                    P.act(gt[:], gt[:], AF.Sigmoid, r=[bgt], w=[bgt])
                    P.tt("pool", G[:, T, j, :], ya[:], gt[:], ALU.mult, r=[bya, bgt], w=[bG[T]])
                    if dbg is not None and dbg.get("stage") == "S5" and T == 0:
                        P.dma("sp", dbg["out32"][0:128, j * 256:(j + 1) * 256], ya[:], r=[bya])
            if pas == 0:
                for q in range(8):
                    g = 8 * T + q
                    cur = 0
                    for i in range(8):
                        d = 2 ** i
                        am, bam = Af[afi % len(Af)]
                        afi += 1
                        gen_A(am, bam, 1 + i, g, "pool" if False else "dve")
                        Ec, bEc = Epp[q][cur]
                        En, bEn = Epp[q][1 - cur]
                        pp, bp = pstep[ppi % len(pstep)]
                        ppi += 1
                        P.mm(pp[:, 0:256 - d], am[:], Ec[:, 0:256 - d], True, True, r=[bam, bEc], w=[bp])
                        P.tt("dve", En[:, d:256], Ec[:, d:256], pp[:, 0:256 - d], ALU.add, r=[bEc, bp], w=[bEn])
                        P.cp("act", En[:, 0:d], Ec[:, 0:d], r=[bEc], w=[bEn])
                        cur = 1 - cur
                    Ef, bEf = Epp[q][cur]
                    ep, bep = Eprev[q]
                    P.memset("pool", ep[:, 0:1], 0.0, w=[bep])
                    P.cp("act", ep[:, 1:256], Ef[:, 0:255], r=[bEf], w=[bep])


def load_cast(C, wdst, bw, wsrc_fn, nk, ncols, stage):
    P = C.P
    for k in range(nk):
        stg, bs = stage[k % len(stage)]
        P.dma("sp", stg[:, 0:ncols], wsrc_fn(k), w=[bs])
        P.cp("dve" if k % 2 == 0 else "pool", wdst[:, k, 0:ncols], stg[:, 0:ncols], r=[bs], w=[bw])


def glu_stage(C, ins, G, bG, za_d, ycat_d, es):
    nc, P = C.nc, C.P
    wg = sb(nc, es, "wglu", [128, 4, 512], BF16)
    bwg = Buf()
    stage = [(sb(nc, es, "gst%d" % i, [128, 512], F32), Buf()) for i in range(2)]
    load_cast(C, wg, bwg, lambda k: ins["e_w_glu"][0][k * 128:(k + 1) * 128, :], 4, 512, stage)
    bgl = sb(nc, es, "bglu", [128, 4], F32)
    bb = Buf()
    P.dma("sp", bgl[:], ins["e_b_glu"][0].rearrange("(t p) -> p t", p=128), w=[bb], slow=True)
    YAn = sb(nc, es, "YAn", [128, 4, L], BF16)
    bY = [Buf() for _ in range(4)]
    pg = [(ps(nc, es, "pglu%d" % i, [128, 512], F32), PBuf()) for i in range(3)]
    sg = [(sb(nc, es, "gsig%d" % i, [128, 512], F32), Buf()) for i in range(2)]
    zt = [(sb(nc, es, "gza%d" % i, [128, 512], BF16), Buf()) for i in range(3)]
    tm = [(sb(nc, es, "gtm%d" % i, [128, 512], F32), Buf()) for i in range(2)]
    n = 0
    for b in range(8):
        for T in range(4):
            p, bp = pg[n % 3]
            s, bs = sg[n % 2]
            z, bz = zt[n % 3]
            t, bt = tm[n % 2]
            n += 1
            P.dma("sp", z[:], za_d[T][:, b * 512:(b + 1) * 512], w=[bz])
            for kc in range(4):
                P.mm(p[:], wg[:, kc, T * 128:(T + 1) * 128],
                     G[:, kc, 2 * b:2 * b + 2, :].rearrange("p j m -> p (j m)"), kc == 0, kc == 3,
                     r=[bwg, bG[kc]], w=[bp])
            P.act(s[:], p[:], AF.Sigmoid, bias=bgl[:, T:T + 1], r=[bp, bb], w=[bs])
            P.tt("dve", t[:], G[:, T, 2 * b:2 * b + 2, :].rearrange("p j m -> p (j m)"), s[:], ALU.mult,
                 r=[bG[T], bs], w=[bt])
            P.tt("pool", YAn[:, T].rearrange("p (m j) -> p j m", j=16)[:, 2 * b:2 * b + 2, :],
                 t[:].rearrange("p (j m) -> p j m", j=2), z[:].rearrange("p (j m) -> p j m", j=2), ALU.mult,
                 r=[bt, bz], w=[bY[T]])
    for T in range(4):
        P.dma("sp", ycat_d[T], YAn[:, T], r=[bY[T]])


def conv_stage(C, ins, H, bH, zb_d, ycat_d, es):
    nc, P = C.nc, C.P
    bc = Buf("convsetup")
    cwn = sb(nc, es, "cwn", [31, 512], F32)
    P.dma("sp", cwn[:], ins["e_conv_w"][0], w=[bc])
    cw = sb(nc, es, "cw", [128, 4, 31], F32)
    pcw = ps(nc, es, "pcw", [128, 31], F32)
    for T in range(4):
        P.tr(pcw[:], cwn[0:31, T * 128:(T + 1) * 128], C.ident_f[0:31, 0:31], r=[bc, C.b_ident], w=[bc])
        P.cp("dve", cw[:, T, :], pcw[:], r=[bc], w=[bc])
    vecs = {}
    for nm in ("e_conv_b", "e_ln_g", "e_ln_b"):
        v = sb(nc, es, "cv_" + nm, [128, 4], F32)
        P.dma("sp", v[:], ins[nm][0].rearrange("(t p) -> p t", p=128), w=[bc], slow=True)
        vecs[nm] = v
    Dg = sb(nc, es, "Dg", [128, 4, 31, 128], BF16)
    for T in range(4):
        for k in range(31):
            P.ts("dve" if (k % 2 == 0) else "pool", Dg[:, T, k, :], C.ident_b[:], cw[:, T, k:k + 1], None, ALU.mult,
                 r=[bc, C.b_ident], w=[bc])
    ones = sb(nc, es, "ones_f", [128, 128], F32)
    P.memset("pool", ones[:], 1.0, w=[bc])
    pc = [(ps(nc, es, "pconv%d" % i, [128, 512], F32), PBuf()) for i in range(3)]
    pmu = (ps(nc, es, "pmu", [128, 512], F32), PBuf())
    psq = (ps(nc, es, "psq", [128, 512], F32), PBuf())
    HC = [[(sb(nc, es, "HC%d_%d" % (i, T), [128, 512], F32), Buf()) for T in range(4)] for i in range(2)]
    SQ = [(sb(nc, es, "SQ%d" % i, [128, 512], F32), Buf()) for i in range(2)]
    mu = (sb(nc, es, "c_mu", [128, 512], F32), Buf())
    rs = (sb(nc, es, "c_rs", [128, 512], F32), Buf())
    t1 = [(sb(nc, es, "c_t1%d" % i, [128, 512], F32), Buf()) for i in range(2)]
    zb = [(sb(nc, es, "c_zb%d" % i, [128, 512], BF16), Buf()) for i in range(3)]
    yo = [(sb(nc, es, "c_yo%d" % i, [128, 512], BF16), Buf()) for i in range(3)]
    n = 0
    nq = 0
    for b in range(8):
        hc = HC[b % 2]
        for T in range(4):
            p, bp = pc[n % 3]
            n += 1
            for k in range(31):
                P.mm(p[:], Dg[:, T, k, :], H[:, T, b * 512 + k:b * 512 + k + 512], k == 0, k == 30,
                     r=[bc, bH[T]], w=[bp])
            P.act(hc[T][0][:], p[:], AF.Identity, bias=vecs["e_conv_b"][:, T:T + 1], r=[bp, bc], w=[hc[T][1]])
            sq, bsq = SQ[nq % 2]
            nq += 1
            P.act(sq[:], p[:], AF.Square, bias=vecs["e_conv_b"][:, T:T + 1], r=[bp, bc], w=[bsq])
            P.mm(pmu[0][:], ones[:], hc[T][0][:], T == 0, T == 3, r=[bc, hc[T][1]], w=[pmu[1]])
            P.mm(psq[0][:], ones[:], sq[:], T == 0, T == 3, r=[bc, bsq], w=[psq[1]])
        P.act(mu[0][:], pmu[0][:], AF.Copy, scale=1.0 / 512, r=[pmu[1]], w=[mu[1]])
        P.act(rs[0][:], pmu[0][:], AF.Square, scale=1.0 / 512, r=[pmu[1]], w=[rs[1]])
        P.stt(rs[0][:], psq[0][:], 1.0 / 512, rs[0][:], ALU.mult, ALU.subtract, r=[psq[1], rs[1]], w=[rs[1]])
        P.act(rs[0][:], rs[0][:], AF.Sqrt, bias=C.eps_t[:], r=[rs[1], C.b_eps], w=[rs[1]])
        P.op("dve", lambda e: e.reciprocal(rs[0][:], rs[0][:]), [rs[1]], [rs[1]])
        for T in range(4):
            t, bt = t1[T % 2]
            z, bz = zb[(b * 4 + T) % 3]
            y, by = yo[(b * 4 + T) % 3]
            P.dma("sp", z[:], zb_d[T][:, b * 512:(b + 1) * 512], w=[bz])
            P.tt("dve", t[:], hc[T][0][:], mu[0][:], ALU.subtract, r=[hc[T][1], mu[1]], w=[bt])
            P.tt("pool", t[:], t[:], rs[0][:], ALU.mult, r=[bt, rs[1]], w=[bt])
            P.act(t[:], t[:], AF.Silu, bias=vecs["e_ln_b"][:, T:T + 1], scale=vecs["e_ln_g"][:, T:T + 1],
                  r=[bt, bc], w=[bt])
            P.tt("dve", y[:], t[:], z[:], ALU.mult, r=[bt, bz], w=[by])
            P.dma("sp", ycat_d[4 + T][:, b * 512:(b + 1) * 512], y[:], r=[by])


def outproj_stage(C, w_ap, ycat_d, x_ap, o_ap, es, final_g=None):
    nc, P = C.nc, C.P
    wo = sb(nc, es, "wout", [128, 8, D], BF16)
    bwo = Buf()
    stage = [(sb(nc, es, "ost%d" % i, [128, D], F32), Buf()) for i in range(2)]
    load_cast(C, wo, bwo, lambda k: w_ap[k * 128:(k + 1) * 128, :], 8, D, stage)
    yc = [(sb(nc, es, "o_yc%d" % i, [128, 8, 128], BF16), Buf()) for i in range(3)]
    xs = [(sb(nc, es, "o_xs%d" % i, [128, D], F32), Buf()) for i in range(3)]
    ho = [(sb(nc, es, "o_ho%d" % i, [128, D], F32), Buf()) for i in range(2)]
    po = [(ps(nc, es, "o_ps%d" % i, [128, 512], F32), PBuf()) for i in range(4)]
    if final_g is not None:
        fg = sb(nc, es, "o_fg", [128, D], F32)
        bfg = Buf()
        P.dma("sp", fg[:], final_g.partition_broadcast(128), w=[bfg])
        ss = [(sb(nc, es, "o_ss%d" % i, [128, 1], F32), Buf()) for i in range(2)]
        junk = (sb(nc, es, "o_junk", [128, D], BF16), Buf())
    n = 0
    for tt in range(NT):
        y, by = yc[tt % 3]
        x, bx = xs[tt % 3]
        h, bh = ho[tt % 2]
        P.dma("sp", y[:], ycat_d[:, :, tt * 128:(tt + 1) * 128].rearrange("c p t -> p c t"), w=[by])
        P.dma("sp", x[:], x_ap[tt * 128:(tt + 1) * 128, :], w=[bx])
        for half in range(2):
            p, bp = po[n % 4]
            n += 1
            for c in range(8):
                P.mm(p[:], y[:, c, :], wo[:, c, half * 512:(half + 1) * 512], c == 0, c == 7, r=[by, bwo], w=[bp])
            P.tt("dve", h[:, half * 512:(half + 1) * 512], p[:], x[:, half * 512:(half + 1) * 512], ALU.add,
                 r=[bp, bx], w=[bh])
        if final_g is not None:
            s, bs = ss[tt % 2]
            P.act(junk[0][:], h[:], AF.Square, accum=s[:], r=[bh], w=[junk[1], bs])
            P.act(s[:], s[:], AF.Sqrt, bias=C.eps_t[:], scale=1.0 / D, r=[bs, C.b_eps], w=[bs])
            P.op("dve", lambda e, s=s: e.reciprocal(s[:], s[:]), [bs], [bs])
            P.stt(h[:], h[:], s[:], fg[:], ALU.mult, ALU.mult, r=[bh, bs, bfg], w=[bh])
        P.dma("sp", o_ap[tt * 128:(tt + 1) * 128, :], h[:], r=[bh])


def layer0(C, x_ap, h1_ap, ins, dbg=None):
    nc, P = C.nc, C.P
    with ExitStack() as es:
        U = sb(nc, es, "U", [128, 4, 16, 256], BF16)
        bU = [Buf("U%d" % t) for t in range(4)]
        H = sb(nc, es, "H", [128, 4, 30 + L], BF16)
        bH = [Buf("H%d" % t) for t in range(4)]
        for t in range(4):
            P.memset("pool", H[:, t, 0:30], 0.0, w=[bH[t]])
        za_d = nc.dram_tensor("za_d", [4, 128, L], BF16, kind="Internal").ap()
        zb_d = nc.dram_tensor("zb_d", [4, 128, L], BF16, kind="Internal").ap()

        with ExitStack() as ea:
            W = sb(nc, ea, "W0", [128, 8, E_IN], BF16)
            bW = Buf("W0")
            g_t = sb(nc, ea, "g0", [128, 8], F32)
            bg = Buf("g0")
            P.dma("sp", g_t[:], ins["norm_g"][0].rearrange("(k p) -> p k", p=128), w=[bg], slow=True)
            stage = [(sb(nc, ea, "wst%d" % i, [128, E_IN], F32), Buf()) for i in range(2)]
            load_weight_bf16(C, W, bW, ins["e_w_in"][0], E_IN, g_t, bg, stage)
            st = {
                "xs": [(sb(nc, ea, "xs%d" % i, [128, D], F32), Buf()) for i in range(3)],
                "ss": [(sb(nc, ea, "ss%d" % i, [128, 1], F32), Buf()) for i in range(3)],
                "xn": [(sb(nc, ea, "xn%d" % i, [128, D], BF16), Buf()) for i in range(3)],
                "junk": (sb(nc, ea, "junk", [128, D], BF16), Buf()),
                "pst": [(ps(nc, ea, "pst%d" % i, [128, 8, 128], BF16), PBuf()) for i in range(2)],
                "xs_i": 0, "pst_i": 0,
            }
            hnTs = [(sb(nc, ea, "hnT%d" % i, [128, 8, 512], BF16), Buf()) for i in range(2)]
            pmm = [(ps(nc, ea, "pmm%d" % i, [128, 512], F32), PBuf()) for i in range(4)]
            sig = [(sb(nc, ea, "sig%d" % i, [128, 512], F32), Buf()) for i in range(2)]
            zst = [(sb(nc, ea, "zst%d" % i, [128, 512], BF16), Buf()) for i in range(4)]
            pi = 0
            zi = 0
            order = [0, 1, 2, 3, 4, 5, 6, 7, 12, 8, 13, 9, 14, 10, 15, 11, 16, 17, 18, 19]
            for blk in range(NB):
                hnT, bh = hnTs[blk % 2]
                rms_tiles_to_hnT(C, lambda tt: x_ap[tt * 128:(tt + 1) * 128, :], hnT, bh, blk, st)
                for ct in order:
                    pm, bp = pmm[pi % 4]
                    pi += 1
                    for k in range(8):
                        P.mm(pm[:], W[:, k, ct * 128:(ct + 1) * 128], hnT[:, k, :], k == 0, k == 7,
                             r=[bW, bh], w=[bp])
                    m0 = blk * 32
                    if ct < 4:
                        P.cp("dve", U[:, ct, :, m0:m0 + 32],
                             pm[:].rearrange("p (m j) -> p j m", j=16), r=[bp], w=[bU[ct]])
                    elif ct < 8:
                        z, bz = zst[zi % 4]
                        zi += 1
                        P.act(z[:].rearrange("p (j m) -> p j m", j=16),
                              pm[:].rearrange("p (m j) -> p j m", j=16), AF.Silu, r=[bp], w=[bz])
                        P.dma("sp", za_d[ct - 4].rearrange("p (j m) -> p j m", j=16)[:, :, m0:m0 + 32],
                              z[:].rearrange("p (j m) -> p j m", j=16), r=[bz])
                    elif ct < 12:
                        sg, bs = sig[(ct - 8) % 2]
                        P.tt("dve", H[:, ct - 8, 30 + blk * 512:30 + (blk + 1) * 512], pm[:], sg[:], ALU.mult,
                             r=[bp, bs], w=[bH[ct - 8]])
                    elif ct < 16:
                        sg, bs = sig[(ct - 12) % 2]
                        P.act(sg[:], pm[:], AF.Sigmoid, r=[bp], w=[bs])
                    else:
                        z, bz = zst[zi % 4]
                        zi += 1
                        P.act(z[:], pm[:], AF.Silu, r=[bp], w=[bz])
                        P.dma("sp", zb_d[ct - 16][:, blk * 512:(blk + 1) * 512], z[:], r=[bz])
            if dbg is not None and dbg.get("stage") == "A":
                P.dma("sp", dbg["out"][0:128, 0:L], U[:, 0].rearrange("p j m -> p (j m)"), r=[bU[0]])
                P.dma("sp", dbg["out"][128:256, 0:L], H[:, 0, 30:30 + L], r=[bH[0]])
            P.barrier()
        if dbg is not None and dbg.get("stage") == "A":
            return

        G = sb(nc, es, "G", [128, 4, 16, 256], BF16)
        bG = [Buf("G%d" % t) for t in range(4)]
        with ExitStack() as e5:
            S5 = s5_setup(C, ins, e5)
            s5_main(C, S5, U, bU, G, bG, e5, dbg)
            P.barrier()
        if dbg is not None and dbg.get("stage") == "S5":
            return
        ycat_d = nc.dram_tensor("ycat_d", [8, 128, L], BF16, kind="Internal").ap()
        with ExitStack() as eg:
            glu_stage(C, ins, G, bG, za_d, ycat_d, eg)
            P.barrier()
        with ExitStack() as ec:
            conv_stage(C, ins, H, bH, zb_d, ycat_d, ec)
            P.barrier()
        with ExitStack() as eo:
            outproj_stage(C, ins["e_w_out"][0], ycat_d, x_ap, h1_ap, eo)
            P.barrier()
        if dbg is not None and dbg.get("stage") == "L0":
            return


NIT = 18
cQ, cZ, cK, cQI, cKI, cV, cWI, W1C = 0, 1024, 2048, 2560, 3072, 3200, 3456, 3464
AX = mybir.AxisListType


def rope_tables(C, es, cosT, sinT, bT):
    nc, P = C.nc, C.P
    b = Buf("ropesetup")
    I32 = mybir.dt.int32

    def t(name, shape, dt=F32):
        return sb(nc, es, "rp_" + name, shape, dt)

    pidx_i, tmp_i = t("pidx_i", [128, 1], I32), t("tmp_i", [128, 1], I32)
    P.op("pool", lambda e: e.iota(pidx_i[:], pattern=[[0, 1]], base=0, channel_multiplier=1), (), [b])
    invp, m16, sg, f = t("invp", [128, 1]), t("m16", [128, 1]), t("sg", [128, 1]), t("f", [128, 1])
    P.ts("dve", tmp_i[:], pidx_i[:], 7, None, ALU.bitwise_and, r=[b], w=[b])
    P.cp("dve", f[:], tmp_i[:], r=[b], w=[b])
    P.act(invp[:], f[:], AF.Exp, scale=-math.log(500000.0) / 8.0, r=[b], w=[b])
    P.ts("dve", tmp_i[:], pidx_i[:], 48, None, ALU.bitwise_and, r=[b], w=[b])
    P.cp("dve", f[:], tmp_i[:], r=[b], w=[b])
    P.ts("dve", m16[:], f[:], 0.5, None, ALU.is_lt, r=[b], w=[b])
    P.tt("dve", invp[:], invp[:], m16[:], ALU.mult, r=[b], w=[b])
    P.ts("dve", tmp_i[:], pidx_i[:], 56, None, ALU.bitwise_and, r=[b], w=[b])
    P.cp("dve", f[:], tmp_i[:], r=[b], w=[b])
    P.ts("dve", sg[:], f[:], 0.5, None, ALU.is_lt, r=[b], w=[b])
    P.ts("dve", sg[:], sg[:], -2.0, 1.0, ALU.mult, ALU.add, r=[b], w=[b])
    CH = 1024
    pos_i, pos, ang, ph, kf = t("pos_i", [128, CH], I32), t("pos", [128, CH]), t("ang", [128, CH]), t("ph", [128, CH]), t("kf", [128, CH])
    ki_ = t("ki", [128, CH], I32)
    for c in range(L // CH):
        P.op("pool", lambda e, c=c: e.iota(pos_i[:], pattern=[[1, CH]], base=c * CH, channel_multiplier=0), (), [b])
        P.cp("dve", pos[:], pos_i[:], r=[b], w=[b])
        P.ts("dve", ang[:], pos[:], invp[:], None, ALU.mult, r=[b], w=[b])
        for dst, off in ((sinT, 0.0), (cosT, 0.5 * PI)):
            P.ts("dve", ph[:], ang[:], off, None, ALU.add, r=[b], w=[b])
            P.ts("dve", ki_[:], ph[:], 1.0 / (2 * PI), None, ALU.mult, r=[b], w=[b])
            P.cp("dve", kf[:], ki_[:], r=[b], w=[b])
            P.stt(ph[:], kf[:], -CW1, ph[:], ALU.mult, ALU.add, r=[b], w=[b])
            P.stt(ph[:], kf[:], -CW2, ph[:], ALU.mult, ALU.add, r=[b], w=[b])
            P.ts("dve", kf[:], ph[:], PI, None, ALU.is_gt, r=[b], w=[b])
            P.stt(ph[:], kf[:], -2 * PI, ph[:], ALU.mult, ALU.add, r=[b], w=[b])
            P.ts("dve", ph[:], ph[:], -PI, PI, ALU.max, ALU.min, r=[b], w=[b])
            P.act(ph[:], ph[:], AF.Sin, r=[b], w=[b])
            if dst is sinT:
                P.ts("dve", dst[:, c * CH:(c + 1) * CH], ph[:], sg[:], None, ALU.mult, r=[b], w=[b, bT])
            else:
                P.cp("dve", dst[:, c * CH:(c + 1) * CH], ph[:], r=[b], w=[b, bT])


def layer1(C, x_ap, o_ap, ins):
    nc, P = C.nc, C.P
    with ExitStack() as es:
        KT2 = sb(nc, es, "KT2", [128, 4, L], BF16)
        bKT = Buf("KT2")
        Vtok = sb(nc, es, "Vtok", [128, NT, 4, 66], BF16)
        bV = Buf("Vtok")
        KI2 = sb(nc, es, "KI2", [128, L], BF16)
        bKI = Buf("KI2")
        WI = sb(nc, es, "WI", [128, NT, 8], F32)
        bWI = Buf("WI")
        P.memset("pool", Vtok[:, :, :, 64:65], 1.0, w=[bV])
        q_d = nc.dram_tensor("q_d", [8, 128, L], BF16, kind="Internal").ap()
        z_d = nc.dram_tensor("z_d", [8, 128, L], BF16, kind="Internal").ap()
        qi_d = nc.dram_tensor("qi_d", [4, 128, L], BF16, kind="Internal").ap()
        w_in = ins["o_w_in"][0]

        with ExitStack() as ea:
            cosT = sb(nc, ea, "cosT", [128, L], BF16)
            sinT = sb(nc, ea, "sinT", [128, L], BF16)
            bT = Buf("ropeT")
            with ExitStack() as er:
                rope_tables(C, er, cosT, sinT, bT)
                P.barrier()
            if getattr(C, "stop", None) == "R":
                return
            Pm = sb(nc, ea, "Pm", [128, 128], BF16)
            bPm = Buf("Pm")
            P.memset("pool", Pm[:], 0.0, w=[bPm])
            for base in (0, 64):
                P.cp("dve", Pm[:, base:base + 8], C.ident_b[:, base + 8:base + 16], r=[C.b_ident], w=[bPm])
                P.cp("dve", Pm[:, base + 8:base + 16], C.ident_b[:, base:base + 8], r=[C.b_ident], w=[bPm])
            W = sb(nc, ea, "W1", [128, 8, W1C], BF16)
            bW = Buf("W1")
            g_t = sb(nc, ea, "g1", [128, 8], F32)
            bg = Buf("g1")
            P.dma("sp", g_t[:], ins["norm_g"][1].rearrange("(k p) -> p k", p=128), w=[bg], slow=True)
            stg = sb(nc, ea, "w1st", [128, W1C], F32)
            bst = Buf("w1st")
            for k in range(8):
                rows = w_in[k * 128:(k + 1) * 128, :]
                P.dma("sp", stg[:, 0:2048], rows[:, 0:2048], w=[bst])
                for a in range(2):
                    P.dma("sp", stg[:, cK:cK + 512].rearrange("p (g two d) -> p g two d", two=2, d=64)[:, :, a, :],
                          rows[:, 2048:2304].rearrange("p (g d) -> p g d", d=64), w=[bst])
                    P.dma("sp", stg[:, cKI + 64 * a:cKI + 64 * a + 64], rows[:, 3072:3136], w=[bst])
                P.dma("sp", stg[:, cQI:cQI + 512], rows[:, 2560:3072], w=[bst])
                P.dma("sp", stg[:, cV:cV + 256], rows[:, 2304:2560], w=[bst])
                P.dma("sp", stg[:, cWI:cWI + 8], rows[:, 3136:3144], w=[bst])
                P.ts("dve", W[:, k, 0:1792], stg[:, 0:1792], g_t[:, k:k + 1], None, ALU.mult, r=[bst, bg], w=[bW])
                P.ts("pool", W[:, k, 1792:W1C], stg[:, 1792:W1C], g_t[:, k:k + 1], None, ALU.mult, r=[bst, bg], w=[bW])
            if getattr(C, "stop", None) == "W":
                return
            st = {
                "xs": [(sb(nc, ea, "b_xs%d" % i, [128, D], F32), Buf()) for i in range(2)],
                "ss": [(sb(nc, ea, "b_ss%d" % i, [128, 1], F32), Buf()) for i in range(2)],
                "xn": [(sb(nc, ea, "b_xn%d" % i, [128, D], BF16), Buf()) for i in range(2)],
                "junk": (sb(nc, ea, "b_junk", [128, D], BF16), Buf()),
                "pst": [(ps(nc, ea, "b_pst%d" % i, [128, 8, 128], BF16), PBuf()) for i in range(2)],
                "xs_i": 0, "pst_i": 0,
            }
            hnTs = [(sb(nc, ea, "b_hnT%d" % i, [128, 8, 512], BF16), Buf()) for i in range(2)]
            pmm = [(ps(nc, ea, "b_pmm%d" % i, [128, 512], F32), PBuf()) for i in range(3)]
            prr = [(ps(nc, ea, "b_prr%d" % i, [128, 512], F32), PBuf()) for i in range(2)]
            pvv = (ps(nc, ea, "b_pvv", [128, 512], F32), PBuf())
            xb = [(sb(nc, ea, "b_xb%d" % i, [128, 512], BF16), Buf()) for i in range(2)]
            ta = [(sb(nc, ea, "b_ta%d" % i, [128, 512], F32), Buf()) for i in range(2)]
            tb = [(sb(nc, ea, "b_tb%d" % i, [128, 512], F32), Buf()) for i in range(2)]
            so = [(sb(nc, ea, "b_so%d" % i, [128, 512], BF16), Buf()) for i in range(4)]
            n_mm = n_r = n_so = 0
            tiles = [("q", cQ + 128 * i, i) for i in range(8)] + [("z", cZ + 128 * i, i) for i in range(8)] + \
                    [("k", cK + 128 * i, i) for i in range(4)] + [("qi", cQI + 128 * i, i) for i in range(4)] + \
                    [("ki", cKI, 0)]
            sub = getattr(C, "stop", None)
            nblk = NB
            do_v = True
            if sub is not None and sub.startswith("B1") and len(sub) > 2:
                nblk = 1
                kinds = {"q": ["q"], "z": ["z"], "k": ["k"], "i": ["ki"], "v": []}[sub[2]]
                tiles = [t_ for t_ in tiles if t_[0] in kinds]
                do_v = sub[2] == "v"
            for blk in range(nblk):
                hnT, bh = hnTs[blk % 2]
                rms_tiles_to_hnT(C, lambda tt: x_ap[tt * 128:(tt + 1) * 128, :], hnT, bh, blk, st)
                sl = slice(blk * 512, (blk + 1) * 512)
                for kind, c0, idx in tiles:
                    pm, bp = pmm[n_mm % 3]
                    n_mm += 1
                    for k in range(8):
                        P.mm(pm[:], W[:, k, c0:c0 + 128], hnT[:, k, :], k == 0, k == 7, r=[bW, bh], w=[bp])
                    if kind == "z":
                        s, bs = so[n_so % 4]
                        n_so += 1
                        P.act(s[:], pm[:], AF.Silu, r=[bp], w=[bs])
                        P.dma("sp", z_d[idx][:, sl], s[:], r=[bs])
                        continue
                    x2, bx2 = xb[n_r % 2]
                    pr, bpr = prr[n_r % 2]
                    a, ba = ta[n_r % 2]
                    b2, bb2 = tb[n_r % 2]
                    n_r += 1
                    P.cp("act", x2[:], pm[:], r=[bp], w=[bx2])
                    P.mm(pr[:], Pm[:], x2[:], True, True, r=[bPm, bx2], w=[bpr])
                    P.tt("dve", a[:], pm[:], cosT[:, sl], ALU.mult, r=[bp, bT], w=[ba])
                    P.tt("dve", b2[:], pr[:], sinT[:, sl], ALU.mult, r=[bpr, bT], w=[bb2])
                    if kind == "k":
                        P.tt("pool", KT2[:, idx, sl], a[:], b2[:], ALU.add, r=[ba, bb2], w=[bKT])
                    elif kind == "ki":
                        P.tt("pool", KI2[:, sl], a[:], b2[:], ALU.add, r=[ba, bb2], w=[bKI])
                    else:
                        s, bs = so[n_so % 4]
                        n_so += 1
                        P.tt("pool", s[:], a[:], b2[:], ALU.add, r=[ba, bb2], w=[bs])
                        dst = q_d if kind == "q" else qi_d
                        P.dma("sp", dst[idx][:, sl], s[:], r=[bs])
                for i in range(4 if do_v else 0):
                    tt_ = blk * 4 + i
                    pv, bpv = pvv
                    for k in range(8):
                        P.mm(pv[:, 0:264], hnT[:, k, i * 128:(i + 1) * 128], W[:, k, cV:cV + 264], k == 0, k == 7,
                             r=[bh, bW], w=[bpv])
                    import os
                    vv = os.environ.get("VVAR", "")
                    if vv != "2":
                        P.cp("act", Vtok[:, tt_, :, 0:64], pv[:, 0:256].rearrange("p (g d) -> p g d", d=64), r=[bpv], w=[bV])
                    if vv != "1":
                        P.ts("dve", WI[:, tt_, :], pv[:, 256:264], (8 ** -0.5) * (64 ** -0.5), None, ALU.mult,
                             r=[bpv], w=[bWI])
            P.barrier()

        if getattr(C, "stop", "").startswith("B1"):
            return
        with ExitStack() as eb:
            w_out = ins["o_w_out"][0]
            wo = sb(nc, eb, "wo64", [64, 16, D], BF16)
            bwo = Buf("wo64")
            wst = [(sb(nc, eb, "wo_st%d" % i, [64, 1, D], F32), Buf()) for i in range(2)]
            for hp in range(16):
                s, bs = wst[hp % 2]
                P.dma("sp", s[:], w_out[hp * 64:(hp + 1) * 64, :].rearrange("(h d) c -> d h c", d=64), w=[bs])
                P.cp("dve" if hp % 2 == 0 else "pool", wo[:, hp:hp + 1, :], s[:], r=[bs], w=[bwo])
            fg = sb(nc, eb, "fg", [128, D], F32)
            bfg = Buf("fg")
            P.dma("sp", fg[:], ins["final_g"].partition_broadcast(128), w=[bfg])
            ones = sb(nc, eb, "Esel", [128, 128], F32)
            bon = Buf("Esel")
            P.memset("pool", ones[:], 0.0, w=[bon])
            P.memset("pool", ones[64:65, :], 1.0, w=[bon])
            cpow = sb(nc, eb, "cpow", [128, NIT + 1], F32)
            bcp = Buf("cpow")
            for n in range(NIT + 1):
                P.memset("pool", cpow[:, n:n + 1], 2.0 ** -(n + 1), w=[bcp])
            SC = [(sb(nc, eb, "SC%d" % i, [128, L], F32), Buf()) for i in range(1)]
            Mq = (sb(nc, eb, "Mq", [128, L], BF16), Buf())
            junk = Mq
            MT = [(sb(nc, eb, "MT%d" % i, [128, NT, 128], BF16), Buf()) for i in range(2)]
            qq = [(sb(nc, eb, "qq%d" % i, [64, 16, 128], BF16), Buf()) for i in range(2)]
            zq = [(sb(nc, eb, "zq%d" % i, [64, 16, 128], BF16), Buf()) for i in range(2)]
            qiq = [(sb(nc, eb, "qiq%d" % i, [128, 4, 128], BF16), Buf()) for i in range(2)]
            xs = [(sb(nc, eb, "c_xs%d" % i, [128, D], F32), Buf()) for i in range(2)]
            rl = [(sb(nc, eb, "rl%d" % i, [128, 512], F32), Buf()) for i in range(2)]
            sm = {nm: (sb(nc, eb, "sm_" + nm, [128, 1], F32), Buf()) for nm in ("rmax", "rmin", "w0", "mid", "cnt", "t", "thr")}
            hs = (sb(nc, eb, "hs", [128, NIT + 1], F32), Buf())
            pe_ = [(sb(nc, eb, "pexp%d" % i, [128, 512], BF16), Buf()) for i in range(4)]
            pmk = [(sb(nc, eb, "pmk%d" % i, [128, 512], BF16), Buf()) for i in range(4)]
            osb = [(sb(nc, eb, "osb%d" % i, [65, 512], F32), Buf()) for i in range(2)]
            on_ = [(sb(nc, eb, "on%d" % i, [64, 512], F32), Buf()) for i in range(2)]
            rbc = [(sb(nc, eb, "rbc%d" % i, [64, 512], F32), Buf()) for i in range(2)]
            og = [(sb(nc, eb, "og%d" % i, [64, 16, 128], BF16), Buf()) for i in range(2)]
            ss = [(sb(nc, eb, "c_ss%d" % i, [128, 1], F32), Buf()) for i in range(2)]
            fjunk = (sb(nc, eb, "c_fjunk", [128, D], BF16), Buf())
            psc = [(ps(nc, eb, "psc%d" % i, [128, 512], F32), PBuf()) for i in range(2)]
            ptr = [(ps(nc, eb, "ptr%d" % i, [128, 4, 128], BF16), PBuf()) for i in range(1)]
            pl_ = [(ps(nc, eb, "pl%d" % i, [128, 512], F32), PBuf()) for i in range(2)]
            po_ = [(ps(nc, eb, "po%d" % i, [128, 512], F32), PBuf()) for i in range(2)]
            pop = (ps(nc, eb, "pop", [128, 512], F32), PBuf())
            pbc = (pop[0][0:64, :], pop[1])
            cn = {"sc": 0, "rl": 0, "l": 0, "e": 0, "g": 0}

            def gen_S(qt):
                nk = (qt + 1) * 128
                nkb = (nk + 511) // 512
                tsl = slice(qt * 128, (qt + 1) * 128)
                q_t, bq = qq[qt % 2]
                z_t, bz = zq[qt % 2]
                qi_t, bqi = qiq[qt % 2]
                x_t, bx = xs[qt % 2]
                P.dma("sp", q_t[:], q_d.rearrange("t (a d) l -> d (t a) l", a=2)[:, :, tsl], w=[bq])
                P.dma("sp", qi_t[:], qi_d[:, :, tsl].rearrange("t p l -> p t l"), w=[bqi])
                P.dma("sp", z_t[:], z_d.rearrange("t (a d) l -> d (t a) l", a=2)[:, :, tsl], w=[bz])
                P.dma("sp", x_t[:], x_ap[tsl, :], w=[bx])
                sc, bsc = SC[0]
                for hi in range(8):
                    til, half = hi // 2, hi % 2
                    for kb in range(nkb):
                        w_ = min(512, nk - 512 * kb)
                        ksl = slice(512 * kb, 512 * kb + w_)
                        p, bp = psc[cn["sc"] % len(psc)]
                        cn["sc"] += 1
                        P.mm(p[:, 0:w_], qi_t[64 * half:64 * half + 64, til, :], KI2[64 * half:64 * half + 64, ksl],
                             True, True, r=[bqi, bKI], w=[bp])
                        if hi == 0:
                            P.ts("dve", sc[:, ksl], p[:, 0:w_], 0.0, WI[:, qt, 0:1], ALU.max, ALU.mult,
                                 r=[bp, bWI], w=[bsc])
                        else:
                            r_, br = rl[cn["rl"] % 2]
                            cn["rl"] += 1
                            P.act(r_[:, 0:w_], p[:, 0:w_], AF.Relu, r=[bp], w=[br])
                            P.stt(sc[:, ksl], r_[:, 0:w_], WI[:, qt, hi:hi + 1], sc[:, ksl], ALU.mult, ALU.add,
                                  r=[br, bWI, bsc], w=[bsc])
                        yield
                rmax, rmin, w0, mid, cnt, t_, thr = (sm[k_] for k_ in ("rmax", "rmin", "w0", "mid", "cnt", "t", "thr"))
                P.op("dve", lambda e: e.tensor_reduce(rmax[0][:], sc[:, 0:nk], AX.X, ALU.max), [bsc], [rmax[1]])
                P.op("dve", lambda e: e.tensor_reduce(rmin[0][:], sc[:, 0:nk], AX.X, ALU.min), [bsc], [rmin[1]])
                P.memset("pool", sc[0:64, nk - 64:nk], -1e30, w=[bsc])
                P.tt("dve", w0[0][:], rmax[0][:], rmin[0][:], ALU.subtract, r=[rmax[1], rmin[1]], w=[w0[1]])
                P.tt("dve", hs[0][:], cpow[:], w0[0][:].broadcast_to([128, NIT + 1]), ALU.mult, r=[bcp, w0[1]], w=[hs[1]])
                P.tt("dve", mid[0][:], rmin[0][:], hs[0][:, 0:1], ALU.add, r=[rmin[1], hs[1]], w=[mid[1]])
                yield
                for n in range(NIT):
                    P.ts("dve", junk[0][:, 0:nk], sc[:, 0:nk], mid[0][:], 0.0, ALU.is_ge, ALU.add, accum=cnt[0][:],
                         r=[bsc, mid[1]], w=[junk[1], cnt[1]])
                    P.ts("dve", t_[0][:], cnt[0][:], 255.5, hs[0][:, n:n + 1], ALU.is_gt, ALU.mult,
                         r=[cnt[1], hs[1]], w=[t_[1]])
                    P.stt(mid[0][:], t_[0][:], hs[0][:, n + 1:n + 2], mid[0][:], ALU.subtract, ALU.add,
                          r=[t_[1], hs[1], mid[1]], w=[mid[1]])
                    yield
                P.tt("dve", thr[0][:], mid[0][:], hs[0][:, NIT:NIT + 1], ALU.subtract, r=[mid[1], hs[1]], w=[thr[1]])
                P.ts("dve", Mq[0][:, 0:nk], sc[:, 0:nk], thr[0][:], None, ALU.is_ge, r=[bsc, thr[1]], w=[Mq[1]])
                mt, bmt = MT[qt % 2]
                for k0 in range(0, qt + 1, 4):
                    kn = min(4, qt + 1 - k0)
                    pt, bpt = ptr[0]
                    for kk in range(kn):
                        kt = k0 + kk
                        P.tr(pt[:, kk, :], Mq[0][:, kt * 128:(kt + 1) * 128], C.ident_b[:], r=[Mq[1], C.b_ident], w=[bpt])
                    P.cp("act", mt[:, k0:k0 + kn, :], pt[:, 0:kn, :], r=[bpt], w=[bmt])
                    yield

            def n_S(qt):
                nk = (qt + 1) * 128
                return 8 * ((nk + 511) // 512) + 1 + NIT + (qt + 4) // 4

            def gen_A(qt):
                tsl = slice(qt * 128, (qt + 1) * 128)
                q_t, bq = qq[qt % 2]
                z_t, bz = zq[qt % 2]
                x_t, bx = xs[qt % 2]
                mt, bmt = MT[qt % 2]
                og_t, bog = og[qt % 2]
                iters = [(g, kt) for g in range(4) for kt in range(qt + 1)]
                pend = {}

                def stage1(i):
                    g, kt = iters[i]
                    pl, bpl = pl_[cn["l"] % 2]
                    cn["l"] += 1
                    ks = slice(kt * 128, (kt + 1) * 128)
                    P.mm(pl[:], KT2[0:64, g, ks], q_t[0:64, 4 * g:4 * g + 4, :], True, True, r=[bKT, bq], w=[bpl])
                    pe, bpe = pe_[cn["e"] % 4]
                    pk, bpk = pmk[cn["e"] % 4]
                    cn["e"] += 1
                    P.act(pe[:], pl[:], AF.Exp, scale=0.125, r=[bpl], w=[bpe])
                    P.tt("dve" if (i % 3 == 2) else "pool", pk[:].rearrange("p (s t) -> p s t", s=4),
                         pe[:].rearrange("p (s t) -> p s t", s=4),
                         mt[:, kt, :].unsqueeze(1).broadcast_to([128, 4, 128]), ALU.mult, r=[bpe, bmt], w=[bpk])
                    pend[i] = (pk, bpk)

                def stage2(i):
                    g, kt = iters[i]
                    pk, bpk = pend.pop(i)
                    po, bpo = po_[g % 2]
                    P.mm(po[0:65, :], Vtok[:, kt, g, 0:65], pk[:], kt == 0, kt == qt, r=[bV, bpk], w=[bpo])
                    if kt != qt:
                        return
                    o_s, bos = osb[cn["g"] % 2]
                    o_n, bonn = on_[cn["g"] % 2]
                    r_b, brb = rbc[cn["g"] % 2]
                    cn["g"] += 1
                    P.cp("act", o_s[:], po[0:65, :], r=[bpo], w=[bos])
                    P.mm(pop[0][:], ones[0:65, :], o_s[0:65, :], True, True, r=[bon, bos], w=[pop[1]])
                    P.op("dve", lambda e, r_b=r_b: e.reciprocal(r_b[:], pop[0][0:64, :]), [pop[1]], [brb])
                    P.tt("dve", o_n[:], o_s[0:64, :], r_b[:], ALU.mult, r=[bos, brb], w=[bonn])
                    P.tt("pool", og_t[:, 4 * g:4 * g + 4, :], o_n[:].rearrange("p (s t) -> p s t", s=4),
                         z_t[:, 4 * g:4 * g + 4, :], ALU.mult, r=[bonn, bz], w=[bog])

                LOOK = 3
                for i in range(min(LOOK, len(iters))):
                    stage1(i)
                for i in range(len(iters)):
                    if i + LOOK < len(iters):
                        stage1(i + LOOK)
                    stage2(i)
                    yield
                h, bh = x_t, bx
                for half in range(2):
                    for hh in range(16):
                        P.mm(pop[0][:], og_t[:, hh, :], wo[:, hh, half * 512:(half + 1) * 512], hh == 0, hh == 15,
                             r=[bog, bwo], w=[pop[1]])
                    P.tt("dve", h[:, half * 512:(half + 1) * 512], pop[0][:], x_t[:, half * 512:(half + 1) * 512], ALU.add,
                         r=[pop[1]], w=[bh])
                    yield
                s_, bs_ = ss[qt % 2]
                P.act(fjunk[0][:], h[:], AF.Square, accum=s_[:], r=[bh], w=[fjunk[1], bs_])
                P.act(s_[:], s_[:], AF.Sqrt, bias=C.eps_t[:], scale=1.0 / D, r=[bs_, C.b_eps], w=[bs_])
                P.op("dve", lambda e: e.reciprocal(s_[:], s_[:]), [bs_], [bs_])
                P.stt(h[:], h[:], s_[:], fg[:], ALU.mult, ALU.mult, r=[bh, bs_, bfg], w=[bh])
                P.dma("sp", o_ap[tsl, :], h[:], r=[bh])
                yield

            def n_A(qt):
                return 4 * (qt + 1) + 2 + 1

            for _ in gen_S(0):
                pass
            for qt in range(NT):
                ga = gen_A(qt)
                gs = gen_S(qt + 1) if qt + 1 < NT else None
                na, ns = n_A(qt), (n_S(qt + 1) if gs is not None else 0)
                ia = is_ = 0
                a_done = False
                s_done = gs is None
                while not (a_done and s_done):
                    if not s_done and (a_done or is_ * na <= ia * ns):
                        try:
                            next(gs)
                            is_ += 1
                        except StopIteration:
                            s_done = True
                    else:
                        try:
                            next(ga)
                            ia += 1
                        except StopIteration:
                            a_done = True
            P.barrier()


def build(mode="full"):
    nc = bass.Bass("TRN2", target_bir_lowering=False)
    ins = {}

    def din(name, shape):
        ins[name] = nc.dram_tensor(name, list(shape), F32, kind="ExternalInput").ap()

    need0 = mode in ("full", "L0", "A", "S5")
    need1 = mode in ("full",) + ("L1", "B1", "R", "W", "B1q", "B1z", "B1k", "B1i", "B1v")
    din("x", (L, D))
    din("norm_g", (2, D))
    if need0:
        for nm in L0_NAMES:
            din(nm, SHAPES[nm])
    if need1:
        for nm in L1_NAMES:
            din(nm, SHAPES[nm])
    out = nc.dram_tensor("out", [L, D], F32, kind="ExternalOutput").ap()
    dbg = None
    if mode in ("A", "S5"):
        dbg = {"stage": mode,
               "out": nc.dram_tensor("dbg", [512, L], BF16, kind="ExternalOutput").ap(),
               "out32": nc.dram_tensor("dbg32", [512, L], F32, kind="ExternalOutput").ap()}
    with ExitStack() as es:
        C = Ctx()
        C.nc = nc
        C.es = es
        C.P = Prog(nc, es)
        build_consts(C)
        if mode == "full":
            h1 = nc.dram_tensor("h1_d", [L, D], F32, kind="Internal").ap()
            layer0(C, ins["x"], h1, ins, None)
            layer1(C, h1, out, ins)
        elif mode in ("L1", "B1", "R", "W", "B1q", "B1z", "B1k", "B1i", "B1v"):
            C.stop = mode if mode != "L1" else ""
            layer1(C, ins["x"], out, ins)
        else:
            layer0(C, ins["x"], out, ins, dbg)
        C.P.emit()
    return nc


SHAPES = {"e_w_in": (1, D, E_IN), "e_lam_re": (1, 32, 64), "e_lam_im": (1, 32, 64), "e_log_step": (1, 32),
          "e_b_re": (1, 32, 64, 16), "e_b_im": (1, 32, 64, 16), "e_c_re": (1, 32, 16, 64), "e_c_im": (1, 32, 16, 64),
          "e_d_skip": (1, 512), "e_w_glu": (1, 512, 512), "e_b_glu": (1, 512), "e_conv_w": (1, 31, 512),
          "e_conv_b": (1, 512), "e_ln_g": (1, 512), "e_ln_b": (1, 512), "e_w_out": (1, D, D),
          "o_w_in": (1, D, O_IN), "o_w_out": (1, D, D), "final_g": (D,)}
L0_NAMES = ["e_w_in", "e_lam_re", "e_lam_im", "e_log_step", "e_b_re", "e_b_im", "e_c_re", "e_c_im", "e_d_skip",
            "e_w_glu", "e_b_glu", "e_conv_w", "e_conv_b", "e_ln_g", "e_ln_b", "e_w_out"]
L1_NAMES = ["o_w_in", "o_w_out", "final_g"]


def run(inputs, mode="full", x_override=None, trace=False):
    nc = build(mode)
    names = ["norm_g"]
    if mode in ("full", "L0", "A", "S5"):
        names += L0_NAMES
    if mode in ("full",) + ("L1", "B1", "R", "W", "B1q", "B1z", "B1k", "B1i", "B1v"):
        names += L1_NAMES
    shared = {k: np.ascontiguousarray(np.asarray(inputs[k], dtype=np.float32)) for k in names}
    x = x_override if x_override is not None else inputs["x"]
    in_maps = []
    for c in range(8):
        m = dict(shared)
        m["x"] = np.ascontiguousarray(np.asarray(x[c], dtype=np.float32))
        in_maps.append(m)
    return run_bass_kernel_spmd(nc, in_maps, core_ids=list(range(8)), trace=trace)


MODE = "full"


def kernel(**inputs):
    if MODE == "full":
        res = run(inputs, "full")
        return np.stack([np.asarray(r["out"]) for r in res.results], axis=0).astype(np.float32)
    r0 = run(inputs, "L0")
    h1 = [np.asarray(r["out"]) for r in r0.results]
    r1 = run(inputs, "L1", x_override=h1)
    return np.stack([np.asarray(r["out"]) for r in r1.results], axis=0).astype(np.float32)
```

```python
import math
from contextlib import ExitStack

import numpy as np
import concourse.bass as bass
import concourse.mybir as mybir
from concourse.bass_utils import run_bass_kernel_spmd

F32 = mybir.dt.float32
BF16 = mybir.dt.bfloat16
ALU = mybir.AluOpType
AF = mybir.ActivationFunctionType

L = 4096
D = 1024
NT = L // 128
NB = L // 512
EPS = 1e-6
E_IN = 2560
O_IN = 3144

ENGS = ["pe", "act", "dve", "pool", "sp"]
EPOCH_LEN = 3000


class Buf:
    __slots__ = ("lw", "rd", "name")

    def __init__(self, name=""):
        self.lw = None
        self.rd = {}
        self.name = name


class PBuf(Buf):
    __slots__ = ()


class Prog:
    def __init__(self, nc, es, n_dma_sems=24):
        self.nc = nc
        self.es = es
        self.sem = {}
        self.epoch = {e: 0 for e in ENGS}
        self.cnt = {e: 0 for e in ENGS}
        for e in ENGS:
            self.sem[e + "#0"] = es.enter_context(nc.semaphore("s_%s_0" % e))
        self.ops = {e: [] for e in ENGS}
        self.known = {e: {} for e in ENGS}
        self.pending = {e: {} for e in ENGS}
        self.dsem = [es.enter_context(nc.semaphore("d%d" % i)) for i in range(n_dma_sems)]
        self.dval = [0] * n_dma_sems
        self.dnext = 0

    def semh(self, key):
        if isinstance(key, tuple):
            return self.dsem[key[1]]
        return self.sem[key]

    def op(self, E, fn, r=(), w=(), dma=False):
        waits = {}
        known = self.known[E]

        def need(k, v):
            if E == "pe" and isinstance(k, str) and k.startswith("pe#"):
                return
            if known.get(k, 0) >= v:
                return
            if waits.get(k, 0) < v:
                waits[k] = v

        for k, v in self.pending[E].items():
            need(k, v)
        self.pending[E] = {}
        for b in r:
            if b.lw is not None:
                need(*b.lw)
            if isinstance(b, PBuf):
                for k, v in b.rd.items():
                    if not (isinstance(k, str) and k.split("#")[0] == E):
                        need(k, v)
        for b in w:
            if b.lw is not None:
                need(*b.lw)
            for k, v in b.rd.items():
                need(k, v)
        if dma:
            i = self.dnext
            self.dnext = (i + 1) % len(self.dsem)
            if self.dval[i] > 0:
                need(("d", i), self.dval[i])
            self.dval[i] += 16
            tok = (("d", i), self.dval[i])
            inc = 16
        else:
            if self.cnt[E] >= EPOCH_LEN:
                self.epoch[E] += 1
                self.cnt[E] = 0
                self.sem["%s#%d" % (E, self.epoch[E])] = self.es.enter_context(
                    self.nc.semaphore("s_%s_%d" % (E, self.epoch[E])))
            self.cnt[E] += 1
            tok = ("%s#%d" % (E, self.epoch[E]), self.cnt[E])
            inc = 1
        for k, v in waits.items():
            known[k] = v
        self.ops[E].append((list(waits.items()), fn, tok[0], inc))
        for b in r:
            if b.rd.get(tok[0], 0) < tok[1]:
                b.rd[tok[0]] = tok[1]
        for b in w:
            b.lw = tok
            b.rd = {}
        return tok

    def barrier(self):
        snap = {"%s#%d" % (e, self.epoch[e]): self.cnt[e] for e in ENGS if self.cnt[e] > 0}
        for i, v in enumerate(self.dval):
            if v > 0:
                snap[("d", i)] = v
        for e in ENGS:
            p = self.pending[e]
            for k, v in snap.items():
                if p.get(k, 0) < v:
                    p[k] = v

    def emit(self):
        self.barrier()
        final = {e: [(k, v) for k, v in self.pending[e].items()
                     if not (e == "pe" and isinstance(k, str) and k.startswith("pe#"))
                     and self.known[e].get(k, 0) < v] for e in ENGS}
        nc = self.nc
        with nc.Block() as block:
            def mk(E):
                def body(eng):
                    for waits, fn, key, inc in self.ops[E]:
                        for k, v in waits:
                            eng.wait_ge(self.semh(k), v)
                        ins = fn(eng)
                        ins.then_inc(self.semh(key), inc)
                    for k, v in final[E]:
                        eng.wait_ge(self.semh(k), v)
                return body
            block.tensor(mk("pe"))
            block.scalar(mk("act"))
            block.vector(mk("dve"))
            block.gpsimd(mk("pool"))
            block.sync(mk("sp"))

    def mm(self, out, lhsT, rhs, start, stop, r=(), w=()):
        return self.op("pe", lambda e: e.matmul(out, lhsT, rhs, start=start, stop=stop), r, w)

    def tr(self, out, in_, ident, r=(), w=()):
        return self.op("pe", lambda e: e.transpose(out, in_, ident), r, w)

    def act(self, out, in_, func, bias=None, scale=None, accum=None, r=(), w=()):
        kw = {}
        if bias is not None:
            kw["bias"] = bias
        if scale is not None:
            kw["scale"] = scale
        if accum is not None:
            kw["accum_out"] = accum
        return self.op("act", lambda e: e.activation(out, in_, func, **kw), r, w)

    def ts(self, E, out, in0, s1, s2, op0, op1=None, accum=None, r=(), w=()):
        kw = {}
        if op1 is not None:
            kw["op1"] = op1
        if accum is not None:
            kw["accum_out"] = accum
        return self.op(E, lambda e: e.tensor_scalar(out, in0, s1, s2, op0, **kw), r, w)

    def tt(self, E, out, in0, in1, op, r=(), w=()):
        return self.op(E, lambda e: e.tensor_tensor(out, in0, in1, op), r, w)

    def stt(self, out, in0, scalar, in1, op0, op1, r=(), w=()):
        return self.op("dve", lambda e: e.scalar_tensor_tensor(out, in0, scalar, in1, op0, op1), r, w)

    def cp(self, E, out, in_, r=(), w=()):
        if E == "act":
            return self.op("act", lambda e: e.copy(out, in_), r, w)
        return self.op(E, lambda e: e.tensor_copy(out, in_), r, w)

    def memset(self, E, ap, val, w=()):
        return self.op(E, lambda e: e.memset(ap, val), (), w)

    def dma(self, E, out, in_, r=(), w=(), slow=False):
        if slow:
            return self.op(E, lambda e: e.dma_start(out=out, in_=in_, allow_slow_non_contiguous=True), r, w, dma=True)
        return self.op(E, lambda e: e.dma_start(out=out, in_=in_), r, w, dma=True)


class Ctx:
    pass


def sb(nc, es, name, shape, dt):
    return es.enter_context(nc.sbuf_tensor(name, shape, dt))


def ps(nc, es, name, shape, dt):
    return es.enter_context(nc.psum_tensor(name, shape, dt))


def build_consts(C):
    nc, P, es = C.nc, C.P, C.es
    C.ident_f = sb(nc, es, "ident_f", [128, 128], F32)
    C.ident_b = sb(nc, es, "ident_b", [128, 128], BF16)
    C.b_ident = Buf("ident")
    P.memset("pool", C.ident_f[:], 0.0, w=[C.b_ident])
    P.op("pool", lambda e: e.affine_select(out=C.ident_f[:], in_=C.ident_f[:], pattern=[[-1, 128]],
                                           compare_op=ALU.not_equal, fill=1.0, base=0,
                                           channel_multiplier=1), [C.b_ident], [C.b_ident])
    P.cp("pool", C.ident_b[:], C.ident_f[:], r=[C.b_ident], w=[C.b_ident])
    C.eps_t = sb(nc, es, "eps_t", [128, 1], F32)
    C.b_eps = Buf("eps")
    P.memset("pool", C.eps_t[:], EPS, w=[C.b_eps])


def rms_tiles_to_hnT(C, src_ap_fn, hnT, hnT_bufs, blk, st):
    nc, P = C.nc, C.P
    for i in range(4):
        tt = blk * 4 + i
        s = st["xs_i"] % len(st["xs"])
        st["xs_i"] += 1
        xt, bx = st["xs"][s]
        P.dma("sp", xt[:], src_ap_fn(tt), w=[bx])
        ss, bss = st["ss"][s]
        junk, bj = st["junk"]
        P.act(junk[:], xt[:], AF.Square, accum=ss[:], r=[bx], w=[bj, bss])
        P.act(ss[:], ss[:], AF.Sqrt, bias=C.eps_t[:], scale=1.0 / D, r=[bss, C.b_eps], w=[bss])
        P.op("dve", lambda e, ss=ss: e.reciprocal(ss[:], ss[:]), [bss], [bss])
        xn, bxn = st["xn"][s % len(st["xn"])]
        P.ts("dve", xn[:], xt[:], ss[:], None, ALU.mult, r=[bx, bss], w=[bxn])
        pt, bpt = st["pst"][st["pst_i"] % len(st["pst"])]
        st["pst_i"] += 1
        for k in range(8):
            P.tr(pt[:, k, :], xn[:, k * 128:(k + 1) * 128], C.ident_b[:], r=[bxn, C.b_ident], w=[bpt])
        eng = "act" if (i % 2 == 0) else "dve"
        P.cp(eng, hnT[:, :, i * 128:(i + 1) * 128], pt[:], r=[bpt], w=[hnT_bufs])


def load_weight_bf16(C, wdst, bw, wsrc, ncols, g_t, bg, stage, col0=0, colmap=None):
    P = C.P
    for k in range(8):
        stg, bs = stage[k % len(stage)]
        P.dma("sp", stg[:, 0:ncols], wsrc[k * 128:(k + 1) * 128, :], w=[bs])
        eng = "dve" if k % 2 == 0 else "pool"
        P.ts(eng, wdst[:, k, col0:col0 + ncols], stg[:, 0:ncols], g_t[:, k:k + 1], None, ALU.mult,
             r=[bs, bg], w=[bw])


PW = [1] + [16 * (2 ** i) for i in range(8)]
PI = math.pi
CW1 = 6.28125
CW2 = 2 * math.pi - 6.28125


def s5_setup(C, ins, es):
    nc, P = C.nc, C.P
    S = Ctx()
    bS = Buf("s5setup")
    S.b = bS

    def t(name, shape, dt=F32):
        return sb(nc, es, "s5_" + name, shape, dt)

    lr, li, dt_ = t("lr", [128, 32]), t("li", [128, 32]), t("dt", [128, 32])
    for half in range(2):
        P.dma("sp", lr[half * 64:(half + 1) * 64, :], ins["e_lam_re"][0].rearrange("g n -> n g"), w=[bS], slow=True)
        P.dma("sp", li[half * 64:(half + 1) * 64, :], ins["e_lam_im"][0].rearrange("g n -> n g"), w=[bS], slow=True)
    P.dma("sp", dt_[:], ins["e_log_step"][0].partition_broadcast(128), w=[bS])
    bre, bim = t("bre", [128, 32, 16]), t("bim", [128, 32, 16])
    for half in range(2):
        P.dma("sp", bre[half * 64:(half + 1) * 64], ins["e_b_re"][0].rearrange("g n c -> n g c"), w=[bS])
        P.dma("sp", bim[half * 64:(half + 1) * 64], ins["e_b_im"][0].rearrange("g n c -> n g c"), w=[bS])
    cnat = t("cnat", [128, 4, 128])
    P.dma("sp", cnat[:, :, 0:64], ins["e_c_re"][0].rearrange("(t q) c n -> (q c) t n", q=8), w=[bS])
    P.dma("sp", cnat[:, :, 64:128], ins["e_c_im"][0].rearrange("(t q) c n -> (q c) t n", q=8), w=[bS])
    S.dskip = t("dskip", [128, 4])
    P.dma("sp", S.dskip[:], ins["e_d_skip"][0].rearrange("(t p) -> p t", p=128), w=[bS], slow=True)

    pidx_i = t("pidx_i", [128, 1], mybir.dt.int32)
    pidx = t("pidx", [128, 1])
    P.op("pool", lambda e: e.iota(pidx_i[:], pattern=[[0, 1]], base=0, channel_multiplier=1), (), [bS])
    P.cp("dve", pidx[:], pidx_i[:], r=[bS], w=[bS])
    mlo, mhi, sgn = t("mlo", [128, 1]), t("mhi", [128, 1]), t("sgn", [128, 1])
    P.ts("dve", mlo[:], pidx[:], 64.0, None, ALU.is_lt, r=[bS], w=[bS])
    P.ts("dve", mhi[:], mlo[:], -1.0, 1.0, ALU.mult, ALU.add, r=[bS], w=[bS])
    P.ts("dve", sgn[:], mlo[:], 2.0, -1.0, ALU.mult, ALU.add, r=[bS], w=[bS])
    mpar = t("mpar", [128, 4])
    pm64 = t("pm64", [128, 1])
    mtmp = t("mtmp", [128, 1])
    P.stt(pm64[:], mhi[:], -64.0, pidx[:], ALU.mult, ALU.add, r=[bS], w=[bS])
    for r4 in range(4):
        P.ts("dve", mtmp[:], pm64[:], 16.0 * r4 - 0.5, None, ALU.is_gt, r=[bS], w=[bS])
        P.ts("dve", mpar[:, r4:r4 + 1], pm64[:], 16.0 * r4 + 15.5, mtmp[:], ALU.is_lt, ALU.mult, r=[bS], w=[bS])
    negpi = t("negpi", [128, 1])
    P.memset("dve", negpi[:], -PI, w=[bS])
    S.sgn = sgn

    P.ts("dve", lr[:], lr[:], -1e-4, None, ALU.min, r=[bS], w=[bS])
    P.act(dt_[:], dt_[:], AF.Exp, r=[bS], w=[bS])
    lrdt, lidt = t("lrdt", [128, 32]), t("lidt", [128, 32])
    P.tt("dve", lrdt[:], lr[:], dt_[:], ALU.mult, r=[bS], w=[bS])
    P.tt("dve", lidt[:], li[:], dt_[:], ALU.mult, r=[bS], w=[bS])
    npw = len(PW)
    S.vr, S.vi = t("vr", [128, npw, 32]), t("vi", [128, npw, 32])
    mag, sn, cs, ph = t("mag", [128, 32]), t("sn", [128, 32]), t("cs", [128, 32]), t("ph", [128, 32])
    ai1 = t("ai1", [128, 32])
    ki_ = t("ki_", [128, 32], mybir.dt.int32)
    kf = t("kf", [128, 32])
    for ip, p in enumerate(PW):
        P.act(mag[:], lrdt[:], AF.Exp, scale=float(p), r=[bS], w=[bS])
        for dst, off in ((sn, 0.0), (cs, 0.5 * PI)):
            P.ts("dve", ph[:], lidt[:], float(p), off, ALU.mult, ALU.add, r=[bS], w=[bS])
            P.ts("dve", ki_[:], ph[:], 1.0 / (2 * PI), None, ALU.mult, r=[bS], w=[bS])
            P.cp("dve", kf[:], ki_[:], r=[bS], w=[bS])
            P.stt(ph[:], kf[:], -CW1, ph[:], ALU.mult, ALU.add, r=[bS], w=[bS])
            P.stt(ph[:], kf[:], -CW2, ph[:], ALU.mult, ALU.add, r=[bS], w=[bS])
            P.ts("dve", kf[:], ph[:], PI, None, ALU.is_gt, r=[bS], w=[bS])
            P.stt(ph[:], kf[:], -2 * PI, ph[:], ALU.mult, ALU.add, r=[bS], w=[bS])
            P.ts("dve", ph[:], ph[:], -PI, PI, ALU.max, ALU.min, r=[bS], w=[bS])
            P.act(dst[:], ph[:], AF.Sin, r=[bS], w=[bS])
        P.tt("dve", S.vr[:, ip, :], mag[:], cs[:], ALU.mult, r=[bS], w=[bS])
        if ip == 0:
            P.tt("dve", ai1[:], mag[:], sn[:], ALU.mult, r=[bS], w=[bS])
        P.stt(S.vi[:, ip, :], sn[:], sgn[:], mag[:], ALU.mult, ALU.mult, r=[bS], w=[bS])
    nr, den, kr, ki, tmp = t("nr", [128, 32]), t("den", [128, 32]), t("kr", [128, 32]), t("ki", [128, 32]), t("tmp", [128, 32])
    P.ts("dve", nr[:], S.vr[:, 0, :], -1.0, None, ALU.add, r=[bS], w=[bS])
    P.tt("dve", den[:], lr[:], lr[:], ALU.mult, r=[bS], w=[bS])
    P.tt("dve", tmp[:], li[:], li[:], ALU.mult, r=[bS], w=[bS])
    P.tt("dve", den[:], den[:], tmp[:], ALU.add, r=[bS], w=[bS])
    P.op("dve", lambda e: e.reciprocal(den[:], den[:]), [bS], [bS])
    P.tt("dve", kr[:], nr[:], lr[:], ALU.mult, r=[bS], w=[bS])
    P.tt("dve", tmp[:], ai1[:], li[:], ALU.mult, r=[bS], w=[bS])
    P.tt("dve", kr[:], kr[:], tmp[:], ALU.add, r=[bS], w=[bS])
    P.tt("dve", kr[:], kr[:], den[:], ALU.mult, r=[bS], w=[bS])
    P.tt("dve", ki[:], ai1[:], lr[:], ALU.mult, r=[bS], w=[bS])
    P.tt("dve", tmp[:], nr[:], li[:], ALU.mult, r=[bS], w=[bS])
    P.tt("dve", ki[:], ki[:], tmp[:], ALU.subtract, r=[bS], w=[bS])
    P.tt("dve", ki[:], ki[:], den[:], ALU.mult, r=[bS], w=[bS])
    KA, KB = t("KA", [128, 32]), t("KB", [128, 32])
    P.ts("dve", KA[:], kr[:], mlo[:], None, ALU.mult, r=[bS], w=[bS])
    P.stt(KA[:], ki[:], mhi[:], KA[:], ALU.mult, ALU.add, r=[bS], w=[bS])
    P.ts("dve", KB[:], kr[:], mhi[:], None, ALU.mult, r=[bS], w=[bS])
    P.ts("dve", tmp[:], ki[:], mlo[:], None, ALU.mult, r=[bS], w=[bS])
    P.tt("dve", KB[:], KB[:], tmp[:], ALU.subtract, r=[bS], w=[bS])
    bbar, btmp = t("bbar", [128, 32, 16]), t("btmp", [128, 32, 16])
    P.tt("dve", bbar[:], bre[:], KA[:].unsqueeze(2).broadcast_to([128, 32, 16]), ALU.mult, r=[bS], w=[bS])
    P.tt("dve", btmp[:], bim[:], KB[:].unsqueeze(2).broadcast_to([128, 32, 16]), ALU.mult, r=[bS], w=[bS])
    P.tt("dve", bbar[:], bbar[:], btmp[:], ALU.add, r=[bS], w=[bS])
    S.BT = t("BT", [128, 4, 4, 128], BF16)
    S.Cpad = t("Cpad", [128, 32, 128], BF16)
    P.memset("pool", S.Cpad[:], 0.0, w=[bS])
    pst = ps(nc, es, "s5_pst", [128, 128], F32)
    CSt = t("CSt", [128, 128])
    for T in range(4):
        P.tr(pst[:], bbar[:, 8 * T:8 * T + 8, :].rearrange("p g c -> p (g c)"), C.ident_f[:], r=[bS, C.b_ident], w=[bS])
        for par in range(4):
            P.ts("dve", S.BT[:, T, par, :], pst[:], mpar[:, par:par + 1], None, ALU.mult, r=[bS], w=[bS])
        P.tr(pst[:], cnat[:, T, :], C.ident_f[:], r=[bS, C.b_ident], w=[bS])
        P.ts("dve", CSt[:], pst[:], sgn[:], None, ALU.mult, r=[bS], w=[bS])
        for q in range(8):
            P.cp("dve", S.Cpad[:, 8 * T + q, 16 * q:16 * q + 16], CSt[:, 16 * q:16 * q + 16], r=[bS], w=[bS])
    S.J = t("J", [128, 128])
    P.cp("dve", S.J[:, 64:128], C.ident_f[:, 0:64], r=[C.b_ident], w=[bS])
    P.cp("dve", S.J[:, 0:64], C.ident_f[:, 64:128], r=[C.b_ident], w=[bS])
    return S


def s5_main(C, S, U, bU, G, bG, es, dbg):
    nc, P = C.nc, C.P
    bS = S.b
    pstep = [(ps(nc, es, "s5_pp%d" % i, [128, 256], F32), PBuf()) for i in range(5)]
    py = [(ps(nc, es, "s5_py%d" % i, [128, 256], F32), PBuf()) for i in range(2)]
    Sst = [[(sb(nc, es, "s5_S%d_%d" % (q, i), [128, 256], BF16), Buf()) for i in range(2)] for q in range(8)]
    Epp = [[(sb(nc, es, "s5_E%d_%d" % (q, i), [128, 256], F32), Buf()) for i in range(2)] for q in range(8)]
    Eprev = [(sb(nc, es, "s5_Ep%d" % q, [128, 256], BF16), Buf()) for q in range(8)]
    A1 = [(sb(nc, es, "s5_A1_%d" % q, [128, 2, 128], BF16), Buf()) for q in range(8)]
    Af = [(sb(nc, es, "s5_Af%d" % i, [128, 128], F32), Buf()) for i in range(4)]
    Atmp = (sb(nc, es, "s5_Atmp", [128, 128], F32), Buf())
    Ahi32 = (sb(nc, es, "s5_Ahi32", [128, 128], F32), Buf())
    yat = [(sb(nc, es, "s5_ya%d" % i, [128, 256], F32), Buf()) for i in range(2)]
    gtm = [(sb(nc, es, "s5_gt%d" % i, [128, 256], F32), Buf()) for i in range(2)]
    ppi = 0
    afi = 0
    yi = 0

    def gen_A(dst, bd, ip, g, eng):
        P.ts(eng, dst[:], C.ident_f[:], S.vr[:, ip, g:g + 1], None, ALU.mult, r=[C.b_ident, bS], w=[bd])
        P.stt(dst[:], S.J[:], S.vi[:, ip, g:g + 1], dst[:], ALU.mult, ALU.add, r=[bS, bd], w=[bd])

    for T in range(4):
        for q in range(8):
            g = 8 * T + q
            a1, ba = A1[q]
            gen_A(Atmp[0], Atmp[1], 0, g, "dve")
            P.cp("dve", a1[:, 0, :], Atmp[0][:], r=[Atmp[1]], w=[ba])
            P.cp("dve", Ahi32[0][:], a1[:, 0, :], r=[ba], w=[Ahi32[1]])
            P.tt("dve", a1[:, 1, :], Atmp[0][:], Ahi32[0][:], ALU.subtract, r=[Atmp[1], Ahi32[1]], w=[ba])
        for pas in range(2):
            for j in range(16):
                for q in range(8):
                    g = 8 * T + q
                    pr = q // 4
                    pp, bp = pstep[ppi % len(pstep)]
                    ppi += 1
                    a1, ba = A1[q]
                    sprev = None
                    if j > 0:
                        sprev = Sst[q][(j - 1) % 2]
                    elif pas == 1:
                        sprev = Eprev[q]
                    P.mm(pp[:], S.BT[64 * pr:64 * pr + 64, T, q % 4, :], U[64 * pr:64 * pr + 64, T, j, :],
                         True, sprev is None, r=[bS, bU[T]], w=[bp])
                    if sprev is not None:
                        P.mm(pp[:], a1[:, 0, :], sprev[0][:], False, False, r=[ba, sprev[1]], w=[bp])
                        P.mm(pp[:], a1[:, 1, :], sprev[0][:], False, True, r=[ba, sprev[1]], w=[bp])
                    sc, bs = Sst[q][j % 2]
                    if pas == 0 and j == 15:
                        P.cp("act", Epp[q][0][0][:], pp[:], r=[bp], w=[Epp[q][0][1]])
                    else:
                        eng = "act" if (q % 2 == 0) else "dve"
                        P.cp(eng, sc[:], pp[:], r=[bp], w=[bs])
                if pas == 1:
                    pyt, bpy = py[yi % 2]
                    ya, bya = yat[yi % 2]
                    yi += 1
                    for q in range(8):
                        sc, bs = Sst[q][j % 2]
                        P.mm(pyt[:], S.Cpad[:, 8 * T + q, :], sc[:], q == 0, q == 7, r=[bS, bs], w=[bpy])
                    P.stt(ya[:], U[:, T, j, :], S.dskip[:, T:T + 1], pyt[:], ALU.mult, ALU.add,
                          r=[bU[T], bS, bpy], w=[bya])
                    gt, bgt = gtm[yi % 2]
                    P.act(gt[:], ya[:], AF.Square, r=[bya], w=[bgt])
                    P.ts("dve", gt[:], gt[:], 0.0713548162726, 1.5957691216057308, ALU.mult, ALU.add, r=[bgt], w=[bgt])
                    P.tt("pool", gt[:], gt[:], ya[:], ALU.mult, r=[bgt, bya], w=[bgt])
                    P.act(gt[:], gt[:], AF.Sigmoid, r=[bgt], w=[bgt])
                    P.tt("pool", G[:, T, j, :], ya[:], gt[:], ALU.mult, r=[bya, bgt], w=[bG[T]])
                    if dbg is not None and dbg.get("stage") == "S5" and T == 0:
                        P.dma("sp", dbg["out32"][0:128, j * 256:(j + 1) * 256], ya[:], r=[bya])
            if pas == 0:
                for q in range(8):
                    g = 8 * T + q
                    cur = 0
                    for i in range(8):
                        d = 2 ** i
                        am, bam = Af[afi % len(Af)]
                        afi += 1
                        gen_A(am, bam, 1 + i, g, "pool" if False else "dve")
                        Ec, bEc = Epp[q][cur]
                        En, bEn = Epp[q][1 - cur]
                        pp, bp = pstep[ppi % len(pstep)]
                        ppi += 1
                        P.mm(pp[:, 0:256 - d], am[:], Ec[:, 0:256 - d], True, True, r=[bam, bEc], w=[bp])
                        P.tt("dve", En[:, d:256], Ec[:, d:256], pp[:, 0:256 - d], ALU.add, r=[bEc, bp], w=[bEn])
                        P.cp("act", En[:, 0:d], Ec[:, 0:d], r=[bEc], w=[bEn])
                        cur = 1 - cur
                    Ef, bEf = Epp[q][cur]
                    ep, bep = Eprev[q]
                    P.memset("pool", ep[:, 0:1], 0.0, w=[bep])
                    P.cp("act", ep[:, 1:256], Ef[:, 0:255], r=[bEf], w=[bep])


def load_cast(C, wdst, bw, wsrc_fn, nk, ncols, stage):
    P = C.P
    for k in range(nk):
        stg, bs = stage[k % len(stage)]
        P.dma("sp", stg[:, 0:ncols], wsrc_fn(k), w=[bs])
        P.cp("dve" if k % 2 == 0 else "pool", wdst[:, k, 0:ncols], stg[:, 0:ncols], r=[bs], w=[bw])


def glu_stage(C, ins, G, bG, za_d, ycat_d, es):
    nc, P = C.nc, C.P
    wg = sb(nc, es, "wglu", [128, 4, 512], BF16)
    bwg = Buf()
    stage = [(sb(nc, es, "gst%d" % i, [128, 512], F32), Buf()) for i in range(2)]
    load_cast(C, wg, bwg, lambda k: ins["e_w_glu"][0][k * 128:(k + 1) * 128, :], 4, 512, stage)
    bgl = sb(nc, es, "bglu", [128, 4], F32)
    bb = Buf()
    P.dma("sp", bgl[:], ins["e_b_glu"][0].rearrange("(t p) -> p t", p=128), w=[bb], slow=True)
    YAn = sb(nc, es, "YAn", [128, 4, L], BF16)
    bY = [Buf() for _ in range(4)]
    pg = [(ps(nc, es, "pglu%d" % i, [128, 512], F32), PBuf()) for i in range(3)]
    sg = [(sb(nc, es, "gsig%d" % i, [128, 512], F32), Buf()) for i in range(2)]
    zt = [(sb(nc, es, "gza%d" % i, [128, 512], BF16), Buf()) for i in range(3)]
    tm = [(sb(nc, es, "gtm%d" % i, [128, 512], F32), Buf()) for i in range(2)]
    n = 0
    for b in range(8):
        for T in range(4):
            p, bp = pg[n % 3]
            s, bs = sg[n % 2]
            z, bz = zt[n % 3]
            t, bt = tm[n % 2]
            n += 1
            P.dma("sp", z[:], za_d[T][:, b * 512:(b + 1) * 512], w=[bz])
            for kc in range(4):
                P.mm(p[:], wg[:, kc, T * 128:(T + 1) * 128],
                     G[:, kc, 2 * b:2 * b + 2, :].rearrange("p j m -> p (j m)"), kc == 0, kc == 3,
                     r=[bwg, bG[kc]], w=[bp])
            P.act(s[:], p[:], AF.Sigmoid, bias=bgl[:, T:T + 1], r=[bp, bb], w=[bs])
            P.tt("dve", t[:], G[:, T, 2 * b:2 * b + 2, :].rearrange("p j m -> p (j m)"), s[:], ALU.mult,
                 r=[bG[T], bs], w=[bt])
            P.tt("pool", YAn[:, T].rearrange("p (m j) -> p j m", j=16)[:, 2 * b:2 * b + 2, :],
                 t[:].rearrange("p (j m) -> p j m", j=2), z[:].rearrange("p (j m) -> p j m", j=2), ALU.mult,
                 r=[bt, bz], w=[bY[T]])
    for T in range(4):
        P.dma("sp", ycat_d[T], YAn[:, T], r=[bY[T]])


def conv_stage(C, ins, H, bH, zb_d, ycat_d, es):
    nc, P = C.nc, C.P
    bc = Buf("convsetup")
    cwn = sb(nc, es, "cwn", [31, 512], F32)
    P.dma("sp", cwn[:], ins["e_conv_w"][0], w=[bc])
    cw = sb(nc, es, "cw", [128, 4, 31], F32)
    pcw = ps(nc, es, "pcw", [128, 31], F32)
    for T in range(4):
        P.tr(pcw[:], cwn[0:31, T * 128:(T + 1) * 128], C.ident_f[0:31, 0:31], r=[bc, C.b_ident], w=[bc])
        P.cp("dve", cw[:, T, :], pcw[:], r=[bc], w=[bc])
    vecs = {}
    for nm in ("e_conv_b", "e_ln_g", "e_ln_b"):
        v = sb(nc, es, "cv_" + nm, [128, 4], F32)
        P.dma("sp", v[:], ins[nm][0].rearrange("(t p) -> p t", p=128), w=[bc], slow=True)
        vecs[nm] = v
    Dg = sb(nc, es, "Dg", [128, 4, 31, 128], BF16)
    for T in range(4):
        for k in range(31):
            P.ts("dve" if (k % 2 == 0) else "pool", Dg[:, T, k, :], C.ident_b[:], cw[:, T, k:k + 1], None, ALU.mult,
                 r=[bc, C.b_ident], w=[bc])
    ones = sb(nc, es, "ones_f", [128, 128], F32)
    P.memset("pool", ones[:], 1.0, w=[bc])
    pc = [(ps(nc, es, "pconv%d" % i, [128, 512], F32), PBuf()) for i in range(3)]
    pmu = (ps(nc, es, "pmu", [128, 512], F32), PBuf())
    psq = (ps(nc, es, "psq", [128, 512], F32), PBuf())
    HC = [[(sb(nc, es, "HC%d_%d" % (i, T), [128, 512], F32), Buf()) for T in range(4)] for i in range(2)]
    SQ = [(sb(nc, es, "SQ%d" % i, [128, 512], F32), Buf()) for i in range(2)]
    mu = (sb(nc, es, "c_mu", [128, 512], F32), Buf())
    rs = (sb(nc, es, "c_rs", [128, 512], F32), Buf())
    t1 = [(sb(nc, es, "c_t1%d" % i, [128, 512], F32), Buf()) for i in range(2)]
    zb = [(sb(nc, es, "c_zb%d" % i, [128, 512], BF16), Buf()) for i in range(3)]
    yo = [(sb(nc, es, "c_yo%d" % i, [128, 512], BF16), Buf()) for i in range(3)]
    n = 0
    nq = 0
    for b in range(8):
        hc = HC[b % 2]
        for T in range(4):
            p, bp = pc[n % 3]
            n += 1
            for k in range(31):
                P.mm(p[:], Dg[:, T, k, :], H[:, T, b * 512 + k:b * 512 + k + 512], k == 0, k == 30,
                     r=[bc, bH[T]], w=[bp])
            P.act(hc[T][0][:], p[:], AF.Identity, bias=vecs["e_conv_b"][:, T:T + 1], r=[bp, bc], w=[hc[T][1]])
            sq, bsq = SQ[nq % 2]
            nq += 1
            P.act(sq[:], p[:], AF.Square, bias=vecs["e_conv_b"][:, T:T + 1], r=[bp, bc], w=[bsq])
            P.mm(pmu[0][:], ones[:], hc[T][0][:], T == 0, T == 3, r=[bc, hc[T][1]], w=[pmu[1]])
            P.mm(psq[0][:], ones[:], sq[:], T == 0, T == 3, r=[bc, bsq], w=[psq[1]])
        P.act(mu[0][:], pmu[0][:], AF.Copy, scale=1.0 / 512, r=[pmu[1]], w=[mu[1]])
        P.act(rs[0][:], pmu[0][:], AF.Square, scale=1.0 / 512, r=[pmu[1]], w=[rs[1]])
        P.stt(rs[0][:], psq[0][:], 1.0 / 512, rs[0][:], ALU.mult, ALU.subtract, r=[psq[1], rs[1]], w=[rs[1]])
        P.act(rs[0][:], rs[0][:], AF.Sqrt, bias=C.eps_t[:], r=[rs[1], C.b_eps], w=[rs[1]])
        P.op("dve", lambda e: e.reciprocal(rs[0][:], rs[0][:]), [rs[1]], [rs[1]])
        for T in range(4):
            t, bt = t1[T % 2]
            z, bz = zb[(b * 4 + T) % 3]
            y, by = yo[(b * 4 + T) % 3]
            P.dma("sp", z[:], zb_d[T][:, b * 512:(b + 1) * 512], w=[bz])
            P.tt("dve", t[:], hc[T][0][:], mu[0][:], ALU.subtract, r=[hc[T][1], mu[1]], w=[bt])
            P.tt("pool", t[:], t[:], rs[0][:], ALU.mult, r=[bt, rs[1]], w=[bt])
            P.act(t[:], t[:], AF.Silu, bias=vecs["e_ln_b"][:, T:T + 1], scale=vecs["e_ln_g"][:, T:T + 1],
                  r=[bt, bc], w=[bt])
            P.tt("dve", y[:], t[:], z[:], ALU.mult, r=[bt, bz], w=[by])
            P.dma("sp", ycat_d[4 + T][:, b * 512:(b + 1) * 512], y[:], r=[by])


def outproj_stage(C, w_ap, ycat_d, x_ap, o_ap, es, final_g=None):
    nc, P = C.nc, C.P
    wo = sb(nc, es, "wout", [128, 8, D], BF16)
    bwo = Buf()
    stage = [(sb(nc, es, "ost%d" % i, [128, D], F32), Buf()) for i in range(2)]
    load_cast(C, wo, bwo, lambda k: w_ap[k * 128:(k + 1) * 128, :], 8, D, stage)
    yc = [(sb(nc, es, "o_yc%d" % i, [128, 8, 128], BF16), Buf()) for i in range(3)]
    xs = [(sb(nc, es, "o_xs%d" % i, [128, D], F32), Buf()) for i in range(3)]
    ho = [(sb(nc, es, "o_ho%d" % i, [128, D], F32), Buf()) for i in range(2)]
    po = [(ps(nc, es, "o_ps%d" % i, [128, 512], F32), PBuf()) for i in range(4)]
    if final_g is not None:
        fg = sb(nc, es, "o_fg", [128, D], F32)
        bfg = Buf()
        P.dma("sp", fg[:], final_g.partition_broadcast(128), w=[bfg])
        ss = [(sb(nc, es, "o_ss%d" % i, [128, 1], F32), Buf()) for i in range(2)]
        junk = (sb(nc, es, "o_junk", [128, D], BF16), Buf())
    n = 0
    for tt in range(NT):
        y, by = yc[tt % 3]
        x, bx = xs[tt % 3]
        h, bh = ho[tt % 2]
        P.dma("sp", y[:], ycat_d[:, :, tt * 128:(tt + 1) * 128].rearrange("c p t -> p c t"), w=[by])
        P.dma("sp", x[:], x_ap[tt * 128:(tt + 1) * 128, :], w=[bx])
        for half in range(2):
            p, bp = po[n % 4]
            n += 1
            for c in range(8):
                P.mm(p[:], y[:, c, :], wo[:, c, half * 512:(half + 1) * 512], c == 0, c == 7, r=[by, bwo], w=[bp])
            P.tt("dve", h[:, half * 512:(half + 1) * 512], p[:], x[:, half * 512:(half + 1) * 512], ALU.add,
                 r=[bp, bx], w=[bh])
        if final_g is not None:
            s, bs = ss[tt % 2]
            P.act(junk[0][:], h[:], AF.Square, accum=s[:], r=[bh], w=[junk[1], bs])
            P.act(s[:], s[:], AF.Sqrt, bias=C.eps_t[:], scale=1.0 / D, r=[bs, C.b_eps], w=[bs])
            P.op("dve", lambda e, s=s: e.reciprocal(s[:], s[:]), [bs], [bs])
            P.stt(h[:], h[:], s[:], fg[:], ALU.mult, ALU.mult, r=[bh, bs, bfg], w=[bh])
        P.dma("sp", o_ap[tt * 128:(tt + 1) * 128, :], h[:], r=[bh])


def layer0(C, x_ap, h1_ap, ins, dbg=None):
    nc, P = C.nc, C.P
    with ExitStack() as es:
        U = sb(nc, es, "U", [128, 4, 16, 256], BF16)
        bU = [Buf("U%d" % t) for t in range(4)]
        H = sb(nc, es, "H", [128, 4, 30 + L], BF16)
        bH = [Buf("H%d" % t) for t in range(4)]
        for t in range(4):
            P.memset("pool", H[:, t, 0:30], 0.0, w=[bH[t]])
        za_d = nc.dram_tensor("za_d", [4, 128, L], BF16, kind="Internal").ap()
        zb_d = nc.dram_tensor("zb_d", [4, 128, L], BF16, kind="Internal").ap()

        with ExitStack() as ea:
            W = sb(nc, ea, "W0", [128, 8, E_IN], BF16)
            bW = Buf("W0")
            g_t = sb(nc, ea, "g0", [128, 8], F32)
            bg = Buf("g0")
            P.dma("sp", g_t[:], ins["norm_g"][0].rearrange("(k p) -> p k", p=128), w=[bg], slow=True)
            stage = [(sb(nc, ea, "wst%d" % i, [128, E_IN], F32), Buf()) for i in range(2)]
            load_weight_bf16(C, W, bW, ins["e_w_in"][0], E_IN, g_t, bg, stage)
            st = {
                "xs": [(sb(nc, ea, "xs%d" % i, [128, D], F32), Buf()) for i in range(3)],
                "ss": [(sb(nc, ea, "ss%d" % i, [128, 1], F32), Buf()) for i in range(3)],
                "xn": [(sb(nc, ea, "xn%d" % i, [128, D], BF16), Buf()) for i in range(3)],
                "junk": (sb(nc, ea, "junk", [128, D], BF16), Buf()),
                "pst": [(ps(nc, ea, "pst%d" % i, [128, 8, 128], BF16), PBuf()) for i in range(2)],
                "xs_i": 0, "pst_i": 0,
            }
            hnTs = [(sb(nc, ea, "hnT%d" % i, [128, 8, 512], BF16), Buf()) for i in range(2)]
            pmm = [(ps(nc, ea, "pmm%d" % i, [128, 512], F32), PBuf()) for i in range(4)]
            sig = [(sb(nc, ea, "sig%d" % i, [128, 512], F32), Buf()) for i in range(2)]
            zst = [(sb(nc, ea, "zst%d" % i, [128, 512], BF16), Buf()) for i in range(4)]
            pi = 0
            zi = 0
            order = [0, 1, 2, 3, 4, 5, 6, 7, 12, 8, 13, 9, 14, 10, 15, 11, 16, 17, 18, 19]
            for blk in range(NB):
                hnT, bh = hnTs[blk % 2]
                rms_tiles_to_hnT(C, lambda tt: x_ap[tt * 128:(tt + 1) * 128, :], hnT, bh, blk, st)
                for ct in order:
                    pm, bp = pmm[pi % 4]
                    pi += 1
                    for k in range(8):
                        P.mm(pm[:], W[:, k, ct * 128:(ct + 1) * 128], hnT[:, k, :], k == 0, k == 7,
                             r=[bW, bh], w=[bp])
                    m0 = blk * 32
                    if ct < 4:
                        P.cp("dve", U[:, ct, :, m0:m0 + 32],
                             pm[:].rearrange("p (m j) -> p j m", j=16), r=[bp], w=[bU[ct]])
                    elif ct < 8:
                        z, bz = zst[zi % 4]
                        zi += 1
                        P.act(z[:].rearrange("p (j m) -> p j m", j=16),
                              pm[:].rearrange("p (m j) -> p j m", j=16), AF.Silu, r=[bp], w=[bz])
                        P.dma("sp", za_d[ct - 4].rearrange("p (j m) -> p j m", j=16)[:, :, m0:m0 + 32],
                              z[:].rearrange("p (j m) -> p j m", j=16), r=[bz])
                    elif ct < 12:
                        sg, bs = sig[(ct - 8) % 2]
                        P.tt("dve", H[:, ct - 8, 30 + blk * 512:30 + (blk + 1) * 512], pm[:], sg[:], ALU.mult,
                             r=[bp, bs], w=[bH[ct - 8]])
                    elif ct < 16:
                        sg, bs = sig[(ct - 12) % 2]
                        P.act(sg[:], pm[:], AF.Sigmoid, r=[bp], w=[bs])
                    else:
                        z, bz = zst[zi % 4]
                        zi += 1
                        P.act(z[:], pm[:], AF.Silu, r=[bp], w=[bz])
                        P.dma("sp", zb_d[ct - 16][:, blk * 512:(blk + 1) * 512], z[:], r=[bz])
            if dbg is not None and dbg.get("stage") == "A":
                P.dma("sp", dbg["out"][0:128, 0:L], U[:, 0].rearrange("p j m -> p (j m)"), r=[bU[0]])
                P.dma("sp", dbg["out"][128:256, 0:L], H[:, 0, 30:30 + L], r=[bH[0]])
            P.barrier()
        if dbg is not None and dbg.get("stage") == "A":
            return

        G = sb(nc, es, "G", [128, 4, 16, 256], BF16)
        bG = [Buf("G%d" % t) for t in range(4)]
        with ExitStack() as e5:
            S5 = s5_setup(C, ins, e5)
            s5_main(C, S5, U, bU, G, bG, e5, dbg)
            P.barrier()
        if dbg is not None and dbg.get("stage") == "S5":
            return
        ycat_d = nc.dram_tensor("ycat_d", [8, 128, L], BF16, kind="Internal").ap()
        with ExitStack() as eg:
            glu_stage(C, ins, G, bG, za_d, ycat_d, eg)
            P.barrier()
        with ExitStack() as ec:
            conv_stage(C, ins, H, bH, zb_d, ycat_d, ec)
            P.barrier()
        with ExitStack() as eo:
            outproj_stage(C, ins["e_w_out"][0], ycat_d, x_ap, h1_ap, eo)
            P.barrier()
        if dbg is not None and dbg.get("stage") == "L0":
            return


NIT = 18
cQ, cZ, cK, cQI, cKI, cV, cWI, W1C = 0, 1024, 2048, 2560, 3072, 3200, 3456, 3464
AX = mybir.AxisListType


def rope_tables(C, es, cosT, sinT, bT):
    nc, P = C.nc, C.P
    b = Buf("ropesetup")
    I32 = mybir.dt.int32

    def t(name, shape, dt=F32):
        return sb(nc, es, "rp_" + name, shape, dt)

    pidx_i, tmp_i = t("pidx_i", [128, 1], I32), t("tmp_i", [128, 1], I32)
    P.op("pool", lambda e: e.iota(pidx_i[:], pattern=[[0, 1]], base=0, channel_multiplier=1), (), [b])
    invp, m16, sg, f = t("invp", [128, 1]), t("m16", [128, 1]), t("sg", [128, 1]), t("f", [128, 1])
    P.ts("dve", tmp_i[:], pidx_i[:], 7, None, ALU.bitwise_and, r=[b], w=[b])
    P.cp("dve", f[:], tmp_i[:], r=[b], w=[b])
    P.act(invp[:], f[:], AF.Exp, scale=-math.log(500000.0) / 8.0, r=[b], w=[b])
    P.ts("dve", tmp_i[:], pidx_i[:], 48, None, ALU.bitwise_and, r=[b], w=[b])
    P.cp("dve", f[:], tmp_i[:], r=[b], w=[b])
    P.ts("dve", m16[:], f[:], 0.5, None, ALU.is_lt, r=[b], w=[b])
    P.tt("dve", invp[:], invp[:], m16[:], ALU.mult, r=[b], w=[b])
    P.ts("dve", tmp_i[:], pidx_i[:], 56, None, ALU.bitwise_and, r=[b], w=[b])
    P.cp("dve", f[:], tmp_i[:], r=[b], w=[b])
    P.ts("dve", sg[:], f[:], 0.5, None, ALU.is_lt, r=[b], w=[b])
    P.ts("dve", sg[:], sg[:], -2.0, 1.0, ALU.mult, ALU.add, r=[b], w=[b])
    CH = 1024
    pos_i, pos, ang, ph, kf = t("pos_i", [128, CH], I32), t("pos", [128, CH]), t("ang", [128, CH]), t("ph", [128, CH]), t("kf", [128, CH])
    ki_ = t("ki", [128, CH], I32)
    for c in range(L // CH):
        P.op("pool", lambda e, c=c: e.iota(pos_i[:], pattern=[[1, CH]], base=c * CH, channel_multiplier=0), (), [b])
        P.cp("dve", pos[:], pos_i[:], r=[b], w=[b])
        P.ts("dve", ang[:], pos[:], invp[:], None, ALU.mult, r=[b], w=[b])
        for dst, off in ((sinT, 0.0), (cosT, 0.5 * PI)):
            P.ts("dve", ph[:], ang[:], off, None, ALU.add, r=[b], w=[b])
            P.ts("dve", ki_[:], ph[:], 1.0 / (2 * PI), None, ALU.mult, r=[b], w=[b])
            P.cp("dve", kf[:], ki_[:], r=[b], w=[b])
            P.stt(ph[:], kf[:], -CW1, ph[:], ALU.mult, ALU.add, r=[b], w=[b])
            P.stt(ph[:], kf[:], -CW2, ph[:], ALU.mult, ALU.add, r=[b], w=[b])
            P.ts("dve", kf[:], ph[:], PI, None, ALU.is_gt, r=[b], w=[b])
            P.stt(ph[:], kf[:], -2 * PI, ph[:], ALU.mult, ALU.add, r=[b], w=[b])
            P.ts("dve", ph[:], ph[:], -PI, PI, ALU.max, ALU.min, r=[b], w=[b])
            P.act(ph[:], ph[:], AF.Sin, r=[b], w=[b])
            if dst is sinT:
                P.ts("dve", dst[:, c * CH:(c + 1) * CH], ph[:], sg[:], None, ALU.mult, r=[b], w=[b, bT])
            else:
                P.cp("dve", dst[:, c * CH:(c + 1) * CH], ph[:], r=[b], w=[b, bT])


def layer1(C, x_ap, o_ap, ins):
    nc, P = C.nc, C.P
    with ExitStack() as es:
        KT2 = sb(nc, es, "KT2", [128, 4, L], BF16)
        bKT = Buf("KT2")
        Vtok = sb(nc, es, "Vtok", [128, NT, 4, 66], BF16)
        bV = Buf("Vtok")
        KI2 = sb(nc, es, "KI2", [128, L], BF16)
        bKI = Buf("KI2")
        WI = sb(nc, es, "WI", [128, NT, 8], F32)
        bWI = Buf("WI")
        P.memset("pool", Vtok[:, :, :, 64:65], 1.0, w=[bV])
        q_d = nc.dram_tensor("q_d", [8, 128, L], BF16, kind="Internal").ap()
        z_d = nc.dram_tensor("z_d", [8, 128, L], BF16, kind="Internal").ap()
        qi_d = nc.dram_tensor("qi_d", [4, 128, L], BF16, kind="Internal").ap()
        w_in = ins["o_w_in"][0]

        with ExitStack() as ea:
            cosT = sb(nc, ea, "cosT", [128, L], BF16)
            sinT = sb(nc, ea, "sinT", [128, L], BF16)
            bT = Buf("ropeT")
            with ExitStack() as er:
                rope_tables(C, er, cosT, sinT, bT)
                P.barrier()
            if getattr(C, "stop", None) == "R":
                return
            Pm = sb(nc, ea, "Pm", [128, 128], BF16)
            bPm = Buf("Pm")
            P.memset("pool", Pm[:], 0.0, w=[bPm])
            for base in (0, 64):
                P.cp("dve", Pm[:, base:base + 8], C.ident_b[:, base + 8:base + 16], r=[C.b_ident], w=[bPm])
                P.cp("dve", Pm[:, base + 8:base + 16], C.ident_b[:, base:base + 8], r=[C.b_ident], w=[bPm])
            W = sb(nc, ea, "W1", [128, 8, W1C], BF16)
            bW = Buf("W1")
            g_t = sb(nc, ea, "g1", [128, 8], F32)
            bg = Buf("g1")
            P.dma("sp", g_t[:], ins["norm_g"][1].rearrange("(k p) -> p k", p=128), w=[bg], slow=True)
            stg = sb(nc, ea, "w1st", [128, W1C], F32)
            bst = Buf("w1st")
            for k in range(8):
                rows = w_in[k * 128:(k + 1) * 128, :]
                P.dma("sp", stg[:, 0:2048], rows[:, 0:2048], w=[bst])
                for a in range(2):
                    P.dma("sp", stg[:, cK:cK + 512].rearrange("p (g two d) -> p g two d", two=2, d=64)[:, :, a, :],
                          rows[:, 2048:2304].rearrange("p (g d) -> p g d", d=64), w=[bst])
                    P.dma("sp", stg[:, cKI + 64 * a:cKI + 64 * a + 64], rows[:, 3072:3136], w=[bst])
                P.dma("sp", stg[:, cQI:cQI + 512], rows[:, 2560:3072], w=[bst])
                P.dma("sp", stg[:, cV:cV + 256], rows[:, 2304:2560], w=[bst])
                P.dma("sp", stg[:, cWI:cWI + 8], rows[:, 3136:3144], w=[bst])
                P.ts("dve", W[:, k, 0:1792], stg[:, 0:1792], g_t[:, k:k + 1], None, ALU.mult, r=[bst, bg], w=[bW])
                P.ts("pool", W[:, k, 1792:W1C], stg[:, 1792:W1C], g_t[:, k:k + 1], None, ALU.mult, r=[bst, bg], w=[bW])
            if getattr(C, "stop", None) == "W":
                return
            st = {
                "xs": [(sb(nc, ea, "b_xs%d" % i, [128, D], F32), Buf()) for i in range(2)],
                "ss": [(sb(nc, ea, "b_ss%d" % i, [128, 1], F32), Buf()) for i in range(2)],
                "xn": [(sb(nc, ea, "b_xn%d" % i, [128, D], BF16), Buf()) for i in range(2)],
                "junk": (sb(nc, ea, "b_junk", [128, D], BF16), Buf()),
                "pst": [(ps(nc, ea, "b_pst%d" % i, [128, 8, 128], BF16), PBuf()) for i in range(2)],
                "xs_i": 0, "pst_i": 0,
            }
            hnTs = [(sb(nc, ea, "b_hnT%d" % i, [128, 8, 512], BF16), Buf()) for i in range(2)]
            pmm = [(ps(nc, ea, "b_pmm%d" % i, [128, 512], F32), PBuf()) for i in range(3)]
            prr = [(ps(nc, ea, "b_prr%d" % i, [128, 512], F32), PBuf()) for i in range(2)]
            pvv = (ps(nc, ea, "b_pvv", [128, 512], F32), PBuf())
            xb = [(sb(nc, ea, "b_xb%d" % i, [128, 512], BF16), Buf()) for i in range(2)]
            ta = [(sb(nc, ea, "b_ta%d" % i, [128, 512], F32), Buf()) for i in range(2)]
            tb = [(sb(nc, ea, "b_tb%d" % i, [128, 512], F32), Buf()) for i in range(2)]
            so = [(sb(nc, ea, "b_so%d" % i, [128, 512], BF16), Buf()) for i in range(4)]
            n_mm = n_r = n_so = 0
            tiles = [("q", cQ + 128 * i, i) for i in range(8)] + [("z", cZ + 128 * i, i) for i in range(8)] + \
                    [("k", cK + 128 * i, i) for i in range(4)] + [("qi", cQI + 128 * i, i) for i in range(4)] + \
                    [("ki", cKI, 0)]
            sub = getattr(C, "stop", None)
            nblk = NB
            do_v = True
            if sub is not None and sub.startswith("B1") and len(sub) > 2:
                nblk = 1
                kinds = {"q": ["q"], "z": ["z"], "k": ["k"], "i": ["ki"], "v": []}[sub[2]]
                tiles = [t_ for t_ in tiles if t_[0] in kinds]
                do_v = sub[2] == "v"
            for blk in range(nblk):
                hnT, bh = hnTs[blk % 2]
                rms_tiles_to_hnT(C, lambda tt: x_ap[tt * 128:(tt + 1) * 128, :], hnT, bh, blk, st)
                sl = slice(blk * 512, (blk + 1) * 512)
                for kind, c0, idx in tiles:
                    pm, bp = pmm[n_mm % 3]
                    n_mm += 1
                    for k in range(8):
                        P.mm(pm[:], W[:, k, c0:c0 + 128], hnT[:, k, :], k == 0, k == 7, r=[bW, bh], w=[bp])
                    if kind == "z":
                        s, bs = so[n_so % 4]
                        n_so += 1
                        P.act(s[:], pm[:], AF.Silu, r=[bp], w=[bs])
                        P.dma("sp", z_d[idx][:, sl], s[:], r=[bs])
                        continue
                    x2, bx2 = xb[n_r % 2]
                    pr, bpr = prr[n_r % 2]
                    a, ba = ta[n_r % 2]
                    b2, bb2 = tb[n_r % 2]
                    n_r += 1
                    P.cp("act", x2[:], pm[:], r=[bp], w=[bx2])
                    P.mm(pr[:], Pm[:], x2[:], True, True, r=[bPm, bx2], w=[bpr])
                    P.tt("dve", a[:], pm[:], cosT[:, sl], ALU.mult, r=[bp, bT], w=[ba])
                    P.tt("dve", b2[:], pr[:], sinT[:, sl], ALU.mult, r=[bpr, bT], w=[bb2])
                    if kind == "k":
                        P.tt("pool", KT2[:, idx, sl], a[:], b2[:], ALU.add, r=[ba, bb2], w=[bKT])
                    elif kind == "ki":
                        P.tt("pool", KI2[:, sl], a[:], b2[:], ALU.add, r=[ba, bb2], w=[bKI])
                    else:
                        s, bs = so[n_so % 4]
                        n_so += 1
                        P.tt("pool", s[:], a[:], b2[:], ALU.add, r=[ba, bb2], w=[bs])
                        dst = q_d if kind == "q" else qi_d
                        P.dma("sp", dst[idx][:, sl], s[:], r=[bs])
                for i in range(4 if do_v else 0):
                    tt_ = blk * 4 + i
                    pv, bpv = pvv
                    for k in range(8):
                        P.mm(pv[:, 0:264], hnT[:, k, i * 128:(i + 1) * 128], W[:, k, cV:cV + 264], k == 0, k == 7,
                             r=[bh, bW], w=[bpv])
                    import os
                    vv = os.environ.get("VVAR", "")
                    if vv != "2":
                        P.cp("act", Vtok[:, tt_, :, 0:64], pv[:, 0:256].rearrange("p (g d) -> p g d", d=64), r=[bpv], w=[bV])
                    if vv != "1":
                        P.ts("dve", WI[:, tt_, :], pv[:, 256:264], (8 ** -0.5) * (64 ** -0.5), None, ALU.mult,
                             r=[bpv], w=[bWI])
            P.barrier()

        if getattr(C, "stop", "").startswith("B1"):
            return
        with ExitStack() as eb:
            w_out = ins["o_w_out"][0]
            wo = sb(nc, eb, "wo64", [64, 16, D], BF16)
            bwo = Buf("wo64")
            wst = [(sb(nc, eb, "wo_st%d" % i, [64, 1, D], F32), Buf()) for i in range(2)]
            for hp in range(16):
                s, bs = wst[hp % 2]
                P.dma("sp", s[:], w_out[hp * 64:(hp + 1) * 64, :].rearrange("(h d) c -> d h c", d=64), w=[bs])
                P.cp("dve" if hp % 2 == 0 else "pool", wo[:, hp:hp + 1, :], s[:], r=[bs], w=[bwo])
            fg = sb(nc, eb, "fg", [128, D], F32)
            bfg = Buf("fg")
            P.dma("sp", fg[:], ins["final_g"].partition_broadcast(128), w=[bfg])
            ones = sb(nc, eb, "Esel", [128, 128], F32)
            bon = Buf("Esel")
            P.memset("pool", ones[:], 0.0, w=[bon])
            P.memset("pool", ones[64:65, :], 1.0, w=[bon])
            cpow = sb(nc, eb, "cpow", [128, NIT + 1], F32)
            bcp = Buf("cpow")
            for n in range(NIT + 1):
                P.memset("pool", cpow[:, n:n + 1], 2.0 ** -(n + 1), w=[bcp])
            SC = [(sb(nc, eb, "SC%d" % i, [128, L], F32), Buf()) for i in range(1)]
            Mq = (sb(nc, eb, "Mq", [128, L], BF16), Buf())
            junk = Mq
            MT = [(sb(nc, eb, "MT%d" % i, [128, NT, 128], BF16), Buf()) for i in range(2)]
            qq = [(sb(nc, eb, "qq%d" % i, [64, 16, 128], BF16), Buf()) for i in range(2)]
            zq = [(sb(nc, eb, "zq%d" % i, [64, 16, 128], BF16), Buf()) for i in range(2)]
            qiq = [(sb(nc, eb, "qiq%d" % i, [128, 4, 128], BF16), Buf()) for i in range(2)]
            xs = [(sb(nc, eb, "c_xs%d" % i, [128, D], F32), Buf()) for i in range(2)]
            rl = [(sb(nc, eb, "rl%d" % i, [128, 512], F32), Buf()) for i in range(2)]
            sm = {nm: (sb(nc, eb, "sm_" + nm, [128, 1], F32), Buf()) for nm in ("rmax", "rmin", "w0", "mid", "cnt", "t", "thr")}
            hs = (sb(nc, eb, "hs", [128, NIT + 1], F32), Buf())
            pe_ = [(sb(nc, eb, "pexp%d" % i, [128, 512], BF16), Buf()) for i in range(5)]
            pmk = [(sb(nc, eb, "pmk%d" % i, [128, 512], BF16), Buf()) for i in range(5)]
            osb = [(sb(nc, eb, "osb%d" % i, [65, 512], F32), Buf()) for i in range(2)]
            on_ = [(sb(nc, eb, "on%d" % i, [64, 512], F32), Buf()) for i in range(2)]
            rbc = [(sb(nc, eb, "rbc%d" % i, [64, 512], F32), Buf()) for i in range(2)]
            og = [(sb(nc, eb, "og%d" % i, [64, 16, 128], BF16), Buf()) for i in range(2)]
            ss = [(sb(nc, eb, "c_ss%d" % i, [128, 1], F32), Buf()) for i in range(2)]
            fjunk = (sb(nc, eb, "c_fjunk", [128, D], BF16), Buf())
            psc = [(ps(nc, eb, "psc%d" % i, [128, 512], F32), PBuf()) for i in range(2)]
            ptr = [(ps(nc, eb, "ptr%d" % i, [128, 4, 128], BF16), PBuf()) for i in range(1)]
            pl_ = [(ps(nc, eb, "pl%d" % i, [128, 512], F32), PBuf()) for i in range(2)]
            po_ = [(ps(nc, eb, "po%d" % i, [128, 512], F32), PBuf()) for i in range(2)]
            pop = (ps(nc, eb, "pop", [128, 512], F32), PBuf())
            pbc = (pop[0][0:64, :], pop[1])
            cn = {"sc": 0, "rl": 0, "l": 0, "e": 0, "g": 0}

            def gen_S(qt):
                nk = (qt + 1) * 128
                nkb = (nk + 511) // 512
                tsl = slice(qt * 128, (qt + 1) * 128)
                q_t, bq = qq[qt % 2]
                z_t, bz = zq[qt % 2]
                qi_t, bqi = qiq[qt % 2]
                x_t, bx = xs[qt % 2]
                P.dma("sp", q_t[:], q_d.rearrange("t (a d) l -> d (t a) l", a=2)[:, :, tsl], w=[bq])
                P.dma("sp", qi_t[:], qi_d[:, :, tsl].rearrange("t p l -> p t l"), w=[bqi])
                P.dma("sp", z_t[:], z_d.rearrange("t (a d) l -> d (t a) l", a=2)[:, :, tsl], w=[bz])
                P.dma("sp", x_t[:], x_ap[tsl, :], w=[bx])
                sc, bsc = SC[0]
                for hi in range(8):
                    til, half = hi // 2, hi % 2
                    for kb in range(nkb):
                        w_ = min(512, nk - 512 * kb)
                        ksl = slice(512 * kb, 512 * kb + w_)
                        p, bp = psc[cn["sc"] % len(psc)]
                        cn["sc"] += 1
                        P.mm(p[:, 0:w_], qi_t[64 * half:64 * half + 64, til, :], KI2[64 * half:64 * half + 64, ksl],
                             True, True, r=[bqi, bKI], w=[bp])
                        if hi == 0:
                            P.ts("dve", sc[:, ksl], p[:, 0:w_], 0.0, WI[:, qt, 0:1], ALU.max, ALU.mult,
                                 r=[bp, bWI], w=[bsc])
                        else:
                            r_, br = rl[cn["rl"] % 2]
                            cn["rl"] += 1
                            P.act(r_[:, 0:w_], p[:, 0:w_], AF.Relu, r=[bp], w=[br])
                            P.stt(sc[:, ksl], r_[:, 0:w_], WI[:, qt, hi:hi + 1], sc[:, ksl], ALU.mult, ALU.add,
                                  r=[br, bWI, bsc], w=[bsc])
                        yield
                rmax, rmin, w0, mid, cnt, t_, thr = (sm[k_] for k_ in ("rmax", "rmin", "w0", "mid", "cnt", "t", "thr"))
                P.op("dve", lambda e: e.tensor_reduce(rmax[0][:], sc[:, 0:nk], AX.X, ALU.max), [bsc], [rmax[1]])
                P.op("dve", lambda e: e.tensor_reduce(rmin[0][:], sc[:, 0:nk], AX.X, ALU.min), [bsc], [rmin[1]])
                P.memset("pool", sc[0:64, nk - 64:nk], -1e30, w=[bsc])
                P.tt("dve", w0[0][:], rmax[0][:], rmin[0][:], ALU.subtract, r=[rmax[1], rmin[1]], w=[w0[1]])
                P.tt("dve", hs[0][:], cpow[:], w0[0][:].broadcast_to([128, NIT + 1]), ALU.mult, r=[bcp, w0[1]], w=[hs[1]])
                P.tt("dve", mid[0][:], rmin[0][:], hs[0][:, 0:1], ALU.add, r=[rmin[1], hs[1]], w=[mid[1]])
                yield
                for n in range(NIT):
                    P.ts("dve", junk[0][:, 0:nk], sc[:, 0:nk], mid[0][:], 0.0, ALU.is_ge, ALU.add, accum=cnt[0][:],
                         r=[bsc, mid[1]], w=[junk[1], cnt[1]])
                    P.ts("dve", t_[0][:], cnt[0][:], 255.5, hs[0][:, n:n + 1], ALU.is_gt, ALU.mult,
                         r=[cnt[1], hs[1]], w=[t_[1]])
                    P.stt(mid[0][:], t_[0][:], hs[0][:, n + 1:n + 2], mid[0][:], ALU.subtract, ALU.add,
                          r=[t_[1], hs[1], mid[1]], w=[mid[1]])
                    yield
                P.tt("dve", thr[0][:], mid[0][:], hs[0][:, NIT:NIT + 1], ALU.subtract, r=[mid[1], hs[1]], w=[thr[1]])
                P.ts("dve", Mq[0][:, 0:nk], sc[:, 0:nk], thr[0][:], None, ALU.is_ge, r=[bsc, thr[1]], w=[Mq[1]])
                mt, bmt = MT[qt % 2]
                for k0 in range(0, qt + 1, 4):
                    kn = min(4, qt + 1 - k0)
                    pt, bpt = ptr[0]
                    for kk in range(kn):
                        kt = k0 + kk
                        P.tr(pt[:, kk, :], Mq[0][:, kt * 128:(kt + 1) * 128], C.ident_b[:], r=[Mq[1], C.b_ident], w=[bpt])
                    P.cp("act", mt[:, k0:k0 + kn, :], pt[:, 0:kn, :], r=[bpt], w=[bmt])
                    yield

            def n_S(qt):
                nk = (qt + 1) * 128
                return 8 * ((nk + 511) // 512) + 1 + NIT + (qt + 4) // 4

            def gen_A(qt):
                tsl = slice(qt * 128, (qt + 1) * 128)
                q_t, bq = qq[qt % 2]
                z_t, bz = zq[qt % 2]
                x_t, bx = xs[qt % 2]
                mt, bmt = MT[qt % 2]
                og_t, bog = og[qt % 2]
                iters = [(g, kt) for g in range(4) for kt in range(qt + 1)]
                pend = {}

                def stage1(i):
                    g, kt = iters[i]
                    pl, bpl = pl_[cn["l"] % 2]
                    cn["l"] += 1
                    ks = slice(kt * 128, (kt + 1) * 128)
                    P.mm(pl[:], KT2[0:64, g, ks], q_t[0:64, 4 * g:4 * g + 4, :], True, True, r=[bKT, bq], w=[bpl])
                    pe, bpe = pe_[cn["e"] % 5]
                    pk, bpk = pmk[cn["e"] % 5]
                    cn["e"] += 1
                    P.act(pe[:], pl[:], AF.Exp, scale=0.125, r=[bpl], w=[bpe])
                    P.tt("dve" if (i % 3 == 2) else "pool", pk[:].rearrange("p (s t) -> p s t", s=4),
                         pe[:].rearrange("p (s t) -> p s t", s=4),
                         mt[:, kt, :].unsqueeze(1).broadcast_to([128, 4, 128]), ALU.mult, r=[bpe, bmt], w=[bpk])
                    pend[i] = (pk, bpk)

                def stage2(i):
                    g, kt = iters[i]
                    pk, bpk = pend.pop(i)
                    po, bpo = po_[g % 2]
                    P.mm(po[0:65, :], Vtok[:, kt, g, 0:65], pk[:], kt == 0, kt == qt, r=[bV, bpk], w=[bpo])
                    if kt != qt:
                        return
                    o_s, bos = osb[cn["g"] % 2]
                    o_n, bonn = on_[cn["g"] % 2]
                    r_b, brb = rbc[cn["g"] % 2]
                    cn["g"] += 1
                    P.cp("act", o_s[:], po[0:65, :], r=[bpo], w=[bos])
                    P.mm(pop[0][:], ones[0:65, :], o_s[0:65, :], True, True, r=[bon, bos], w=[pop[1]])
                    P.op("dve", lambda e, r_b=r_b: e.reciprocal(r_b[:], pop[0][0:64, :]), [pop[1]], [brb])
                    P.tt("dve", o_n[:], o_s[0:64, :], r_b[:], ALU.mult, r=[bos, brb], w=[bonn])
                    P.tt("pool", og_t[:, 4 * g:4 * g + 4, :], o_n[:].rearrange("p (s t) -> p s t", s=4),
                         z_t[:, 4 * g:4 * g + 4, :], ALU.mult, r=[bonn, bz], w=[bog])

                LOOK = 4
                for i in range(min(LOOK, len(iters))):
                    stage1(i)
                for i in range(len(iters)):
                    if i + LOOK < len(iters):
                        stage1(i + LOOK)
                    stage2(i)
                    yield
                h, bh = x_t, bx
                for half in range(2):
                    for hh in range(16):
                        P.mm(pop[0][:], og_t[:, hh, :], wo[:, hh, half * 512:(half + 1) * 512], hh == 0, hh == 15,
                             r=[bog, bwo], w=[pop[1]])
                    P.tt("dve", h[:, half * 512:(half + 1) * 512], pop[0][:], x_t[:, half * 512:(half + 1) * 512], ALU.add,
                         r=[pop[1]], w=[bh])
                    yield
                s_, bs_ = ss[qt % 2]
                P.act(fjunk[0][:], h[:], AF.Square, accum=s_[:], r=[bh], w=[fjunk[1], bs_])
                P.act(s_[:], s_[:], AF.Sqrt, bias=C.eps_t[:], scale=1.0 / D, r=[bs_, C.b_eps], w=[bs_])
                P.op("dve", lambda e: e.reciprocal(s_[:], s_[:]), [bs_], [bs_])
                P.stt(h[:], h[:], s_[:], fg[:], ALU.mult, ALU.mult, r=[bh, bs_, bfg], w=[bh])
                P.dma("sp", o_ap[tsl, :], h[:], r=[bh])
                yield

            def n_A(qt):
                return 4 * (qt + 1) + 2 + 1

            for _ in gen_S(0):
                pass
            for qt in range(NT):
                ga = gen_A(qt)
                gs = gen_S(qt + 1) if qt + 1 < NT else None
                na, ns = n_A(qt), (n_S(qt + 1) if gs is not None else 0)
                ia = is_ = 0
                a_done = False
                s_done = gs is None
                while not (a_done and s_done):
                    if not s_done and (a_done or is_ * na <= ia * ns):
                        try:
                            next(gs)
                            is_ += 1
                        except StopIteration:
                            s_done = True
                    else:
                        try:
                            next(ga)
                            ia += 1
                        except StopIteration:
                            a_done = True
            P.barrier()


def build(mode="full"):
    nc = bass.Bass("TRN2", target_bir_lowering=False)
    ins = {}

    def din(name, shape):
        ins[name] = nc.dram_tensor(name, list(shape), F32, kind="ExternalInput").ap()

    need0 = mode in ("full", "L0", "A", "S5")
    need1 = mode in ("full",) + ("L1", "B1", "R", "W", "B1q", "B1z", "B1k", "B1i", "B1v")
    din("x", (L, D))
    din("norm_g", (2, D))
    if need0:
        for nm in L0_NAMES:
            din(nm, SHAPES[nm])
    if need1:
        for nm in L1_NAMES:
            din(nm, SHAPES[nm])
    out = nc.dram_tensor("out", [L, D], F32, kind="ExternalOutput").ap()
    dbg = None
    if mode in ("A", "S5"):
        dbg = {"stage": mode,
               "out": nc.dram_tensor("dbg", [512, L], BF16, kind="ExternalOutput").ap(),
               "out32": nc.dram_tensor("dbg32", [512, L], F32, kind="ExternalOutput").ap()}
    with ExitStack() as es:
        C = Ctx()
        C.nc = nc
        C.es = es
        C.P = Prog(nc, es)
        build_consts(C)
        if mode == "full":
            h1 = nc.dram_tensor("h1_d", [L, D], F32, kind="Internal").ap()
            layer0(C, ins["x"], h1, ins, None)
            layer1(C, h1, out, ins)
        elif mode in ("L1", "B1", "R", "W", "B1q", "B1z", "B1k", "B1i", "B1v"):
            C.stop = mode if mode != "L1" else ""
            layer1(C, ins["x"], out, ins)
        else:
            layer0(C, ins["x"], out, ins, dbg)
        C.P.emit()
    return nc


SHAPES = {"e_w_in": (1, D, E_IN), "e_lam_re": (1, 32, 64), "e_lam_im": (1, 32, 64), "e_log_step": (1, 32),
          "e_b_re": (1, 32, 64, 16), "e_b_im": (1, 32, 64, 16), "e_c_re": (1, 32, 16, 64), "e_c_im": (1, 32, 16, 64),
          "e_d_skip": (1, 512), "e_w_glu": (1, 512, 512), "e_b_glu": (1, 512), "e_conv_w": (1, 31, 512),
          "e_conv_b": (1, 512), "e_ln_g": (1, 512), "e_ln_b": (1, 512), "e_w_out": (1, D, D),
          "o_w_in": (1, D, O_IN), "o_w_out": (1, D, D), "final_g": (D,)}
L0_NAMES = ["e_w_in", "e_lam_re", "e_lam_im", "e_log_step", "e_b_re", "e_b_im", "e_c_re", "e_c_im", "e_d_skip",
            "e_w_glu", "e_b_glu", "e_conv_w", "e_conv_b", "e_ln_g", "e_ln_b", "e_w_out"]
L1_NAMES = ["o_w_in", "o_w_out", "final_g"]


def run(inputs, mode="full", x_override=None, trace=False):
    nc = build(mode)
    names = ["norm_g"]
    if mode in ("full", "L0", "A", "S5"):
        names += L0_NAMES
    if mode in ("full",) + ("L1", "B1", "R", "W", "B1q", "B1z", "B1k", "B1i", "B1v"):
        names += L1_NAMES
    shared = {k: np.ascontiguousarray(np.asarray(inputs[k], dtype=np.float32)) for k in names}
    x = x_override if x_override is not None else inputs["x"]
    in_maps = []
    for c in range(8):
        m = dict(shared)
        m["x"] = np.ascontiguousarray(np.asarray(x[c], dtype=np.float32))
        in_maps.append(m)
    return run_bass_kernel_spmd(nc, in_maps, core_ids=list(range(8)), trace=trace)


MODE = "full"


def kernel(**inputs):
    if MODE == "full":
        res = run(inputs, "full")
        return np.stack([np.asarray(r["out"]) for r in res.results], axis=0).astype(np.float32)
    r0 = run(inputs, "L0")
    h1 = [np.asarray(r["out"]) for r in r0.results]
    r1 = run(inputs, "L1", x_override=h1)
    return np.stack([np.asarray(r["out"]) for r in r1.results], axis=0).astype(np.float32)
```
